# Optimizing a Trainium2 kernel written in Bass

```python
import math
import jax
import jax.numpy as jnp
from jax import lax
import numpy as np


D_MODEL = 1024
BATCH = 8
SEQ = 2048
DEPTH = 2

F32 = jnp.float32
GRID_W = 64
CTX_LEN = 256
Q_BLOCK = 128
ROPE_THETA = 10000.0
LN_EPS = 1e-5
DEEPNORM_ALPHA = (2 * DEPTH) ** 0.25
DEEPNORM_BETA = (8 * DEPTH) ** -0.25
N_EVEN = (DEPTH + 1) // 2
N_ODD = DEPTH // 2
MIX_WIDTH = D_MODEL

A_HEAD_DIM = 64
A_HEADS = (MIX_WIDTH // 2) // A_HEAD_DIM
A_WIDTH = A_HEADS * A_HEAD_DIM
A_DECAY_LORA = 64
A_ICLR_LORA = 64
A_GATE_LORA = 128
A_GN_EPS = 64e-5
A_IN = 3 * A_WIDTH + 2 * A_DECAY_LORA + 2 * A_ICLR_LORA + A_GATE_LORA

B_HEAD_DIM = 64
B_V_DIM = 2 * B_HEAD_DIM
B_HEADS = (MIX_WIDTH - A_WIDTH) // B_V_DIM
B_WIDTH = B_HEADS * B_V_DIM
B_QK = B_HEADS * 2 * B_HEAD_DIM
B_IN = 2 * B_QK + B_WIDTH
B_SUBLN_EPS = 1e-5
EVEN_IN = A_IN + B_IN

C_HEAD_DIM = 128
C_HEADS = MIX_WIDTH // C_HEAD_DIM
C_KV_HEADS = 2
C_GROUP = C_HEADS // C_KV_HEADS
C_Q = C_HEADS * C_HEAD_DIM
C_KV = C_KV_HEADS * C_HEAD_DIM
ODD_IN = C_Q + 2 * C_KV
QK_NORM_EPS = 1e-6

N_EXPERTS = 16
N_GROUPS = 4
EXPERTS_PER_GROUP = N_EXPERTS // N_GROUPS
TOP_K = 2
EXPERT_FF = 512

kernel_name = 'hybrid_rwkv7_diffattn_gqa_grouped_moe'


def _layer_norm(x, g, b):
    xf = x.astype(F32)
    mu = jnp.mean(xf, -1, keepdims=True)
    var = jnp.mean(jnp.square(xf - mu), -1, keepdims=True)
    return ((xf - mu) * lax.rsqrt(var + LN_EPS) * g + b).astype(x.dtype)


def _rms_norm(x, g, eps):
    xf = x.astype(F32)
    y = xf * lax.rsqrt(jnp.mean(jnp.square(xf), -1, keepdims=True) + eps)
    return (y * g).astype(x.dtype)


def _modulate(x, shift, scale):
    return x * (1.0 + scale) + shift


def _rope_tables(row_pos, col_pos, head_dim):
    axis_dim = head_dim // 2
    inv = ROPE_THETA ** (-jnp.arange(0, axis_dim, 2, dtype=F32) / axis_dim)
    ang = jnp.concatenate([row_pos[:, None] * inv, col_pos[:, None] * inv], -1)
    return jnp.cos(ang), jnp.sin(ang)


def _apply_rope(x, cos, sin):
    half = x.shape[-1] // 2
    shape = (1, x.shape[1]) + (1,) * (x.ndim - 3) + (half,)
    cs, sn = cos.reshape(shape), sin.reshape(shape)
    xf = x.astype(F32)
    x1, x2 = xf[..., :half], xf[..., half:]
    return jnp.concatenate([x1 * cs - x2 * sn, x1 * sn + x2 * cs], -1).astype(x.dtype)


def _sweep_query_blocks(fn, q):
    bsz, s = q.shape[:2]
    nb = s // Q_BLOCK
    qb = jnp.moveaxis(q.reshape((bsz, nb, Q_BLOCK) + q.shape[2:]), 1, 0)
    out = jnp.moveaxis(lax.map(fn, qb), 0, 1)
    return out.reshape((bsz, s) + out.shape[3:])


def _centred_shift(p):
    zero = jnp.zeros_like(p[:, :1])
    prev = jnp.concatenate([zero, p[:, :-1]], axis=1)
    nxt = jnp.concatenate([p[:, 1:], zero], axis=1)
    return 0.5 * (prev + nxt)


def _heads_a(z):
    return z.reshape(z.shape[:-1] + (A_HEADS, A_HEAD_DIM))


def _rwkv_features(pa, mu, w0, w2, a0, a2, g2, k_k, k_a):
    bsz, t, _ = pa.shape
    u = pa + (_centred_shift(pa) - pa) * mu
    o1, o2, o3 = A_WIDTH, 2 * A_WIDTH, 3 * A_WIDTH
    o4 = o3 + 2 * A_DECAY_LORA
    o5 = o4 + 2 * A_ICLR_LORA
    r, k, v = u[..., :o1], u[..., o1:o2], u[..., o2:o3]
    wd = u[..., o3:o4].reshape(bsz, t, 2, A_DECAY_LORA)
    ad = u[..., o4:o5].reshape(bsz, t, 2, A_ICLR_LORA)
    gd = u[..., o5:]
    w_log = -jax.nn.softplus(-(w0 + jnp.einsum('btdl,dlc->btdc', jnp.tanh(wd), w2))) - 0.5
    decay = jnp.exp(-jnp.exp(w_log.astype(F32)))
    a = jax.nn.sigmoid(a0 + jnp.einsum('btdl,dlc->btdc', ad, a2))
    g = jax.nn.sigmoid(gd) @ g2
    kk = _heads_a(k * k_k).astype(F32)
    kk = kk / jnp.maximum(jnp.sqrt(jnp.sum(jnp.square(kk), -1, keepdims=True)), 1e-12)
    k_dir = k[:, :, None, :] * (1.0 + (a - 1.0) * k_a)
    return _heads_a(r), _heads_a(k_dir), _heads_a(v), _heads_a(decay), kk, _heads_a(a), g


def _wkv7_scan(feats, d, s0, reverse):
    r, k_dir, v, decay, kk, a, _ = feats
    tm = lambda z: jnp.moveaxis(z.astype(F32), 1, 0)
    xs = (tm(r), tm(decay[:, :, d]), tm(k_dir[:, :, d]), tm(v), tm(-kk), tm(kk * a[:, :, d]))

    def step(s, inp):
        r_t, w_t, k_t, v_t, a_t, b_t = inp
        sa = jnp.einsum('bhvk,bhk->bhv', s, a_t)
        s = s * w_t[:, :, None, :] + sa[..., :, None] * b_t[..., None, :] + v_t[..., :, None] * k_t[..., None, :]
        return s, jnp.einsum('bhvk,bhk->bhv', s, r_t)

    s_final, ys = lax.scan(step, s0, xs, reverse=reverse)
    return jnp.moveaxis(ys, 0, 1), s_final


def _rwkv_readout(y, feats, r_k, lnx_g, lnx_b):
    r, k_dir, v, _, _, _, g = feats
    bsz, t = y.shape[:2]
    mu = jnp.mean(y, -1, keepdims=True)
    var = jnp.mean(jnp.square(y - mu), -1, keepdims=True)
    yn = ((y - mu) * lax.rsqrt(var + A_GN_EPS)).reshape(bsz, t, A_WIDTH) * lnx_g + lnx_b
    bonus = jnp.sum(r * (k_dir[:, :, 0] + k_dir[:, :, 1]) * r_k, -1, keepdims=True) * v
    return ((yn + bonus.reshape(bsz, t, A_WIDTH)) * g).astype(g.dtype)


def _rwkv7_group(pa_c, pa_l, mu, w0, w2, a0, a2, g2, k_k, k_a, r_k, lnx_g, lnx_b):
    feats_c = _rwkv_features(pa_c, mu, w0, w2, a0, a2, g2, k_k, k_a)
    feats_l = _rwkv_features(pa_l, mu, w0, w2, a0, a2, g2, k_k, k_a)
    s0 = jnp.zeros((pa_l.shape[0], A_HEADS, A_HEAD_DIM, A_HEAD_DIM), F32)
    ys_c, ys_l = [], []
    for d, rev in enumerate((False, True)):
        y_c, s_ctx = _wkv7_scan(feats_c, d, s0, rev)
        y_l, _ = _wkv7_scan(feats_l, d, s_ctx, rev)
        ys_c.append(y_c)
        ys_l.append(y_l)
    out_c = _rwkv_readout(ys_c[0] + ys_c[1], feats_c, r_k, lnx_g, lnx_b)
    out_l = _rwkv_readout(ys_l[0] + ys_l[1], feats_l, r_k, lnx_g, lnx_b)
    return out_c, out_l


def _diff_core(q, k, v, lam):
    s = jnp.einsum('bqhmd,bkhmd->bhmqk', q, k).astype(F32) * (B_HEAD_DIM ** -0.5)
    p = jax.nn.softmax(s, axis=-1)
    w = p[:, :, 0] - lam * p[:, :, 1]
    return jnp.einsum('bhqk,bkhe->bqhe', w.astype(v.dtype), v)


def _diff_attn_group(pb_c, pb_l, lam_vecs, subln_g, lambda_init, cos, sin):
    def split(p):
        bsz, t, _ = p.shape
        q = p[..., :B_QK].reshape(bsz, t, B_HEADS, 2, B_HEAD_DIM)
        k = p[..., B_QK:2 * B_QK].reshape(bsz, t, B_HEADS, 2, B_HEAD_DIM)
        v = p[..., 2 * B_QK:].reshape(bsz, t, B_HEADS, B_V_DIM)
        return q, k, v

    q_c, k_c, v_c = split(pb_c)
    q_l, k_l, v_l = split(pb_l)
    q_l = _apply_rope(q_l, cos, sin)
    k_l = _apply_rope(k_l, cos, sin)
    lv = lam_vecs.astype(F32)
    lam = jnp.exp(jnp.sum(lv[0] * lv[1])) - jnp.exp(jnp.sum(lv[2] * lv[3])) + lambda_init
    k_all = jnp.concatenate([k_c, k_l], axis=1)
    v_all = jnp.concatenate([v_c, v_l], axis=1)
    o_l = _sweep_query_blocks(lambda qb: _diff_core(qb, k_all, v_all, lam), q_l)
    o_c = _diff_core(q_c, k_c, v_c, lam)

    def post(o):
        bsz, t = o.shape[:2]
        return (_rms_norm(o, subln_g, B_SUBLN_EPS) * (1.0 - lambda_init)).reshape(bsz, t, B_WIDTH)

    return post(o_c), post(o_l)


def _even_mixer(hc, hl, w_in, w_out, a_mu, a_w0, a_w2, a_a0, a_a2, a_g2, a_kk, a_ka, a_rk,
                a_lnx_g, a_lnx_b, b_lam, b_subln_g, lambda_init, cos, sin):
    pc = hc @ w_in
    pl = hl @ w_in
    ya_c, ya_l = _rwkv7_group(pc[..., :A_IN], pl[..., :A_IN], a_mu, a_w0, a_w2, a_a0, a_a2, a_g2,
                              a_kk, a_ka, a_rk, a_lnx_g, a_lnx_b)
    yb_c, yb_l = _diff_attn_group(pc[..., A_IN:], pl[..., A_IN:], b_lam, b_subln_g, lambda_init, cos, sin)
    out_c = jnp.concatenate([ya_c, yb_c], -1) @ w_out
    out_l = jnp.concatenate([ya_l, yb_l], -1) @ w_out
    return out_c, out_l


def _gqa_core(q, k, v):
    s = jnp.einsum('bqhgd,bkhd->bhgqk', q, k).astype(F32) * (C_HEAD_DIM ** -0.5)
    p = jax.nn.softmax(s, axis=-1).astype(v.dtype)
    return jnp.einsum('bhgqk,bkhd->bqhgd', p, v)


def _odd_mixer(hc, hl, w_in, w_out, qn_g, kn_g, cos, sin, need_ctx):
    def proj(h, with_q):
        bsz, t, _ = h.shape
        p = h @ (w_in if with_q else w_in[:, C_Q:])
        off = C_Q if with_q else 0
        q = None
        if with_q:
            q = _rms_norm(p[..., :C_Q].reshape(bsz, t, C_KV_HEADS, C_GROUP, C_HEAD_DIM), qn_g, QK_NORM_EPS)
        k = _rms_norm(p[..., off:off + C_KV].reshape(bsz, t, C_KV_HEADS, C_HEAD_DIM), kn_g, QK_NORM_EPS)
        v = p[..., off + C_KV:].reshape(bsz, t, C_KV_HEADS, C_HEAD_DIM)
        return q, k, v

    q_l, k_l, v_l = proj(hl, True)
    q_l = _apply_rope(q_l, cos, sin)
    k_l = _apply_rope(k_l, cos, sin)
    q_c, k_c, v_c = proj(hc, need_ctx)
    k_all = jnp.concatenate([k_c, k_l], axis=1)
    v_all = jnp.concatenate([v_c, v_l], axis=1)
    bsz, s = hl.shape[:2]
    o_l = _sweep_query_blocks(lambda qb: _gqa_core(qb, k_all, v_all), q_l)
    out_l = o_l.reshape(bsz, s, C_Q) @ w_out
    out_c = None
    if need_ctx:
        out_c = _gqa_core(q_c, k_c, v_c).reshape(hc.shape[0], hc.shape[1], C_Q) @ w_out
    return out_c, out_l


def _moe(h, router_w, router_b, w1, w3, w2):
    t = h.shape[0]
    logits = (h @ router_w).astype(F32) + router_b.astype(F32)
    probs = jax.nn.softmax(logits, axis=-1)
    gscore = jnp.sum(lax.top_k(probs.reshape(t, N_GROUPS, EXPERTS_PER_GROUP), TOP_K)[0], -1)
    best = jnp.argmax(gscore, axis=-1)
    in_grp = (jnp.arange(N_EXPERTS) // EXPERTS_PER_GROUP)[None, :] == best[:, None]
    top_p, top_i = lax.top_k(jnp.where(in_grp, probs, -jnp.inf), TOP_K)
    top_w = top_p / jnp.sum(top_p, -1, keepdims=True)
    gates = jnp.sum(jax.nn.one_hot(top_i, N_EXPERTS, dtype=F32) * top_w[..., None], axis=1).astype(h.dtype)
    out = jnp.zeros_like(h)
    for e in range(N_EXPERTS):
        hid = jax.nn.silu(h @ w1[e]) * (h @ w3[e])
        out = out + gates[:, e:e + 1] * (hid @ w2[e])
    return out


def setup_inputs(seed: int = 0) -> dict:
    key = jax.random.key(seed)
    ks = iter(jax.random.split(key, 48))

    def nrm(shape, scale):
        return jax.random.normal(next(ks), shape, F32) * scale

    def unif(shape, lo, hi):
        return jax.random.uniform(next(ks), shape, F32, lo, hi)

    d = D_MODEL
    ev_out = A_WIDTH + B_WIDTH
    return {
        'x': nrm((BATCH, SEQ, d), 1.0),
        'c': nrm((BATCH, d), 1.0),
        'ctx': nrm((BATCH, CTX_LEN, d), 1.0),
        'c_ctx': nrm((d,), 1.0),
        'router_w': nrm((d, N_EXPERTS), d ** -0.5),
        'router_b': nrm((N_EXPERTS,), 0.01),
        'ada_w': nrm((DEPTH, d, 6 * d), 0.5 * d ** -0.5),
        'ada_b': nrm((DEPTH, 6 * d), 0.01),
        'ln1_g': 1.0 + nrm((DEPTH, d), 0.02),
        'ln1_b': nrm((DEPTH, d), 0.02),
        'ln2_g': 1.0 + nrm((DEPTH, d), 0.02),
        'ln2_b': nrm((DEPTH, d), 0.02),
        'moe_w1': nrm((DEPTH, N_EXPERTS, d, EXPERT_FF), d ** -0.5),
        'moe_w3': nrm((DEPTH, N_EXPERTS, d, EXPERT_FF), d ** -0.5),
        'moe_w2': nrm((DEPTH, N_EXPERTS, EXPERT_FF, d), DEEPNORM_BETA * EXPERT_FF ** -0.5),
        'ev_w_in': nrm((N_EVEN, d, EVEN_IN), d ** -0.5),
        'ev_w_out': nrm((N_EVEN, ev_out, d), DEEPNORM_BETA * ev_out ** -0.5),
        'ev_a_mu': unif((N_EVEN, A_IN), 0.0, 1.0),
        'ev_a_w0': unif((N_EVEN, 2, A_WIDTH), -6.0, -1.0),
        'ev_a_w2': nrm((N_EVEN, 2, A_DECAY_LORA, A_WIDTH), 0.5 * A_DECAY_LORA ** -0.5),
        'ev_a_a0': nrm((N_EVEN, 2, A_WIDTH), 0.1),
        'ev_a_a2': nrm((N_EVEN, 2, A_ICLR_LORA, A_WIDTH), 0.5 * A_ICLR_LORA ** -0.5),
        'ev_a_g2': nrm((N_EVEN, A_GATE_LORA, A_WIDTH), A_GATE_LORA ** -0.5),
        'ev_a_kk': 0.85 + nrm((N_EVEN, A_WIDTH), 0.05),
        'ev_a_ka': 1.0 + nrm((N_EVEN, A_WIDTH), 0.05),
        'ev_a_rk': nrm((N_EVEN, A_HEADS, A_HEAD_DIM), 0.1),
        'ev_a_lnx_g': 1.0 + nrm((N_EVEN, A_WIDTH), 0.02),
        'ev_a_lnx_b': nrm((N_EVEN, A_WIDTH), 0.02),
        'ev_b_lam': nrm((N_EVEN, 4, B_HEAD_DIM), 0.1),
        'ev_b_subln_g': 1.0 + nrm((N_EVEN, B_V_DIM), 0.02),
        'od_w_in': nrm((N_ODD, d, ODD_IN), d ** -0.5),
        'od_w_out': nrm((N_ODD, C_Q, d), DEEPNORM_BETA * C_Q ** -0.5),
        'od_qn_g': 1.0 + nrm((N_ODD, C_HEAD_DIM), 0.02),
        'od_kn_g': 1.0 + nrm((N_ODD, C_HEAD_DIM), 0.02),
    }


def reference(x, c, ctx, c_ctx, router_w, router_b, ada_w, ada_b, ln1_g, ln1_b, ln2_g, ln2_b,
              moe_w1, moe_w3, moe_w2, ev_w_in, ev_w_out, ev_a_mu, ev_a_w0, ev_a_w2, ev_a_a0, ev_a_a2,
              ev_a_g2, ev_a_kk, ev_a_ka, ev_a_rk, ev_a_lnx_g, ev_a_lnx_b, ev_b_lam, ev_b_subln_g,
              od_w_in, od_w_out, od_qn_g, od_kn_g):
    bsz, n_lat, d = x.shape
    rows = n_lat // GRID_W
    rr, cc = jnp.meshgrid(jnp.arange(rows), jnp.arange(GRID_W), indexing='ij')
    row_pos = rr.reshape(-1).astype(F32)
    col_pos = cc.reshape(-1).astype(F32)
    cos_b, sin_b = _rope_tables(row_pos, col_pos, B_HEAD_DIM)
    cos_c, sin_c = _rope_tables(row_pos, col_pos, C_HEAD_DIM)
    s_lat = jax.nn.silu(c)
    s_ctx = jax.nn.silu(c_ctx)
    xl, xc = x, ctx
    for i in range(DEPTH):
        last = i == DEPTH - 1
        j = i // 2
        mod_l = jnp.split((s_lat @ ada_w[i] + ada_b[i])[:, None, :], 6, axis=-1)
        mod_c = jnp.split(s_ctx @ ada_w[i] + ada_b[i], 6, axis=-1)
        hl = _modulate(xl, mod_l[0], mod_l[1])
        hc = _modulate(xc, mod_c[0], mod_c[1])
        if i % 2 == 0:
            lambda_init = 0.8 - 0.6 * math.exp(-0.3 * i)
            mix_c, mix_l = _even_mixer(hc, hl, ev_w_in[j], ev_w_out[j], ev_a_mu[j], ev_a_w0[j], ev_a_w2[j],
                                       ev_a_a0[j], ev_a_a2[j], ev_a_g2[j], ev_a_kk[j], ev_a_ka[j], ev_a_rk[j],
                                       ev_a_lnx_g[j], ev_a_lnx_b[j], ev_b_lam[j], ev_b_subln_g[j],
                                       lambda_init, cos_b, sin_b)
        else:
            mix_c, mix_l = _odd_mixer(hc, hl, od_w_in[j], od_w_out[j], od_qn_g[j], od_kn_g[j],
                                      cos_c, sin_c, not last)
        xl = _layer_norm(DEEPNORM_ALPHA * xl + mod_l[2] * mix_l, ln1_g[i], ln1_b[i])
        hl = _modulate(xl, mod_l[3], mod_l[4])
        if last:
            ffn_l = _moe(hl.reshape(-1, d), router_w, router_b, moe_w1[i], moe_w3[i], moe_w2[i]).reshape(hl.shape)
        else:
            xc = _layer_norm(DEEPNORM_ALPHA * xc + mod_c[2] * mix_c, ln1_g[i], ln1_b[i])
            hc = _modulate(xc, mod_c[3], mod_c[4])
            n_c = hc.shape[0] * hc.shape[1]
            ffn = _moe(jnp.concatenate([hc.reshape(-1, d), hl.reshape(-1, d)], axis=0),
                       router_w, router_b, moe_w1[i], moe_w3[i], moe_w2[i])
            ffn_c = ffn[:n_c].reshape(hc.shape)
            ffn_l = ffn[n_c:].reshape(hl.shape)
            xc = _layer_norm(DEEPNORM_ALPHA * xc + mod_c[5] * ffn_c, ln2_g[i], ln2_b[i])
        xl = _layer_norm(DEEPNORM_ALPHA * xl + mod_l[5] * ffn_l, ln2_g[i], ln2_b[i])
    return xl
```

```python
import math
from contextlib import ExitStack
import numpy as np
import concourse.bass as bass
import concourse.mybir as mybir
from concourse.bass_utils import run_bass_kernel_spmd

F32 = mybir.dt.float32
BF16 = mybir.dt.bfloat16
AF = mybir.ActivationFunctionType
ALU = mybir.AluOpType
AX = mybir.AxisListType

D = 1024
NCH = 8
TC = 256
TL = 2048
T = TC + TL
NT = T // 128
ALPHA = 4 ** 0.25
LN_EPS = 1e-5
import os
STOP = int(os.environ.get('KSTOP', '0'))
SEGS = [(0, 256)] + [(256 + 512 * i, 256 + 512 * (i + 1)) for i in range(4)]


class Prog:
    def __init__(self, nc, es):
        self.nc = nc
        self.E = {'pe': nc.tensor, 'act': nc.scalar, 'dve': nc.vector, 'pool': nc.gpsimd, 'sp': nc.sync}
        self.NR = 8
        self.sem = {}
        for e in ('pe', 'act', 'dve', 'pool'):
            self.sem['c_' + e] = es.enter_context(nc.semaphore('c_' + e))
        for q in ('sp', 'pool'):
            for i in range(self.NR):
                k = 'd_%s_%d' % (q, i)
                self.sem[k] = es.enter_context(nc.semaphore(k))
        self.val = {k: 0 for k in self.sem}
        self.dn = {'sp': 0, 'pool': 0}
        self.seen = {e: {} for e in self.E}
        self.recs = {}
        self.ro = set()
        self.nops = 0

    @staticmethod
    def box(ap):
        name = ap.tensor.name
        dims = ap.ap
        off = ap.offset
        if 'DRAM' in str(ap.space).upper():
            return name, 0, 1, off, off + sum(s * (c - 1) for s, c in dims) + 1
        if 'PSUM' in str(ap.space).upper():
            return name, 0, 128, 0, 1 << 30
        pst, pn = dims[0]
        if pst <= 0:
            p0, f0 = 0, off
        else:
            p0, f0 = off // pst, off % pst
        f1 = f0 + sum(s * (c - 1) for s, c in dims[1:]) + 1
        return name, p0, p0 + pn, f0, f1

    def _wait(self, eng, key, v):
        if self.seen[eng].get(key, 0) >= v:
            return
        self.E[eng].wait_ge(self.sem[key], v)
        self.seen[eng][key] = v

    def op(self, eng, emit, reads=(), writes=(), dma=False):
        deps = {}

        def need(r):
            (_, _, _, _, w, key, v, reng) = r
            if not dma and reng == eng:
                if eng == 'pe':
                    return
            if deps.get(key, 0) < v:
                deps[key] = v

        rb = []
        wb = []
        for ap in reads:
            b = self.box(ap)
            if b[0] in self.ro:
                continue
            rb.append(b)
            for r in self.recs.get(b[0], ()):
                if r[4] and r[0] < b[2] and b[1] < r[1] and r[2] < b[4] and b[3] < r[3]:
                    need(r)
        for ap in writes:
            b = self.box(ap)
            wb.append(b)
            for r in self.recs.get(b[0], ()):
                if r[0] < b[2] and b[1] < r[1] and r[2] < b[4] and b[3] < r[3]:
                    need(r)
        if dma:
            slot = self.dn[eng] % self.NR
            use = self.dn[eng] // self.NR
            self.dn[eng] += 1
            key = 'd_%s_%d' % (eng, slot)
            if use > 0 and deps.get(key, 0) < 16 * use:
                deps[key] = 16 * use
            val = 16 * (use + 1)
            inc = 16
            reng = 'dma'
        else:
            key = 'c_' + eng
            val = self.val[key] + 1
            inc = 1
            reng = eng
        for k, v in deps.items():
            self._wait(eng, k, v)
        ins = emit(self.E[eng])
        ins.then_inc(self.sem[key], inc)
        self.val[key] = val
        self.nops += 1
        for b in wb:
            lst = self.recs.setdefault(b[0], [])
            lst[:] = [r for r in lst if not (b[1] <= r[0] and r[1] <= b[2] and b[3] <= r[2] and r[3] <= b[4])]
            lst.append((b[1], b[2], b[3], b[4], True, key, val, reng))
        for b in rb:
            lst = self.recs.setdefault(b[0], [])
            lst[:] = [r for r in lst if not ((not r[4]) and r[7] == reng and reng != 'dma'
                                             and r[0] == b[1] and r[1] == b[2] and r[2] == b[3] and r[3] == b[4])]
            lst.append((b[1], b[2], b[3], b[4], False, key, val, reng))
        return ins

    def barrier(self):
        for e in self.E:
            for k, v in self.val.items():
                if v > 0:
                    self._wait(e, k, v)
        self.recs.clear()

    def mm(self, out, lhsT, rhs, start=True, stop=True):
        return self.op('pe', lambda e: e.matmul(out, lhsT, rhs, start=start, stop=stop),
                       reads=[lhsT, rhs], writes=[out])

    def mmf(self, out, lhsT, rhs, start=True, stop=True):
        return self.op('pe', lambda e: e.matmul(out, lhsT, rhs, start=start, stop=stop), reads=[lhsT, rhs], writes=[out])

    def tr(self, out, in_, ident):
        return self.op('pe', lambda e: e.transpose(out, in_, ident), reads=[in_, ident], writes=[out])

    def act(self, out, in_, func, bias=None, scale=1.0, accum_out=None):
        rd = [in_]
        kw = {}
        if bias is not None:
            kw['bias'] = bias
            if not isinstance(bias, (int, float)):
                rd.append(bias)
        if not isinstance(scale, (int, float)):
            rd.append(scale)
        wr = [out]
        if accum_out is not None:
            kw['accum_out'] = accum_out
            wr.append(accum_out)
        return self.op('act', lambda e: e.activation(out, in_, func, scale=scale, **kw), reads=rd, writes=wr)

    def tt(self, eng, out, in0, in1, op):
        return self.op(eng, lambda e: e.tensor_tensor(out, in0, in1, op), reads=[in0, in1], writes=[out])

    def ts(self, eng, out, in0, s1, s2=None, op0=ALU.mult, op1=None):
        rd = [in0] + [s for s in (s1, s2) if s is not None and not isinstance(s, (int, float))]
        if op1 is None:
            return self.op(eng, lambda e: e.tensor_scalar(out, in0, s1, None, op0), reads=rd, writes=[out])
        return self.op(eng, lambda e: e.tensor_scalar(out, in0, s1, s2, op0, op1), reads=rd, writes=[out])

    def stt(self, eng, out, in0, scalar, in1, op0, op1):
        rd = [in0, in1] + ([] if isinstance(scalar, (int, float)) else [scalar])
        return self.op(eng, lambda e: e.scalar_tensor_tensor(out, in0, scalar, in1, op0, op1), reads=rd, writes=[out])

    def copy(self, eng, out, in_):
        if eng == 'act':
            return self.op('act', lambda e: e.copy(out, in_), reads=[in_], writes=[out])
        return self.op(eng, lambda e: e.tensor_copy(out, in_), reads=[in_], writes=[out])

    def memset(self, eng, ap, v):
        return self.op(eng, lambda e: e.memset(ap, v), writes=[ap])

    def reduce(self, eng, out, in_, op, axis=AX.X):
        return self.op(eng, lambda e: e.tensor_reduce(out, in_, axis, op), reads=[in_], writes=[out])

    def recip(self, out, in_):
        return self.op('dve', lambda e: e.reciprocal(out, in_), reads=[in_], writes=[out])

    def dma(self, q, out, in_):
        return self.op(q, lambda e: e.dma_start(out=out, in_=in_), reads=[in_], writes=[out], dma=True)


def _featT(v):
    v = np.asarray(v, np.float32).reshape(-1)
    return np.ascontiguousarray(v.reshape(v.size // 128, 128).T)


class _Cols:
    def __init__(self):
        self.parts = []
        self.off = {}
        self.n = 0

    def add(self, name, arr):
        self.off[name] = self.n
        self.parts.append(arr)
        self.n += arr.shape[1]

    def build(self):
        return np.ascontiguousarray(np.concatenate(self.parts, axis=1))


def _rope_tables(head_dim):
    rows = TL // 64
    rr, cc = np.meshgrid(np.arange(rows), np.arange(64), indexing='ij')
    row_pos = rr.reshape(-1).astype(np.float32)
    col_pos = cc.reshape(-1).astype(np.float32)
    axis_dim = head_dim // 2
    inv = (np.float32(10000.0) ** (-np.arange(0, axis_dim, 2, dtype=np.float32) / axis_dim)).astype(np.float32)
    ang = np.concatenate([row_pos[:, None] * inv, col_pos[:, None] * inv], -1).astype(np.float32)
    return np.cos(ang).astype(np.float32), np.sin(ang).astype(np.float32)


SV_LAYOUT = {}
RV_LAYOUT = {}


def _pack_small(inp, b):
    sv = _Cols()
    sv.add('c', _featT(inp['c'][b]))
    sv.add('cctx', _featT(inp['c_ctx']))
    for i in range(2):
        sv.add('ada_b%d' % i, _featT(inp['ada_b'][i]))
        for nm in ('ln1_g', 'ln1_b', 'ln2_g', 'ln2_b'):
            sv.add('%s%d' % (nm, i), _featT(inp[nm][i]))
    sv.add('mu', _featT(inp['ev_a_mu'][0]))
    for d in range(2):
        sv.add('w0_%d' % d, _featT(inp['ev_a_w0'][0, d]))
        sv.add('a0_%d' % d, _featT(inp['ev_a_a0'][0, d]))
    for nm in ('kk', 'ka', 'lnx_g', 'lnx_b'):
        sv.add(nm, _featT(inp['ev_a_' + nm][0]))
    sv.add('rk', _featT(inp['ev_a_rk'][0]))
    sv.add('subln_g', _featT(inp['ev_b_subln_g'][0]))
    rv = _Cols()
    rv.add('lam', np.asarray(inp['ev_b_lam'][0], np.float32).reshape(1, 256))
    rv.add('router_b', np.asarray(inp['router_b'], np.float32).reshape(1, 16))
    rv.add('qn_g', np.asarray(inp['od_qn_g'][0], np.float32).reshape(1, 128))
    rv.add('kn_g', np.asarray(inp['od_kn_g'][0], np.float32).reshape(1, 128))
    SV_LAYOUT.update(sv.off)
    SV_LAYOUT['_n'] = sv.n
    RV_LAYOUT.update(rv.off)
    RV_LAYOUT['_n'] = rv.n
    return sv.build(), rv.build()


def build_program(nsv, nrv, taps=()):
    nc = bass.Bass("TRN2", target_bir_lowering=False)
    dram = {}

    def din(name, shape, dt=F32):
        dram[name] = nc.dram_tensor(name, list(shape), dt, kind="ExternalInput").ap()
        return dram[name]

    def dscr(name, shape, dt=F32):
        kind = "ExternalOutput" if name in taps else "Internal"
        dram[name] = nc.dram_tensor(name, list(shape), dt, kind=kind).ap()
        return dram[name]

    SPEC = {
        'xin': [T, D], 'sv': [128, nsv], 'rv': [1, nrv], 'ident': [128, 128],
        'ada_w': [2, D, 6 * D], 'ev_w_in': [D, 3456], 'ev_w_out': [D, D],
        'od_w_in': [D, 1536], 'od_w_out': [D, D],
        'moe_w1': [2, 16, D, 512], 'moe_w3': [2, 16, D, 512], 'moe_w2': [2, 16, 512, D],
        'router_w': [D, 16], 'a_w2': [2, 64, 512], 'a_a2': [2, 64, 512], 'a_g2': [128, 512],
        'cosb': [TL, 32], 'sinb': [TL, 32], 'cosc': [TL, 64], 'sinc': [TL, 64], 'bdmask': [128, 128],
        'masks': [128, 6 * 128],
    }

    def W(name):
        if name not in dram:
            din(name, SPEC[name])
        return dram[name]

    xin = W('xin')
    sv_d = W('sv')
    rv_d = W('rv')
    ident_d = W('ident')
    ada_w = W('ada_w')
    bdmask_d = W('bdmask')
    out_d = nc.dram_tensor('out', [TL, D], F32, kind="ExternalOutput").ap()

    xt_d = dscr('xt_d', [128, NCH, T])
    ua_d = dscr('ua_d', [15, 128, T], BF16)
    ym_d = dscr('ym_d', [8, 128, T], BF16)

    SVO = SV_LAYOUT
    RVO = RV_LAYOUT

    with ExitStack() as es:
        es.enter_context(nc.allow_low_precision("bf16 matmul operands, fp32 accumulation"))
        P = Prog(nc, es)
        P.ro.update(SPEC.keys())

        def sb(name, shape, dt=F32, stack=es):
            return stack.enter_context(nc.sbuf_tensor(name, list(shape), dt))

        psf = [es.enter_context(nc.psum_tensor('psf%d' % i, [128, 512], F32)) for i in range(6)]
        psb = [es.enter_context(nc.psum_tensor('psb%d' % i, [128, 1024], BF16)) for i in range(2)]
        rot = {'f': 0, 'b': 0, 'e': 0}

        def PSF():
            rot['f'] += 1
            return psf[rot['f'] % 6]

        def PSB():
            rot['b'] += 1
            return psb[rot['b'] % 2]

        def EV():
            rot['e'] += 1
            return 'dve' if rot['e'] % 2 else 'act'

        SV = sb('SV', [128, nsv])
        RV = sb('RV', [128, nrv])
        IDF = sb('IDF', [128, 128])
        IDB = sb('IDB', [128, 128], BF16)
        ONESB = sb('ONESB', [128, 128], BF16)
        ONESF = sb('ONESF', [128, 128])
        BDM = sb('BDM', [128, 128])
        BDMB = sb('BDMB', [128, 128], BF16)
        MOD = sb('MOD', [128, 2, 48, 2])
        es_ht = ExitStack()
        HT = sb('HT', [128, NCH, T], BF16, es_ht)
        P.dma('sp', SV[:], sv_d[:, :])
        P.dma('sp', RV[:], rv_d.partition_broadcast(128))
        P.dma('sp', IDF[:], ident_d[:, :])
        P.copy('dve', IDB[:], IDF[:])
        P.dma('sp', BDM[:], bdmask_d[:, :])
        P.copy('dve', BDMB[:], BDM[:])
        P.memset('dve', ONESB[:], 1.0)
        P.memset('dve', ONESF[:], 1.0)

        def svc(name, j=0, n=1):
            o = SVO[name] + j
            return SV[:, o:o + n]

        def modc(i, m, c, w):
            return MOD[:, i, m * 8 + c, w:w + 1]

        with ExitStack() as ph:
            XT = sb('XT', [128, NCH, T], F32, ph)
            XS = [sb('XS%d' % i, [128, D], F32, ph) for i in range(2)]
            for tt in range(NT):
                xs = XS[tt % 2]
                P.dma('sp', xs[:], xin[tt * 128:(tt + 1) * 128, :])
                for hh in range(2):
                    ps = PSF()
                    for j in range(4):
                        c = hh * 4 + j
                        P.tr(ps[:, j * 128:(j + 1) * 128], xs[:, c * 128:(c + 1) * 128], IDF[:])
                    P.copy(EV(), XT[:, hh * 4:hh * 4 + 4, tt * 128:(tt + 1) * 128],
                           ps[:, :].rearrange("p (a b) -> p a b", a=4))
            for c in range(NCH):
                P.dma('sp', xt_d[:, c, :], XT[:, c, :])
            if STOP == 1:
                P.barrier(); nc._declared_inputs = set(k for k in dram if k in SPEC); return nc
            ST = sb('ST', [128, 8, 2], BF16, ph)
            P.act(ST[:, :, 0], svc('c', 0, 8), AF.Silu)
            P.act(ST[:, :, 1], svc('cctx', 0, 8), AF.Silu)
            AWF = [sb('AWF%d' % i, [128, 8, 512], F32, ph) for i in range(2)]
            AWB = [sb('AWB%d' % i, [128, 8, 512], BF16, ph) for i in range(2)]
            for i in range(2):
                for pc in range(12):
                    awf = AWF[(i * 12 + pc) % 2]
                    aw = AWB[(i * 12 + pc) % 2]
                    for kc in range(8):
                        P.dma('sp', awf[:, kc, :], ada_w[i, kc * 128:(kc + 1) * 128, pc * 512:(pc + 1) * 512])
                    P.copy('pool', aw[:, 0:4, :], awf[:, 0:4, :])
                    P.copy('act', aw[:, 4:8, :], awf[:, 4:8, :])
                    ps = PSF()
                    for j in range(4):
                        for kc in range(8):
                            P.mm(ps[:, 16 * j:16 * j + 2], aw[:, kc, j * 128:(j + 1) * 128], ST[:, kc, :],
                                 start=(kc == 0), stop=(kc == 7))
                    for j in range(4):
                        P.ts('dve', MOD[:, i, pc * 4 + j, :], ps[:, 16 * j:16 * j + 2],
                             svc('ada_b%d' % i, pc * 4 + j), None, ALU.add)
                for m in (1, 4):
                    P.ts('dve', MOD[:, i, m * 8:(m + 1) * 8, :], MOD[:, i, m * 8:(m + 1) * 8, :], 1.0, None, ALU.add)
                for m in (2, 5):
                    P.ts('dve', MOD[:, i, m * 8:(m + 1) * 8, :], MOD[:, i, m * 8:(m + 1) * 8, :], 1.0 / ALPHA, None, ALU.mult)
            if 'mod_tap' in taps:
                mdt = dscr('mod_tap', [128, 192])
                P.dma('sp', mdt[:, :], MOD[:].rearrange('p a b c -> p (a b c)'))
            if STOP == 2:
                P.barrier(); nc._declared_inputs = set(k for k in dram if k in SPEC); return nc
            for c in range(NCH):
                P.ts('dve', HT[:, c, 0:TC], XT[:, c, 0:TC], modc(0, 1, c, 1), modc(0, 0, c, 1), ALU.mult, ALU.add)
                P.ts('pool' if c % 2 else 'dve', HT[:, c, TC:T], XT[:, c, TC:T], modc(0, 1, c, 0), modc(0, 0, c, 0),
                     ALU.mult, ALU.add)
            if 'ht_tap' in taps:
                htt = dscr('ht_tap', [128, NCH, T], BF16)
                for c in range(NCH):
                    P.dma('sp', htt[:, c, :], HT[:, c, :])
            P.barrier()


        def rvc(name, j=0, n=1):
            o = RVO[name] + j
            return RV[:, o:o + n]

        def CV():
            rot['c'] = rot.get('c', 0) + 1
            return ('pool', 'act', 'dve')[rot['c'] % 3]

        def load_w(dst, src, rows_kc, c0, c1, stg):
            for kc in range(rows_kc):
                st = stg[kc % 2]
                P.dma('sp', st[:, 0:c1 - c0], src[kc * 128:(kc + 1) * 128, c0:c1])
                P.copy(CV(), dst[:, kc, :], st[:, 0:c1 - c0])

        ev_w_in = W('ev_w_in')
        with ExitStack() as ph:
            WA = sb('WA', [128, 8, 1920], BF16, ph)
            STG = [sb('STGa%d' % i, [128, 1920], F32, ph) for i in range(2)]
            load_w(WA, ev_w_in, 8, 0, 1920, STG)
            OMU = sb('OMU', [128, 15], F32, ph)
            HMU = sb('HMU', [128, 15], F32, ph)
            P.ts('dve', OMU[:], svc('mu', 0, 15), -1.0, 1.0, ALU.mult, ALU.add)
            P.ts('dve', HMU[:], svc('mu', 0, 15), 0.5, None, ALU.mult)
            PP = [sb('PP%d' % i, [128, 2312], F32, ph) for i in range(2)]
            P.memset('dve', PP[0][:], 0.0)
            P.memset('pool', PP[1][:], 0.0)
            T1 = sb('T1', [128, TL], F32, ph)
            T2 = sb('T2', [128, TL], F32, ph)
            UB = [sb('UB%d' % i, [128, T], BF16, ph) for i in range(2)]
            CO, LO = 1, 260
            for f in range(15):
                pp = PP[f % 2]
                for (a, b) in SEGS:
                    n = b - a
                    ps = PSF()
                    for kc in range(8):
                        P.mm(ps[:, :n], WA[:, kc, f * 128:(f + 1) * 128], HT[:, kc, a:b], start=(kc == 0), stop=(kc == 7))
                    off = CO + a if a < TC else LO + (a - TC)
                    P.copy(EV(), pp[:, off:off + n], ps[:, :n])
                ub = UB[f % 2]
                for (lo, n, t0) in ((CO, TC, 0), (LO, TL, TC)):
                    P.tt('pool', T1[:, :n], pp[:, lo - 1:lo - 1 + n], pp[:, lo + 1:lo + 1 + n], ALU.add)
                    P.ts('dve', T2[:, :n], pp[:, lo:lo + n], OMU[:, f:f + 1], None, ALU.mult)
                    P.stt('dve', T2[:, :n], T1[:, :n], HMU[:, f:f + 1], T2[:, :n], ALU.mult, ALU.add)
                    func = AF.Tanh if f == 12 else (AF.Sigmoid if f == 14 else AF.Identity)
                    P.act(ub[:, t0:t0 + n], T2[:, :n], func)
                P.dma('sp', ua_d[f, :, :], ub[:])
            P.barrier()
        if STOP == 3:
            es_ht.close()
            nc._declared_inputs = set(k for k in dram if k in SPEC)
            return nc

        LAMBDA_INIT0 = 0.8 - 0.6 * math.exp(0.0)
        with ExitStack() as ph:
            WB = sb('WB', [128, 8, 1536], BF16, ph)
            STG = [sb('STGb%d' % i, [128, 1536], F32, ph) for i in range(2)]
            load_w(WB, ev_w_in, 8, 1920, 3456, STG)
            CB = sb('CB', [128, 16, 32], F32, ph)
            SNB = sb('SNB', [128, 16, 32], F32, ph)
            P.dma('sp', CB[:], W('cosb').rearrange("(n p) f -> p n f", p=128))
            P.dma('sp', SNB[:], W('sinb').rearrange("(n p) f -> p n f", p=128))
            VB = sb('VB', [128, NT, 512], BF16, ph)
            QT = sb('QT', [128, 4, T], BF16, ph)
            KT = sb('KT', [128, 4, T], BF16, ph)
            QR = [sb('QR%d' % i, [128, 512], BF16, ph) for i in range(4)]
            RTM = [sb('RTM%d' % i, [128, 8, 32], F32, ph) for i in range(4)]
            qi = [0]

            def proj_b(tt, groups):
                tok = slice(tt * 128, (tt + 1) * 128)
                for g in groups:
                    ps = PSF()
                    for kc in range(8):
                        P.mm(ps[:, :], HT[:, kc, tok], WB[:, kc, g * 512:(g + 1) * 512], start=(kc == 0), stop=(kc == 7))
                    if g == 2:
                        P.copy(EV(), VB[:, tt, :], ps[:, :])
                        continue
                    qr = QR[qi[0] % 4]
                    qi[0] += 1
                    if tt < 2:
                        P.copy(EV(), qr[:], ps[:, :])
                    else:
                        v4 = ps[:, :].rearrange("p (g two d) -> p g two d", g=8, two=2)
                        o4 = qr[:].rearrange("p (g two d) -> p g two d", g=8, two=2)
                        x1, x2 = v4[:, :, 0, :], v4[:, :, 1, :]
                        cs = CB[:, tt - 2, :].unsqueeze(1).broadcast_to([128, 8, 32])
                        sn = SNB[:, tt - 2, :].unsqueeze(1).broadcast_to([128, 8, 32])
                        t1, t2, t3, t4 = [r[:] for r in RTM]
                        P.tt('dve', t1, x1, cs, ALU.mult)
                        P.tt('dve', t2, x2, sn, ALU.mult)
                        P.tt('pool', o4[:, :, 0, :], t1, t2, ALU.subtract)
                        P.tt('dve', t3, x1, sn, ALU.mult)
                        P.tt('dve', t4, x2, cs, ALU.mult)
                        P.tt('pool', o4[:, :, 1, :], t3, t4, ALU.add)
                    pb = PSB()
                    for h in range(4):
                        P.tr(pb[:, h * 128:(h + 1) * 128], qr[:, h * 128:(h + 1) * 128], IDB[:])
                    dst = QT if g == 0 else KT
                    P.copy(EV(), dst[:, :, tok], pb[:, 0:512].rearrange("p (a b) -> p a b", a=4))

            for tt in range(NT):
                proj_b(tt, [1, 2])
            if 'qt_tap' in taps:
                for nm, src in (('qt_tap', QT), ('kt_tap', KT)):
                    tp = dscr(nm, [128, 4, T], BF16)
                    for h in range(4):
                        P.dma('sp', tp[:, h, :], src[:, h, :])
            LT = sb('LT', [128, 128], F32, ph)
            LS = sb('LS', [128, 2], F32, ph)
            NL = sb('NL', [128, 1], F32, ph)
            SG = sb('SG', [128, 1], F32, ph)
            P.tt('dve', LT[:, 0:64], rvc('lam', 0, 64), rvc('lam', 64, 64), ALU.mult)
            P.tt('dve', LT[:, 64:128], rvc('lam', 128, 64), rvc('lam', 192, 64), ALU.mult)
            P.reduce('dve', LS[:, 0:2], LT[:].rearrange("p (a b) -> p a b", a=2), ALU.add)
            P.act(LS[:, 0:2], LS[:, 0:2], AF.Exp)
            P.tt('dve', NL[:], LS[:, 1:2], LS[:, 0:1], ALU.subtract)
            P.ts('dve', NL[:], NL[:], -LAMBDA_INIT0, None, ALU.add)
            P.ts('dve', SG[:], svc('subln_g'), 1.0 - LAMBDA_INIT0, None, ALU.mult)
            PT = [sb('PT%d' % i, [128, 512], BF16, ph) for i in range(3)]
            R1 = sb('R1', [128, 512], F32, ph)
            R2 = sb('R2', [128, 512], F32, ph)
            O1 = sb('O1', [128, 512], F32, ph)
            O2 = sb('O2', [128, 512], F32, ph)
            SQ = sb('SQ', [128, 512], F32, ph)
            YB = [sb('YB%d' % i, [128, 512], BF16, ph) for i in range(2)]
            ACC = [[sb('ACC%d%d' % (m_, j_), [128, 512], F32, ph) for j_ in range(2)] for m_ in range(2)]
            cnt = 0
            yi = 0
            def q_seg(si):
                a, b = SEGS[si]
                for tt in range(a // 128, b // 128):
                    proj_b(tt, [0])

            q_seg(0)
            q_seg(1)
            for si, (a, b) in enumerate(SEGS):
                if si + 2 < len(SEGS):
                    q_seg(si + 2)
                for h in range(4):
                    n = b - a
                    kts = list(range(2)) if a < TC else list(range(NT))
                    psO = [psf[0], psf[1]]
                    psD = [psf[2], psf[3]]
                    for m in range(2):
                        def s_mm(i):
                            kt = kts[i]
                            P.mm(psf[4 + (cnt + i) % 2][:, :n], KT[m * 64:(m + 1) * 64, h, kt * 128:(kt + 1) * 128],
                                 QT[m * 64:(m + 1) * 64, h, a:b])
                        s_mm(0)
                        for i, kt in enumerate(kts):
                            pS = psf[4 + (cnt + i) % 2]
                            pt = PT[(cnt + i) % 3]
                            if i + 1 < len(kts):
                                s_mm(i + 1)
                            P.act(pt[:, :n], pS[:, :n], AF.Exp, scale=0.125)
                            P.mm(psO[m][:, :n], VB[:, kt, h * 128:(h + 1) * 128], pt[:, :n],
                                 start=(i == 0), stop=(i == len(kts) - 1))
                            ai = 1 if i % 3 == 2 else 0
                            acc = ACC[m][ai]
                            aeng = 'pool' if ai else 'dve'
                            if i == 0 or i == 2:
                                P.copy(aeng, acc[:, :n], pt[:, :n])
                            else:
                                P.tt(aeng, acc[:, :n], acc[:, :n], pt[:, :n], ALU.add)
                        cnt += len(kts)
                        if len(kts) > 2:
                            P.mmf(psD[m][:, :n], ONESF[:], ACC[m][0][:, :n], start=True, stop=False)
                            P.mmf(psD[m][:, :n], ONESF[:], ACC[m][1][:, :n], start=False, stop=True)
                        else:
                            P.mmf(psD[m][:, :n], ONESF[:], ACC[m][0][:, :n], start=True, stop=True)
                    P.recip(R1[:, :n], psD[0][:, :n])
                    P.recip(R2[:, :n], psD[1][:, :n])
                    P.tt('dve', O1[:, :n], psO[0][:, :n], R1[:, :n], ALU.mult)
                    P.tt('dve', O2[:, :n], psO[1][:, :n], R2[:, :n], ALU.mult)
                    P.stt('dve', O1[:, :n], O2[:, :n], NL[:, 0:1], O1[:, :n], ALU.mult, ALU.add)
                    P.act(SQ[:, :n], O1[:, :n], AF.Square)
                    pq = psf[4 + cnt % 2]
                    cnt += 1
                    P.mmf(pq[:, :n], ONESF[:], SQ[:, :n])
                    P.act(R1[:, :n], pq[:, :n], AF.Sqrt, bias=1e-5, scale=1.0 / 128)
                    P.recip(R1[:, :n], R1[:, :n])
                    P.tt('dve', O1[:, :n], O1[:, :n], R1[:, :n], ALU.mult)
                    yb = YB[yi % 2]
                    yi += 1
                    P.ts('dve', yb[:, :n], O1[:, :n], SG[:, 0:1], None, ALU.mult)
                    P.dma('sp', ym_d[4 + h, :, a:b], yb[:, :n])
            P.barrier()
        if STOP == 4:
            es_ht.close()
            nc._declared_inputs = set(k for k in dram if k in SPEC)
            return nc


        es_ht.close()
        with ExitStack() as ph:
            C = 64
            NCK = T // C
            MSK = sb('MSK', [128, 4 * 128], F32, ph)
            P.dma('sp', MSK[:], W('masks')[:, 0:512])
            MK4 = [sb('MK4_%d' % i, [128, 512], F32, ph) for i in range(2)]
            for d, (m1, m2) in enumerate(((0, 1), (2, 3))):
                for q in range(4):
                    mm_ = m1 if q % 2 == 0 else m2
                    P.copy('dve', MK4[d][:, q * 128:(q + 1) * 128], MSK[:, mm_ * 128:(mm_ + 1) * 128])
            MKA = [MSK[:, 256:384], MSK[:, 0:128]]
            LW = sb('LW', [128, T], F32, ph)
            W2B = sb('W2B', [128, 512], BF16, ph)
            A2B = sb('A2B', [128, 512], BF16, ph)
            G2B = sb('G2B', [128, 512], BF16, ph)
            for dst, src in ((W2B, W('a_w2').rearrange("d l c -> (d l) c")), (A2B, W('a_a2').rearrange("d l c -> (d l) c")),
                             (G2B, W('a_g2'))):
                P.dma('sp', LW[:, 0:512], src)
                P.copy('dve', dst[:], LW[:, 0:512])
            OMKA = sb('OMKA', [128, 4], F32, ph)
            P.ts('dve', OMKA[:], svc('ka', 0, 4), -1.0, 1.0, ALU.mult, ALU.add)
            LORA = sb('LORA', [128, 3, T], BF16, ph)
            for i in range(3):
                P.dma('sp', LORA[:, i, :], ua_d[12 + i, :, :])
            RKV = sb('RKV', [128, 3, T], BF16, ph)
            KKt = sb('KKt', [128, T], BF16, ph)
            Ad = sb('Ad', [128, T], BF16, ph)
            LA = sb('LA', [128, T], F32, ph)
            LB = sb('LB', [128, T], F32, ph)
            PRs = [sb('PR%d' % i, [128, 6, T], BF16, ph) for i in range(2)]
            VBD = sb('VBD', [128, NCK, 128], BF16, ph)
            YACC = sb('YACC', [128, T], F32, ph)
            KDS = sb('KDS', [128, T], BF16, ph)
            LCts = [sb('LCt%d' % i, [128, NCK], F32, ph) for i in range(2)]
            GCs = [sb('GC%d' % i, [128, NCK], F32, ph) for i in range(2)]
            HFs = [sb('HF%d' % i, [128, 128], F32, ph) for i in range(2)]
            HBs = [sb('HB%d' % i, [128, 128], BF16, ph) for i in range(2)]
            TS = [sb('TSg%d' % i, [128, 512], F32, ph) for i in range(4)]
            G = int(os.environ.get('KG', '3'))
            NS = 2 * G
            XBD = [sb('XBD%d' % i, [128, 6, 128], BF16, ph) for i in range(NS)]
            W4 = [sb('W4_%d' % i, [128, 512], BF16, ph) for i in range(NS)]
            NAb = [sb('NAb%d' % i, [128, 256], BF16, ph) for i in range(2 * NS)]
            PQb = [sb('PQb%d' % i, [128, 256], BF16, ph) for i in range(2 * NS)]
            TOK = [sb('TOK%d' % i, [128, 3, 128], BF16, ph) for i in range(NS)]
            ZS = [sb('ZS%d' % i, [128, 128], BF16, ph) for i in range(NS)]
            US = [sb('US%d' % i, [128, 128], BF16, ph) for i in range(NS)]
            YOUT = [sb('YOUT%d' % i, [128, 512], BF16, ph) for i in range(2)]
            bdm3 = BDMB[:].rearrange("p (a b) -> p a b", a=2)
            CEXP = -math.exp(-0.5)
            v3 = lambda t_: t_[:].rearrange("p (c j) -> p c j", j=C)

            for hp in range(4):
                for i in range(3):
                    P.dma('sp', RKV[:, i, :], ua_d[4 * i + hp, :, :])
                r_, k_, v_ = RKV[:, 0, :], RKV[:, 1, :], RKV[:, 2, :]
                hc = slice(hp * 128, (hp + 1) * 128)
                for (a, b) in SEGS:
                    n = b - a
                    kx, sq, rn = TS[0], TS[1], TS[2]
                    P.ts('dve', kx[:, :n], k_[:, a:b], svc('kk', hp), None, ALU.mult)
                    P.act(sq[:, :n], kx[:, :n], AF.Square)
                    ps = PSF()
                    P.mmf(ps[:, :n], BDM[:], sq[:, :n])
                    P.act(rn[:, :n], ps[:, :n], AF.Sqrt, bias=1e-24)
                    P.recip(rn[:, :n], rn[:, :n])
                    P.tt('dve', KKt[:, a:b], kx[:, :n], rn[:, :n], ALU.mult)
                P.tt('pool', VBD[:].rearrange("p c (a b) -> p c a b", a=2),
                     v_.rearrange("p (c j) -> p c j", j=C).unsqueeze(2).broadcast_to([128, NCK, 2, C]),
                     bdm3.unsqueeze(1).broadcast_to([128, NCK, 2, C]), ALU.mult)
                P.memset('pool', YACC[:], 0.0)
                for d in range(2):
                    PR, LCt, GC = PRs[d], LCts[d], GCs[d]
                    dsl = slice(d * 64, (d + 1) * 64)
                    for (a, b) in SEGS:
                        n = b - a
                        ps = PSF()
                        P.mm(ps[:, :n], W2B[dsl, hc], LORA[dsl, 0, a:b])
                        P.act(TS[0][:, :n], ps[:, :n], AF.Sigmoid, bias=svc('w0_%d' % d, hp))
                        P.ts('pool', LW[:, a:b], TS[0][:, :n], CEXP, None, ALU.mult)
                        ps = PSF()
                        P.mm(ps[:, :n], A2B[dsl, hc], LORA[dsl, 1, a:b])
                        P.act(Ad[:, a:b], ps[:, :n], AF.Sigmoid, bias=svc('a0_%d' % d, hp))
                    seq = [(LW, LA), (LA, LB), (LB, LA), (LA, LB), (LB, LA), (LA, LB)]
                    for si, (src, dst) in enumerate(seq):
                        sft = 1 << si
                        s3, d3 = v3(src), v3(dst)
                        if d == 0:
                            P.tt('dve', d3[:, :, sft:], s3[:, :, sft:], s3[:, :, :C - sft], ALU.add)
                            P.copy('pool', d3[:, :, :sft], s3[:, :, :sft])
                        else:
                            P.tt('dve', d3[:, :, :C - sft], s3[:, :, :C - sft], s3[:, :, sft:], ALU.add)
                            P.copy('pool', d3[:, :, C - sft:], s3[:, :, C - sft:])
                    L3 = v3(LB)
                    P.tt('dve', LA[:], LB[:], LW[:], ALU.subtract)
                    P.copy('dve', LCt[:], L3[:, :, C - 1] if d == 0 else L3[:, :, 0])
                    P.act(GC[:], LCt[:], AF.Exp)
                    for (a, b) in SEGS:
                        n = b - a
                        c0, c1 = a // C, b // C
                        e, ba, kd, tq = TS[0], TS[1], TS[2], TS[3]
                        P.act(e[:, :n], LB[:, a:b], AF.Exp)
                        P.tt('dve', PR[:, 1, a:b], r_[:, a:b], e[:, :n], ALU.mult)
                        P.act(e[:, :n], LA[:, a:b], AF.Exp)
                        P.stt('dve', PR[:, 0, a:b], KKt[:, a:b], -1.0, e[:, :n], ALU.mult, ALU.mult)
                        P.tt('pool', ba[:, :n], KKt[:, a:b], Ad[:, a:b], ALU.mult)
                        P.ts('dve', tq[:, :n], Ad[:, a:b], svc('ka', hp), OMKA[:, hp:hp + 1], ALU.mult, ALU.add)
                        P.tt('dve', kd[:, :n], tq[:, :n], k_[:, a:b], ALU.mult)
                        if d == 0:
                            P.copy('pool', KDS[:, a:b], kd[:, :n])
                        else:
                            P.tt('pool', KDS[:, a:b], KDS[:, a:b], kd[:, :n], ALU.add)
                        P.act(e[:, :n], LB[:, a:b], AF.Exp, scale=-1.0)
                        P.tt('dve', PR[:, 2, a:b], ba[:, :n], e[:, :n], ALU.mult)
                        P.tt('pool', PR[:, 3, a:b], kd[:, :n], e[:, :n], ALU.mult)
                        P.tt('dve', tq[:, :n].rearrange("p (c j) -> p c j", j=C),
                             LCt[:, c0:c1].unsqueeze(2).broadcast_to([128, c1 - c0, C]),
                             LB[:, a:b].rearrange("p (c j) -> p c j", j=C), ALU.subtract)
                        P.act(e[:, :n], tq[:, :n], AF.Exp)
                        P.tt('dve', PR[:, 4, a:b], ba[:, :n], e[:, :n], ALU.mult)
                        P.tt('pool', PR[:, 5, a:b], kd[:, :n], e[:, :n], ALU.mult)
                    P.memset('dve', HFs[d][:], 0.0)
                    P.memset('pool', HBs[d][:], 0.0)

                seq_pos = [0, 0]

                freef = list(psf)
                freeb = list(psb)

                def unit(d, pos, c):
                    bi = d * G + pos % G
                    PR, GC, HF, HB = PRs[d], GCs[d], HFs[d], HBs[d]
                    cs = slice(c * C, (c + 1) * C)
                    xbd, w4, tok, zs, us = XBD[bi], W4[bi], TOK[bi], ZS[bi], US[bi]
                    P.tt('dve' if d == 0 else 'pool', xbd[:].rearrange("p s (a b) -> p s a b", a=2),
                         PR[:, :, cs].unsqueeze(2).broadcast_to([128, 6, 2, C]),
                         bdm3.unsqueeze(1).broadcast_to([128, 6, 2, C]), ALU.mult)
                    yield
                    AtBD, RtBD, BtBD, KtBD, BhBD, KhBD = [xbd[:, i, :] for i in range(6)]
                    AR = xbd[:, 0:2, :].rearrange("p s f -> p (s f)")
                    while len(freef) < 2 or len(freeb) < 1:
                        yield
                    ps1, ps2, pb = freef.pop(0), freef.pop(0), freeb.pop(0)
                    P.mm(ps1[:, 0:256], BtBD, AR)
                    P.mm(ps1[:, 256:512], KtBD, AR)
                    P.mm(ps2[:, 0:128], AtBD, BtBD)
                    P.tr(pb[:, 0:128], VBD[:, c, :], IDB[:])
                    P.tr(pb[:, 128:256], BhBD, IDB[:])
                    P.tr(pb[:, 256:384], KhBD, IDB[:])
                    yield
                    na = NAb[2 * bi]
                    pq = PQb[2 * bi]
                    P.tt('dve', w4[:], ps1[:, :], MK4[d][:], ALU.mult)
                    P.tt('dve', na[:, 128:256], ps2[:, 0:128], MKA[d], ALU.mult)
                    P.copy('act', tok[:].rearrange("p s f -> p (s f)"), pb[:, 0:384])
                    freef.extend([ps1, ps2])
                    freeb.append(pb)
                    P.copy('act', na[:, 0:128], w4[:, 0:128])
                    P.tt('pool', pq[:].rearrange("p (a b) -> p a b", a=2), na[:].rearrange("p (a b) -> p a b", a=2),
                         IDB[:].unsqueeze(1).broadcast_to([128, 2, 128]), ALU.add)
                    for lv in range(5):
                        na2 = NAb[2 * bi + (lv + 1) % 2]
                        pq2 = PQb[2 * bi + (lv + 1) % 2]
                        while len(freef) < 1:
                            yield
                        psn = freef.pop(0)
                        P.mm(psn[:, 0:128], na[:, 128:256], na[:, 0:128])
                        P.mm(psn[:, 128:256], na[:, 0:128], na[:, 128:256])
                        yield
                        P.copy('act', na2[:], psn[:, 0:256])
                        freef.append(psn)
                        while len(freef) < 1:
                            yield
                        psp = freef.pop(0)
                        P.mm(psp[:, 0:128], pq[:, 128:256], na2[:, 0:128])
                        P.mm(psp[:, 128:256], na2[:, 0:128], pq[:, 128:256])
                        yield
                        P.tt('dve', pq2[:], psp[:, 0:256], pq[:], ALU.add)
                        freef.append(psp)
                        na, pq = na2, pq2
                    VtBD, BhT, KhT = tok[:, 0, :], tok[:, 1, :], tok[:, 2, :]
                    while seq_pos[d] != pos or len(freef) < 1:
                        yield
                    psz = freef.pop(0)
                    P.mm(psz[:, 0:128], AtBD, HB[:], start=True, stop=False)
                    P.mm(psz[:, 0:128], w4[:, 256:384], VtBD, start=False, stop=True)
                    yield
                    P.copy('act', zs[:], psz[:, 0:128])
                    freef.append(psz)
                    while len(freef) < 1:
                        yield
                    psu = freef.pop(0)
                    P.mm(psu[:, 0:128], pq[:, 0:128], zs[:])
                    yield
                    P.copy('act', us[:], psu[:, 0:128])
                    freef.append(psu)
                    while len(freef) < 2:
                        yield
                    psh, psy = freef.pop(0), freef.pop(0)
                    P.mm(psh[:, 0:128], BhT, us[:], start=True, stop=False)
                    P.mm(psh[:, 0:128], KhT, VtBD, start=False, stop=True)
                    P.mm(psy[:, 0:128], HB[:], RtBD, start=True, stop=False)
                    P.mm(psy[:, 0:128], us[:], w4[:, 128:256], start=False, stop=False)
                    P.mm(psy[:, 0:128], VtBD, w4[:, 384:512], start=False, stop=True)
                    yield
                    P.stt('dve', HF[:], HF[:], GC[:, c:c + 1], psh[:, 0:128], ALU.mult, ALU.add)
                    P.copy('act', HB[:], HF[:])
                    for hh in range(2):
                        rs = slice(hh * 64, (hh + 1) * 64)
                        P.tt('dve', YACC[rs, cs], YACC[rs, cs], psy[rs, hh * 64:(hh + 1) * 64], ALU.add)
                    freef.extend([psh, psy])
                    seq_pos[d] += 1

                orders = [list(range(NCK)), [3, 2, 1, 0] + list(range(NCK - 1, 3, -1))]
                nxt = [0, 0]
                active = []
                while active or nxt[0] < NCK or nxt[1] < NCK:
                    for d in range(2):
                        while sum(1 for (dd, _) in active if dd == d) < G and nxt[d] < NCK:
                            active.append((d, unit(d, nxt[d], orders[d][nxt[d]])))
                            nxt[d] += 1
                    for item in list(active):
                        try:
                            next(item[1])
                        except StopIteration:
                            active.remove(item)
                for (a, b) in SEGS:
                    n = b - a
                    psg = PSF()
                    P.mm(psg[:, :n], G2B[:, hc], LORA[:, 2, a:b])
                    psm = PSF()
                    P.mmf(psm[:, :n], BDM[:], YACC[:, a:b])
                    sq, mu, t3, pr = TS[0], TS[1], TS[2], TS[3]
                    P.act(sq[:, :n], YACC[:, a:b], AF.Square)
                    psq = PSF()
                    P.mmf(psq[:, :n], BDM[:], sq[:, :n])
                    P.ts('dve', mu[:, :n], psm[:, :n], 1.0 / 64, None, ALU.mult)
                    P.tt('dve', t3[:, :n], mu[:, :n], mu[:, :n], ALU.mult)
                    P.stt('dve', t3[:, :n], psq[:, :n], 1.0 / 64, t3[:, :n], ALU.mult, ALU.subtract)
                    P.act(t3[:, :n], t3[:, :n], AF.Sqrt, bias=64e-5)
                    P.recip(t3[:, :n], t3[:, :n])
                    P.tt('dve', sq[:, :n], YACC[:, a:b], mu[:, :n], ALU.subtract)
                    P.tt('dve', sq[:, :n], sq[:, :n], t3[:, :n], ALU.mult)
                    P.ts('dve', sq[:, :n], sq[:, :n], svc('lnx_g', hp), svc('lnx_b', hp), ALU.mult, ALU.add)
                    P.stt('dve', pr[:, :n], r_[:, a:b], svc('rk', hp), KDS[:, a:b], ALU.mult, ALU.mult)
                    psb_ = PSF()
                    P.mmf(psb_[:, :n], BDM[:], pr[:, :n])
                    P.tt('dve', mu[:, :n], psb_[:, :n], v_[:, a:b], ALU.mult)
                    P.tt('dve', sq[:, :n], sq[:, :n], mu[:, :n], ALU.add)
                    yo = YOUT[(a // 512) % 2]
                    P.tt('dve', yo[:, :n], sq[:, :n], psg[:, :n], ALU.mult)
                    P.dma('sp', ym_d[hp, :, a:b], yo[:, :n])
            P.barrier()
        if STOP == 5:
            nc._declared_inputs = set(k for k in dram if k in SPEC)
            return nc


        def load_w2(dst, src, rows_kc, ncols, stg):
            i = 0
            for kc in range(rows_kc):
                for c0 in range(0, ncols, 512):
                    st = stg[i % 2]
                    i += 1
                    P.dma('sp', st[:, 0:512], src[kc * 128:(kc + 1) * 128, c0:c0 + 512])
                    P.copy(CV(), dst[:, kc, c0:c0 + 512], st[:, 0:512])

        def layer_norm(XT, a, b, gname, bname, tmp):
            n = b - a
            SQa, SQb, MU, RS = tmp
            psm = PSF()
            for c in range(8):
                P.mmf(psm[:, :n], ONESF[:], XT[:, c, a:b], start=(c == 0), stop=(c == 7))
            psq = PSF()
            for c in range(8):
                sq = SQa if c % 2 == 0 else SQb
                P.act(sq[:, :n], XT[:, c, a:b], AF.Square)
                P.mmf(psq[:, :n], ONESF[:], sq[:, :n], start=(c == 0), stop=(c == 7))
            P.ts('dve', MU[:, :n], psm[:, :n], 1.0 / D, None, ALU.mult)
            P.tt('dve', RS[:, :n], MU[:, :n], MU[:, :n], ALU.mult)
            P.stt('dve', RS[:, :n], psq[:, :n], 1.0 / D, RS[:, :n], ALU.mult, ALU.subtract)
            P.act(RS[:, :n], RS[:, :n], AF.Sqrt, bias=LN_EPS / (ALPHA * ALPHA))
            P.recip(RS[:, :n], RS[:, :n])
            for c in range(8):
                eng = 'dve' if c % 2 == 0 else 'pool'
                P.tt(eng, XT[:, c, a:b], XT[:, c, a:b], MU[:, :n], ALU.subtract)
                P.tt(eng, XT[:, c, a:b], XT[:, c, a:b], RS[:, :n], ALU.mult)
                P.ts(eng, XT[:, c, a:b], XT[:, c, a:b], svc(gname, c), svc(bname, c), ALU.mult, ALU.add)

        def phase_D(layer, w_out_name, segs, tiles, last):
            L = layer
            with ExitStack() as ph:
                XT = sb('XTd%d' % L, [128, NCH, T], F32, ph)
                for c in range(8):
                    P.dma('sp', XT[:, c, :], xt_d[:, c, :])
                HT2 = sb('HT2_%d' % L, [128, NCH, T], BF16, ph)
                LNT = [sb('LNT%d_%d' % (L, i), [128, 512], F32, ph) for i in range(4)]
                with ExitStack() as p1:
                    WO = sb('WO%d' % L, [128, 8, 1024], BF16, p1)
                    STG = [sb('STGo%d_%d' % (L, i), [128, 512], F32, p1) for i in range(2)]
                    load_w2(WO, W(w_out_name), 8, 1024, STG)
                    YM = [sb('YM%d_%d' % (L, i), [128, 8, 512], BF16, p1) for i in range(2)]
                    for si, (a, b) in enumerate(segs):
                        n = b - a
                        w = 1 if a < TC else 0
                        ym = YM[si % 2]
                        for kc in range(8):
                            P.dma('sp', ym[:, kc, :n], ym_d[kc, :, a:b])
                        for c in range(8):
                            ps = PSF()
                            for kc in range(8):
                                P.mm(ps[:, :n], WO[:, kc, c * 128:(c + 1) * 128], ym[:, kc, :n], start=(kc == 0), stop=(kc == 7))
                            P.stt('dve', XT[:, c, a:b], ps[:, :n], modc(L, 2, c, w), XT[:, c, a:b], ALU.mult, ALU.add)
                        layer_norm(XT, a, b, 'ln1_g%d' % L, 'ln1_b%d' % L, LNT)
                        for c in range(8):
                            P.ts('pool' if c % 2 else 'dve', HT2[:, c, a:b], XT[:, c, a:b], modc(L, 4, c, w), modc(L, 3, c, w),
                                 ALU.mult, ALU.add)
                    P.barrier()
                if ('xln1_tap%d' % L) in taps:
                    tp = dscr('xln1_tap%d' % L, [128, NCH, T])
                    for c in range(8):
                        P.dma('sp', tp[:, c, :], XT[:, c, :])
                if STOP == 61:
                    P.barrier(); return
                GTb = sb('GTb%d' % L, [16, T], BF16, ph)
                with ExitStack() as p2:
                    RW = sb('RW%d' % L, [128, 8, 16], F32, p2)
                    P.dma('sp', RW[:], W('router_w').rearrange("(c p) e -> p c e", p=128))
                    RWS = [sb('RWS%d_%d' % (L, i), [128, 8, 16], F32, p2) for i in range(2)]
                    SHB = [sb('SHB%d_%d' % (L, i), [128, 8, 128], F32, p2) for i in range(2)]
                    for w in range(2):
                        for c in range(8):
                            P.ts('dve', RWS[w][:, c, :], RW[:, c, :], modc(L, 4, c, w), None, ALU.mult)
                            P.ts('pool', SHB[w][:, c, :], ONESF[:], modc(L, 3, c, w), None, ALU.mult)
                    RT_ = [sb('RTR%d_%d' % (L, i), [128, 16], F32, p2) for i in range(8)]
                    RS_ = [sb('RSM%d_%d' % (L, i), [128, 4], F32, p2) for i in range(6)]
                    GTf = sb('GTf%d' % L, [16, 128], F32, p2)
                    for tt in tiles:
                        w = 1 if tt < 2 else 0
                        tok = slice(tt * 128, (tt + 1) * 128)
                        ps = PSF()
                        for c in range(8):
                            P.mmf(ps[:, 0:16], XT[:, c, tok], RWS[w][:, c, :], start=(c == 0), stop=False)
                        for c in range(8):
                            P.mmf(ps[:, 0:16], SHB[w][:, c, :], RW[:, c, :], start=False, stop=(c == 7))
                        LG, E, EQ, EM, SEL, GATE = [t_[:] for t_ in RT_[:6]]
                        M1, M2, GS, ING, MX, RG = [t_[:] for t_ in RS_]
                        v3 = lambda x: x.rearrange("p (g e) -> p g e", g=4)
                        P.tt('dve', LG, ps[:, 0:16], rvc('router_b', 0, 16), ALU.add)
                        P.reduce('dve', MX[:, 0:1], LG, ALU.max)
                        P.ts('dve', MX[:, 1:2], MX[:, 0:1], -1.0, None, ALU.mult)
                        P.act(E, LG, AF.Exp, bias=MX[:, 1:2])
                        P.reduce('dve', M1, v3(E), ALU.max)
                        P.tt('dve', v3(EQ), v3(E), M1.unsqueeze(2).broadcast_to([128, 4, 4]), ALU.is_equal)
                        P.tt('dve', EQ, EQ, E, ALU.mult)
                        P.tt('dve', EM, E, EQ, ALU.subtract)
                        P.reduce('dve', M2, v3(EM), ALU.max)
                        P.tt('dve', GS, M1, M2, ALU.add)
                        P.reduce('dve', MX[:, 2:3], GS, ALU.max)
                        P.ts('dve', ING, GS, MX[:, 2:3], None, ALU.is_equal)
                        P.tt('dve', v3(SEL), v3(E), M2.unsqueeze(2).broadcast_to([128, 4, 4]), ALU.is_ge)
                        P.tt('dve', v3(SEL), v3(SEL), ING.unsqueeze(2).broadcast_to([128, 4, 4]), ALU.mult)
                        P.recip(RG[:, 0:1], MX[:, 2:3])
                        P.stt('dve', GATE, E, RG[:, 0:1], SEL, ALU.mult, ALU.mult)
                        pt_ = PSF()
                        P.tr(pt_[0:16, 0:128], GATE, IDF[:])
                        P.copy('act', GTb[:, tok], pt_[0:16, 0:128])
                    P.barrier()
                if ('gate_tap%d' % L) in taps:
                    tp = dscr('gate_tap%d' % L, [16, T], BF16)
                    P.dma('sp', tp[:, :], GTb[:])
                if STOP == 62:
                    P.barrier(); return
                with ExitStack() as p3:
                    SELM = sb('SELM%d' % L, [16, 16, 128], BF16, p3)
                    for e in range(16):
                        P.ts('dve', SELM[:, e, :], ONESF[0:16, :], IDF[0:16, e:e + 1], None, ALU.mult)
                    W1s = [sb('W1_%d_%d' % (L, i), [128, 8, 512], BF16, p3) for i in range(2)]
                    W3s = [sb('W3_%d_%d' % (L, i), [128, 8, 512], BF16, p3) for i in range(2)]
                    W2s = [sb('W2_%d_%d' % (L, i), [128, 4, 1024], BF16, p3) for i in range(2)]
                    STG = [sb('STGe%d_%d' % (L, i), [128, 512], F32, p3) for i in range(2)]
                    HID = [sb('HID%d_%d' % (L, i), [128, 4, 512], BF16, p3) for i in range(2)]
                    GB = [sb('GB%d_%d' % (L, i), [128, 512], F32, p3) for i in range(2)]
                    S1 = [sb('S1_%d_%d' % (L, i), [128, 512], F32, p3) for i in range(2)]
                    print('MoE phase sbuf remaining', nc.sbuf_bytes_remaining)
                    NE = int(os.environ.get('KEXP', '16'))
                    items = [(e, si, a, b) for e in range(NE) for si, (a, b) in enumerate(segs)]

                    def up(k):
                        e, si, a, b = items[k]
                        n = b - a
                        W1, W3, W2 = W1s[e % 2], W3s[e % 2], W2s[e % 2]
                        if si == 0:
                            load_w2(W1, W('moe_w1')[L, e], 8, 512, STG)
                            load_w2(W3, W('moe_w3')[L, e], 8, 512, STG)
                            load_w2(W2, W('moe_w2')[L, e], 4, 1024, STG)
                        gb, hid = GB[k % 2], HID[k % 2]
                        psg = PSF()
                        P.mm(psg[:, :n], SELM[:, e, :], GTb[:, a:b])
                        P.copy('act', gb[:, :n], psg[:, :n])
                        for fc in range(4):
                            ps1 = PSF()
                            for kc in range(8):
                                P.mm(ps1[:, :n], W1[:, kc, fc * 128:(fc + 1) * 128], HT2[:, kc, a:b], start=(kc == 0), stop=(kc == 7))
                            ps3 = PSF()
                            for kc in range(8):
                                P.mm(ps3[:, :n], W3[:, kc, fc * 128:(fc + 1) * 128], HT2[:, kc, a:b], start=(kc == 0), stop=(kc == 7))
                            s1 = S1[fc % 2]
                            P.act(s1[:, :n], ps1[:, :n], AF.Silu)
                            P.tt('dve', s1[:, :n], s1[:, :n], ps3[:, :n], ALU.mult)
                            P.tt('pool', hid[:, fc, :n], s1[:, :n], gb[:, :n], ALU.mult)

                    def down(k):
                        e, si, a, b = items[k]
                        n = b - a
                        w = 1 if a < TC else 0
                        W2 = W2s[e % 2]
                        hid = HID[k % 2]
                        for c in range(8):
                            ps = PSF()
                            for fc in range(4):
                                P.mm(ps[:, :n], W2[:, fc, c * 128:(c + 1) * 128], hid[:, fc, :n], start=(fc == 0), stop=(fc == 3))
                            P.stt('dve', XT[:, c, a:b], ps[:, :n], modc(L, 5, c, w), XT[:, c, a:b], ALU.mult, ALU.add)

                    up(0)
                    for k in range(len(items)):
                        if k + 1 < len(items):
                            up(k + 1)
                        down(k)
                    P.barrier()
                for (a, b) in segs:
                    layer_norm(XT, a, b, 'ln2_g%d' % L, 'ln2_b%d' % L, LNT)
                if not last:
                    for c in range(8):
                        P.dma('sp', xt_d[:, c, :], XT[:, c, :])
                else:
                    OS = [sb('OS%d' % i, [128, D], F32, ph) for i in range(2)]
                    for tt in range(2, NT):
                        os_ = OS[tt % 2]
                        tok = slice(tt * 128, (tt + 1) * 128)
                        for hh in range(2):
                            ps = PSF()
                            for j in range(4):
                                P.tr(ps[:, j * 128:(j + 1) * 128], XT[:, hh * 4 + j, tok], IDF[:])
                            P.copy(EV(), os_[:, hh * 512:(hh + 1) * 512], ps[:, :])
                        P.dma('sp', out_d[(tt - 2) * 128:(tt - 1) * 128, :], os_[:])
                P.barrier()

        phase_D(0, 'ev_w_out', SEGS, list(range(NT)), False)
        if STOP in (6, 61, 62):
            nc._declared_inputs = set(k for k in dram if k in SPEC)
            return nc


        with ExitStack() as ph:
            HT1 = sb('HT1', [128, NCH, T], BF16, ph)
            XC = [sb('XC%d' % i, [128, T], F32, ph) for i in range(2)]
            for c in range(8):
                xc = XC[c % 2]
                P.dma('sp', xc[:], xt_d[:, c, :])
                P.ts('dve', HT1[:, c, 0:TC], xc[:, 0:TC], modc(1, 1, c, 1), modc(1, 0, c, 1), ALU.mult, ALU.add)
                P.ts('pool', HT1[:, c, TC:T], xc[:, TC:T], modc(1, 1, c, 0), modc(1, 0, c, 0), ALU.mult, ALU.add)
            WQ = sb('WQ', [128, 8, 1536], BF16, ph)
            STG = [sb('STGq%d' % i, [128, 512], F32, ph) for i in range(2)]
            load_w2(WQ, W('od_w_in'), 8, 1536, STG)
            CC = sb('CC', [128, 16, 64], F32, ph)
            SC = sb('SC', [128, 16, 64], F32, ph)
            P.dma('sp', CC[:], W('cosc').rearrange("(n p) f -> p n f", p=128))
            P.dma('sp', SC[:], W('sinc').rearrange("(n p) f -> p n f", p=128))
            QT1 = sb('QT1', [128, 8, TL], BF16, ph)
            KT1 = sb('KT1', [128, 2, T], BF16, ph)
            V1 = sb('V1', [128, NT, 256], BF16, ph)
            XQ = sb('XQ', [128, 512], F32, ph)
            TQ = sb('TQ', [128, 512], F32, ph)
            RTQ = [sb('RTQ%d' % i, [128, 4, 64], F32, ph) for i in range(4)]
            SSQ = sb('SSQ', [128, 4], F32, ph)
            QN = [sb('QN%d' % i, [128, 512], BF16, ph) for i in range(3)]
            qn_i = [0]

            def normrope(psv, H, gname, rope, tile_i):
                n = H * 128
                out = QN[qn_i[0] % 3]
                qn_i[0] += 1
                x3 = XQ[:, :n].rearrange("p (h d) -> p h d", h=H)
                t3 = TQ[:, :n].rearrange("p (h d) -> p h d", h=H)
                o3 = out[:, :n].rearrange("p (h d) -> p h d", h=H)
                P.copy('act', XQ[:, :n], psv)
                P.tt('dve', TQ[:, :n], XQ[:, :n], XQ[:, :n], ALU.mult)
                P.reduce('dve', SSQ[:, :H], t3, ALU.add)
                P.act(SSQ[:, :H], SSQ[:, :H], AF.Sqrt, bias=1e-6, scale=1.0 / 128)
                P.recip(SSQ[:, :H], SSQ[:, :H])
                P.tt('dve', t3, x3, SSQ[:, :H].unsqueeze(2).broadcast_to([128, H, 128]), ALU.mult)
                gb = rvc(gname, 0, 128).unsqueeze(1).broadcast_to([128, H, 128])
                if not rope:
                    P.tt('dve', o3, t3, gb, ALU.mult)
                    return out
                P.tt('pool', x3, t3, gb, ALU.mult)
                x1, x2 = x3[:, :, 0:64], x3[:, :, 64:128]
                cs = CC[:, tile_i, :].unsqueeze(1).broadcast_to([128, H, 64])
                sn = SC[:, tile_i, :].unsqueeze(1).broadcast_to([128, H, 64])
                t1, t2, t3_, t4 = [r[:, :H, :] for r in RTQ]
                P.tt('dve', t1, x1, cs, ALU.mult)
                P.tt('pool', t2, x2, sn, ALU.mult)
                P.tt('dve', o3[:, :, 0:64], t1, t2, ALU.subtract)
                P.tt('pool', t3_, x1, sn, ALU.mult)
                P.tt('dve', t4, x2, cs, ALU.mult)
                P.tt('pool', o3[:, :, 64:128], t3_, t4, ALU.add)
                return out

            for tt in range(NT):
                tok = slice(tt * 128, (tt + 1) * 128)
                lat = tt >= 2
                groups = [0, 1, 2] if lat else [2]
                for g in groups:
                    ps = PSF()
                    for kc in range(8):
                        P.mm(ps[:, :], HT1[:, kc, tok], WQ[:, kc, g * 512:(g + 1) * 512], start=(kc == 0), stop=(kc == 7))
                    if g < 2:
                        qn = normrope(ps[:, 0:512], 4, 'qn_g', True, tt - 2)
                        pb = PSB()
                        for h in range(4):
                            P.tr(pb[:, h * 128:(h + 1) * 128], qn[:, h * 128:(h + 1) * 128], IDB[:])
                        P.copy(EV(), QT1[:, g * 4:(g + 1) * 4, (tt - 2) * 128:(tt - 1) * 128],
                               pb[:, 0:512].rearrange("p (a b) -> p a b", a=4))
                    else:
                        P.copy('act', V1[:, tt, :], ps[:, 256:512])
                        kn = normrope(ps[:, 0:256], 2, 'kn_g', lat, tt - 2)
                        pb = PSB()
                        for h in range(2):
                            P.tr(pb[:, h * 128:(h + 1) * 128], kn[:, h * 128:(h + 1) * 128], IDB[:])
                        P.copy(EV(), KT1[:, :, tok], pb[:, 0:256].rearrange("p (a b) -> p a b", a=2))
            PT1 = [sb('PT1_%d' % i, [128, 512], BF16, ph) for i in range(3)]
            RR = sb('RR', [128, 512], F32, ph)
            ACC1 = [sb('ACC1_%d' % j_, [128, 512], F32, ph) for j_ in range(2)]
            OB = [sb('OB%d' % i, [128, 512], BF16, ph) for i in range(2)]
            cnt = 0
            SCL = 128 ** -0.5
            for kvh in range(2):
                for g4 in range(4):
                    head = kvh * 4 + g4
                    for qs in range(4):
                        a, b = qs * 512, (qs + 1) * 512
                        psO, psD = psf[0], psf[1]
                        def s_mm1(kt):
                            P.mm(psf[2 + (cnt + kt) % 4][:, :], KT1[:, kvh, kt * 128:(kt + 1) * 128], QT1[:, head, a:b])
                        s_mm1(0)
                        s_mm1(1)
                        for kt in range(NT):
                            pS = psf[2 + (cnt + kt) % 4]
                            pt = PT1[(cnt + kt) % 3]
                            if kt + 2 < NT:
                                s_mm1(kt + 2)
                            P.act(pt[:], pS[:, :], AF.Exp, scale=SCL)
                            P.mm(psO[:, :], V1[:, kt, kvh * 128:(kvh + 1) * 128], pt[:], start=(kt == 0), stop=(kt == NT - 1))
                            ai = 1 if kt % 3 == 2 else 0
                            acc = ACC1[ai]
                            aeng = 'pool' if ai else 'dve'
                            if kt == 0 or kt == 2:
                                P.copy(aeng, acc[:], pt[:])
                            else:
                                P.tt(aeng, acc[:], acc[:], pt[:], ALU.add)
                        cnt += NT
                        P.mmf(psD[:, :], ONESF[:], ACC1[0][:], start=True, stop=False)
                        P.mmf(psD[:, :], ONESF[:], ACC1[1][:], start=False, stop=True)
                        P.recip(RR[:], psD[:, :])
                        ob = OB[(head * 4 + qs) % 2]
                        P.tt('dve', ob[:], psO[:, :], RR[:], ALU.mult)
                        P.dma('sp', ym_d[head, :, TC + a:TC + b], ob[:])
            P.barrier()
        if STOP == 7:
            nc._declared_inputs = set(k for k in dram if k in SPEC)
            return nc
        phase_D(1, 'od_w_out', SEGS[1:], list(range(2, NT)), True)

        P.barrier()
        print("ops", P.nops)
    nc._declared_inputs = set(k for k in dram if k in SPEC)
    return nc


_CACHE = {}


def kernel(**inp):
    inp = {k: np.asarray(v) for k, v in inp.items()}
    taps = tuple(inp.pop('_taps', ()))
    ncores = int(inp.pop('_ncores', 8))
    cosb, sinb = _rope_tables(64)
    cosc, sinc = _rope_tables(128)
    ident = np.eye(128, dtype=np.float32)
    bdmask = np.zeros((128, 128), np.float32)
    bdmask[:64, :64] = 1.0
    bdmask[64:, 64:] = 1.0
    tri = np.zeros((4, 128, 128), np.float32)
    for blk in range(2):
        o = blk * 64
        ii, jj = np.meshgrid(np.arange(64), np.arange(64), indexing='ij')
        tri[0, o:o + 64, o:o + 64] = (ii < jj)
        tri[1, o:o + 64, o:o + 64] = (ii <= jj)
        tri[2, o:o + 64, o:o + 64] = (ii > jj)
        tri[3, o:o + 64, o:o + 64] = (ii >= jj)
    masks = np.ascontiguousarray(np.concatenate([tri[0], tri[1], tri[2], tri[3], tri[0], tri[0]], axis=1))
    shared = {
        'ident': ident, 'bdmask': bdmask, 'masks': masks,
        'ada_w': np.ascontiguousarray(inp['ada_w'], np.float32),
        'ev_w_in': np.ascontiguousarray(inp['ev_w_in'][0]), 'ev_w_out': np.ascontiguousarray(inp['ev_w_out'][0]),
        'od_w_in': np.ascontiguousarray(inp['od_w_in'][0]), 'od_w_out': np.ascontiguousarray(inp['od_w_out'][0]),
        'moe_w1': inp['moe_w1'], 'moe_w3': inp['moe_w3'], 'moe_w2': inp['moe_w2'],
        'router_w': inp['router_w'],
        'a_w2': np.ascontiguousarray(inp['ev_a_w2'][0]), 'a_a2': np.ascontiguousarray(inp['ev_a_a2'][0]),
        'a_g2': np.ascontiguousarray(inp['ev_a_g2'][0]),
        'cosb': cosb, 'sinb': sinb, 'cosc': cosc, 'sinc': sinc,
    }
    in_maps = []
    for b in range(ncores):
        sv, rv = _pack_small(inp, b)
        m = dict(shared)
        m['xin'] = np.ascontiguousarray(np.concatenate([inp['ctx'][b], inp['x'][b]], axis=0), np.float32)
        m['sv'] = sv
        m['rv'] = rv
        in_maps.append(m)
    nc = build_program(SV_LAYOUT['_n'], RV_LAYOUT['_n'], taps)
    used = nc._declared_inputs
    in_maps = [{k: v for k, v in m.items() if k in used} for m in in_maps]
    res = run_bass_kernel_spmd(nc, in_maps, core_ids=list(range(ncores)))
    if taps:
        return res
    return np.stack([np.asarray(r['out'], np.float32) for r in res.results], axis=0)
```

```python
import math
from contextlib import ExitStack
import numpy as np
import concourse.bass as bass
import concourse.mybir as mybir
from concourse.bass_utils import run_bass_kernel_spmd

F32 = mybir.dt.float32
BF16 = mybir.dt.bfloat16
AF = mybir.ActivationFunctionType
ALU = mybir.AluOpType
AX = mybir.AxisListType

D = 1024
NCH = 8
TC = 256
TL = 2048
T = TC + TL
NT = T // 128
ALPHA = 4 ** 0.25
LN_EPS = 1e-5
import os
STOP = int(os.environ.get('KSTOP', '0'))
SEGS = [(0, 256)] + [(256 + 512 * i, 256 + 512 * (i + 1)) for i in range(4)]


class Prog:
    def __init__(self, nc, es):
        self.nc = nc
        self.E = {'pe': nc.tensor, 'act': nc.scalar, 'dve': nc.vector, 'pool': nc.gpsimd, 'sp': nc.sync}
        self.NR = 8
        self.sem = {}
        for e in ('pe', 'act', 'dve', 'pool'):
            self.sem['c_' + e] = es.enter_context(nc.semaphore('c_' + e))
        for q in ('sp', 'pool'):
            for i in range(self.NR):
                k = 'd_%s_%d' % (q, i)
                self.sem[k] = es.enter_context(nc.semaphore(k))
        self.val = {k: 0 for k in self.sem}
        self.dn = {'sp': 0, 'pool': 0}
        self.seen = {e: {} for e in self.E}
        self.recs = {}
        self.ro = set()
        self.nops = 0

    @staticmethod
    def box(ap):
        name = ap.tensor.name
        dims = ap.ap
        off = ap.offset
        if 'DRAM' in str(ap.space).upper():
            return name, 0, 1, off, off + sum(s * (c - 1) for s, c in dims) + 1
        if 'PSUM' in str(ap.space).upper():
            return name, 0, 128, 0, 1 << 30
        pst, pn = dims[0]
        if pst <= 0:
            p0, f0 = 0, off
        else:
            p0, f0 = off // pst, off % pst
        f1 = f0 + sum(s * (c - 1) for s, c in dims[1:]) + 1
        return name, p0, p0 + pn, f0, f1

    def _wait(self, eng, key, v):
        if self.seen[eng].get(key, 0) >= v:
            return
        self.E[eng].wait_ge(self.sem[key], v)
        self.seen[eng][key] = v

    def op(self, eng, emit, reads=(), writes=(), dma=False):
        deps = {}

        def need(r):
            (_, _, _, _, w, key, v, reng) = r
            if not dma and reng == eng:
                if eng == 'pe':
                    return
            if deps.get(key, 0) < v:
                deps[key] = v

        rb = []
        wb = []
        for ap in reads:
            b = self.box(ap)
            if b[0] in self.ro:
                continue
            rb.append(b)
            for r in self.recs.get(b[0], ()):
                if r[4] and r[0] < b[2] and b[1] < r[1] and r[2] < b[4] and b[3] < r[3]:
                    need(r)
        for ap in writes:
            b = self.box(ap)
            wb.append(b)
            for r in self.recs.get(b[0], ()):
                if r[0] < b[2] and b[1] < r[1] and r[2] < b[4] and b[3] < r[3]:
                    need(r)
        if dma:
            slot = self.dn[eng] % self.NR
            use = self.dn[eng] // self.NR
            self.dn[eng] += 1
            key = 'd_%s_%d' % (eng, slot)
            if use > 0 and deps.get(key, 0) < 16 * use:
                deps[key] = 16 * use
            val = 16 * (use + 1)
            inc = 16
            reng = 'dma'
        else:
            key = 'c_' + eng
            val = self.val[key] + 1
            inc = 1
            reng = eng
        for k, v in deps.items():
            self._wait(eng, k, v)
        ins = emit(self.E[eng])
        ins.then_inc(self.sem[key], inc)
        self.val[key] = val
        self.nops += 1
        for b in wb:
            lst = self.recs.setdefault(b[0], [])
            lst[:] = [r for r in lst if not (b[1] <= r[0] and r[1] <= b[2] and b[3] <= r[2] and r[3] <= b[4])]
            lst.append((b[1], b[2], b[3], b[4], True, key, val, reng))
        for b in rb:
            lst = self.recs.setdefault(b[0], [])
            lst[:] = [r for r in lst if not ((not r[4]) and r[7] == reng and reng != 'dma'
                                             and r[0] == b[1] and r[1] == b[2] and r[2] == b[3] and r[3] == b[4])]
            lst.append((b[1], b[2], b[3], b[4], False, key, val, reng))
        return ins

    def barrier(self):
        for e in self.E:
            for k, v in self.val.items():
                if v > 0:
                    self._wait(e, k, v)
        self.recs.clear()

    def mm(self, out, lhsT, rhs, start=True, stop=True):
        return self.op('pe', lambda e: e.matmul(out, lhsT, rhs, start=start, stop=stop),
                       reads=[lhsT, rhs], writes=[out])

    def mmf(self, out, lhsT, rhs, start=True, stop=True):
        return self.op('pe', lambda e: e.matmul(out, lhsT, rhs, start=start, stop=stop), reads=[lhsT, rhs], writes=[out])

    def tr(self, out, in_, ident):
        return self.op('pe', lambda e: e.transpose(out, in_, ident), reads=[in_, ident], writes=[out])

    def act(self, out, in_, func, bias=None, scale=1.0, accum_out=None):
        rd = [in_]
        kw = {}
        if bias is not None:
            kw['bias'] = bias
            if not isinstance(bias, (int, float)):
                rd.append(bias)
        if not isinstance(scale, (int, float)):
            rd.append(scale)
        wr = [out]
        if accum_out is not None:
            kw['accum_out'] = accum_out
            wr.append(accum_out)
        return self.op('act', lambda e: e.activation(out, in_, func, scale=scale, **kw), reads=rd, writes=wr)

    def tt(self, eng, out, in0, in1, op):
        return self.op(eng, lambda e: e.tensor_tensor(out, in0, in1, op), reads=[in0, in1], writes=[out])

    def ts(self, eng, out, in0, s1, s2=None, op0=ALU.mult, op1=None):
        rd = [in0] + [s for s in (s1, s2) if s is not None and not isinstance(s, (int, float))]
        if op1 is None:
            return self.op(eng, lambda e: e.tensor_scalar(out, in0, s1, None, op0), reads=rd, writes=[out])
        return self.op(eng, lambda e: e.tensor_scalar(out, in0, s1, s2, op0, op1), reads=rd, writes=[out])

    def stt(self, eng, out, in0, scalar, in1, op0, op1):
        rd = [in0, in1] + ([] if isinstance(scalar, (int, float)) else [scalar])
        return self.op(eng, lambda e: e.scalar_tensor_tensor(out, in0, scalar, in1, op0, op1), reads=rd, writes=[out])

    def copy(self, eng, out, in_):
        if eng == 'act':
            return self.op('act', lambda e: e.copy(out, in_), reads=[in_], writes=[out])
        return self.op(eng, lambda e: e.tensor_copy(out, in_), reads=[in_], writes=[out])

    def memset(self, eng, ap, v):
        return self.op(eng, lambda e: e.memset(ap, v), writes=[ap])

    def reduce(self, eng, out, in_, op, axis=AX.X):
        return self.op(eng, lambda e: e.tensor_reduce(out, in_, axis, op), reads=[in_], writes=[out])

    def recip(self, out, in_):
        return self.op('dve', lambda e: e.reciprocal(out, in_), reads=[in_], writes=[out])

    def dma(self, q, out, in_):
        return self.op(q, lambda e: e.dma_start(out=out, in_=in_), reads=[in_], writes=[out], dma=True)


def _featT(v):
    v = np.asarray(v, np.float32).reshape(-1)
    return np.ascontiguousarray(v.reshape(v.size // 128, 128).T)


class _Cols:
    def __init__(self):
        self.parts = []
        self.off = {}
        self.n = 0

    def add(self, name, arr):
        self.off[name] = self.n
        self.parts.append(arr)
        self.n += arr.shape[1]

    def build(self):
        return np.ascontiguousarray(np.concatenate(self.parts, axis=1))


def _rope_tables(head_dim):
    rows = TL // 64
    rr, cc = np.meshgrid(np.arange(rows), np.arange(64), indexing='ij')
    row_pos = rr.reshape(-1).astype(np.float32)
    col_pos = cc.reshape(-1).astype(np.float32)
    axis_dim = head_dim // 2
    inv = (np.float32(10000.0) ** (-np.arange(0, axis_dim, 2, dtype=np.float32) / axis_dim)).astype(np.float32)
    ang = np.concatenate([row_pos[:, None] * inv, col_pos[:, None] * inv], -1).astype(np.float32)
    return np.cos(ang).astype(np.float32), np.sin(ang).astype(np.float32)


SV_LAYOUT = {}
RV_LAYOUT = {}


def _pack_small(inp, b):
    sv = _Cols()
    sv.add('c', _featT(inp['c'][b]))
    sv.add('cctx', _featT(inp['c_ctx']))
    for i in range(2):
        sv.add('ada_b%d' % i, _featT(inp['ada_b'][i]))
        for nm in ('ln1_g', 'ln1_b', 'ln2_g', 'ln2_b'):
            sv.add('%s%d' % (nm, i), _featT(inp[nm][i]))
    sv.add('mu', _featT(inp['ev_a_mu'][0]))
    for d in range(2):
        sv.add('w0_%d' % d, _featT(inp['ev_a_w0'][0, d]))
        sv.add('a0_%d' % d, _featT(inp['ev_a_a0'][0, d]))
    for nm in ('kk', 'ka', 'lnx_g', 'lnx_b'):
        sv.add(nm, _featT(inp['ev_a_' + nm][0]))
    sv.add('rk', _featT(inp['ev_a_rk'][0]))
    sv.add('subln_g', _featT(inp['ev_b_subln_g'][0]))
    rv = _Cols()
    rv.add('lam', np.asarray(inp['ev_b_lam'][0], np.float32).reshape(1, 256))
    rv.add('router_b', np.asarray(inp['router_b'], np.float32).reshape(1, 16))
    rv.add('qn_g', np.asarray(inp['od_qn_g'][0], np.float32).reshape(1, 128))
    rv.add('kn_g', np.asarray(inp['od_kn_g'][0], np.float32).reshape(1, 128))
    SV_LAYOUT.update(sv.off)
    SV_LAYOUT['_n'] = sv.n
    RV_LAYOUT.update(rv.off)
    RV_LAYOUT['_n'] = rv.n
    return sv.build(), rv.build()


def build_program(nsv, nrv, taps=()):
    nc = bass.Bass("TRN2", target_bir_lowering=False)
    dram = {}

    def din(name, shape, dt=F32):
        dram[name] = nc.dram_tensor(name, list(shape), dt, kind="ExternalInput").ap()
        return dram[name]

    def dscr(name, shape, dt=F32):
        kind = "ExternalOutput" if name in taps else "Internal"
        dram[name] = nc.dram_tensor(name, list(shape), dt, kind=kind).ap()
        return dram[name]

    SPEC = {
        'xin': [T, D], 'sv': [128, nsv], 'rv': [1, nrv], 'ident': [128, 128],
        'ada_w': [2, D, 6 * D], 'ev_w_in': [D, 3456], 'ev_w_out': [D, D],
        'od_w_in': [D, 1536], 'od_w_out': [D, D],
        'moe_w1': [2, 16, D, 512], 'moe_w3': [2, 16, D, 512], 'moe_w2': [2, 16, 512, D],
        'router_w': [D, 16], 'a_w2': [2, 64, 512], 'a_a2': [2, 64, 512], 'a_g2': [128, 512],
        'cosb': [TL, 32], 'sinb': [TL, 32], 'cosc': [TL, 64], 'sinc': [TL, 64], 'bdmask': [128, 128],
        'masks': [128, 6 * 128],
    }

    def W(name):
        if name not in dram:
            din(name, SPEC[name])
        return dram[name]

    xin = W('xin')
    sv_d = W('sv')
    rv_d = W('rv')
    ident_d = W('ident')
    ada_w = W('ada_w')
    bdmask_d = W('bdmask')
    out_d = nc.dram_tensor('out', [TL, D], F32, kind="ExternalOutput").ap()

    xt_d = dscr('xt_d', [128, NCH, T])
    ua_d = dscr('ua_d', [15, 128, T], BF16)
    ym_d = dscr('ym_d', [8, 128, T], BF16)

    SVO = SV_LAYOUT
    RVO = RV_LAYOUT

    with ExitStack() as es:
        es.enter_context(nc.allow_low_precision("bf16 matmul operands, fp32 accumulation"))
        P = Prog(nc, es)
        P.ro.update(SPEC.keys())

        def sb(name, shape, dt=F32, stack=es):
            return stack.enter_context(nc.sbuf_tensor(name, list(shape), dt))

        psf = [es.enter_context(nc.psum_tensor('psf%d' % i, [128, 512], F32)) for i in range(6)]
        psb = [es.enter_context(nc.psum_tensor('psb%d' % i, [128, 1024], BF16)) for i in range(2)]
        rot = {'f': 0, 'b': 0, 'e': 0}

        def PSF():
            rot['f'] += 1
            return psf[rot['f'] % 6]

        def PSB():
            rot['b'] += 1
            return psb[rot['b'] % 2]

        def EV():
            rot['e'] += 1
            return 'dve' if rot['e'] % 2 else 'act'

        SV = sb('SV', [128, nsv])
        RV = sb('RV', [128, nrv])
        IDF = sb('IDF', [128, 128])
        IDB = sb('IDB', [128, 128], BF16)
        ONESB = sb('ONESB', [128, 128], BF16)
        ONESF = sb('ONESF', [128, 128])
        BDM = sb('BDM', [128, 128])
        BDMB = sb('BDMB', [128, 128], BF16)
        MOD = sb('MOD', [128, 2, 48, 2])
        es_ht = ExitStack()
        HT = sb('HT', [128, NCH, T], BF16, es_ht)
        P.dma('sp', SV[:], sv_d[:, :])
        P.dma('sp', RV[:], rv_d.partition_broadcast(128))
        P.dma('sp', IDF[:], ident_d[:, :])
        P.copy('dve', IDB[:], IDF[:])
        P.dma('sp', BDM[:], bdmask_d[:, :])
        P.copy('dve', BDMB[:], BDM[:])
        P.memset('dve', ONESB[:], 1.0)
        P.memset('dve', ONESF[:], 1.0)

        def svc(name, j=0, n=1):
            o = SVO[name] + j
            return SV[:, o:o + n]

        def modc(i, m, c, w):
            return MOD[:, i, m * 8 + c, w:w + 1]

        with ExitStack() as ph:
            XT = sb('XT', [128, NCH, T], F32, ph)
            XS = [sb('XS%d' % i, [128, D], F32, ph) for i in range(2)]
            for tt in range(NT):
                xs = XS[tt % 2]
                P.dma('sp', xs[:], xin[tt * 128:(tt + 1) * 128, :])
                for hh in range(2):
                    ps = PSF()
                    for j in range(4):
                        c = hh * 4 + j
                        P.tr(ps[:, j * 128:(j + 1) * 128], xs[:, c * 128:(c + 1) * 128], IDF[:])
                    P.copy(EV(), XT[:, hh * 4:hh * 4 + 4, tt * 128:(tt + 1) * 128],
                           ps[:, :].rearrange("p (a b) -> p a b", a=4))
            for c in range(NCH):
                P.dma('sp', xt_d[:, c, :], XT[:, c, :])
            if STOP == 1:
                P.barrier(); nc._declared_inputs = set(k for k in dram if k in SPEC); return nc
            ST = sb('ST', [128, 8, 2], BF16, ph)
            P.act(ST[:, :, 0], svc('c', 0, 8), AF.Silu)
            P.act(ST[:, :, 1], svc('cctx', 0, 8), AF.Silu)
            AWF = [sb('AWF%d' % i, [128, 8, 512], F32, ph) for i in range(2)]
            AWB = [sb('AWB%d' % i, [128, 8, 512], BF16, ph) for i in range(2)]
            for i in range(2):
                for pc in range(12):
                    awf = AWF[(i * 12 + pc) % 2]
                    aw = AWB[(i * 12 + pc) % 2]
                    for kc in range(8):
                        P.dma('sp', awf[:, kc, :], ada_w[i, kc * 128:(kc + 1) * 128, pc * 512:(pc + 1) * 512])
                    P.copy('pool', aw[:, 0:4, :], awf[:, 0:4, :])
                    P.copy('act', aw[:, 4:8, :], awf[:, 4:8, :])
                    ps = PSF()
                    for j in range(4):
                        for kc in range(8):
                            P.mm(ps[:, 16 * j:16 * j + 2], aw[:, kc, j * 128:(j + 1) * 128], ST[:, kc, :],
                                 start=(kc == 0), stop=(kc == 7))
                    for j in range(4):
                        P.ts('dve', MOD[:, i, pc * 4 + j, :], ps[:, 16 * j:16 * j + 2],
                             svc('ada_b%d' % i, pc * 4 + j), None, ALU.add)
                for m in (1, 4):
                    P.ts('dve', MOD[:, i, m * 8:(m + 1) * 8, :], MOD[:, i, m * 8:(m + 1) * 8, :], 1.0, None, ALU.add)
                for m in (2, 5):
                    P.ts('dve', MOD[:, i, m * 8:(m + 1) * 8, :], MOD[:, i, m * 8:(m + 1) * 8, :], 1.0 / ALPHA, None, ALU.mult)
            if 'mod_tap' in taps:
                mdt = dscr('mod_tap', [128, 192])
                P.dma('sp', mdt[:, :], MOD[:].rearrange('p a b c -> p (a b c)'))
            if STOP == 2:
                P.barrier(); nc._declared_inputs = set(k for k in dram if k in SPEC); return nc
            for c in range(NCH):
                P.ts('dve', HT[:, c, 0:TC], XT[:, c, 0:TC], modc(0, 1, c, 1), modc(0, 0, c, 1), ALU.mult, ALU.add)
                P.ts('pool' if c % 2 else 'dve', HT[:, c, TC:T], XT[:, c, TC:T], modc(0, 1, c, 0), modc(0, 0, c, 0),
                     ALU.mult, ALU.add)
            if 'ht_tap' in taps:
                htt = dscr('ht_tap', [128, NCH, T], BF16)
                for c in range(NCH):
                    P.dma('sp', htt[:, c, :], HT[:, c, :])
            P.barrier()


        def rvc(name, j=0, n=1):
            o = RVO[name] + j
            return RV[:, o:o + n]

        def CV():
            rot['c'] = rot.get('c', 0) + 1
            return ('pool', 'act', 'dve')[rot['c'] % 3]

        def load_w(dst, src, rows_kc, c0, c1, stg):
            for kc in range(rows_kc):
                st = stg[kc % 2]
                P.dma('sp', st[:, 0:c1 - c0], src[kc * 128:(kc + 1) * 128, c0:c1])
                P.copy(CV(), dst[:, kc, :], st[:, 0:c1 - c0])

        ev_w_in = W('ev_w_in')
        with ExitStack() as ph:
            WA = sb('WA', [128, 8, 1920], BF16, ph)
            STG = [sb('STGa%d' % i, [128, 1920], F32, ph) for i in range(2)]
            load_w(WA, ev_w_in, 8, 0, 1920, STG)
            OMU = sb('OMU', [128, 15], F32, ph)
            HMU = sb('HMU', [128, 15], F32, ph)
            P.ts('dve', OMU[:], svc('mu', 0, 15), -1.0, 1.0, ALU.mult, ALU.add)
            P.ts('dve', HMU[:], svc('mu', 0, 15), 0.5, None, ALU.mult)
            PP = [sb('PP%d' % i, [128, 2312], F32, ph) for i in range(2)]
            P.memset('dve', PP[0][:], 0.0)
            P.memset('pool', PP[1][:], 0.0)
            T1 = sb('T1', [128, TL], F32, ph)
            T2 = sb('T2', [128, TL], F32, ph)
            UB = [sb('UB%d' % i, [128, T], BF16, ph) for i in range(2)]
            CO, LO = 1, 260
            for f in range(15):
                pp = PP[f % 2]
                for (a, b) in SEGS:
                    n = b - a
                    ps = PSF()
                    for kc in range(8):
                        P.mm(ps[:, :n], WA[:, kc, f * 128:(f + 1) * 128], HT[:, kc, a:b], start=(kc == 0), stop=(kc == 7))
                    off = CO + a if a < TC else LO + (a - TC)
                    P.copy(EV(), pp[:, off:off + n], ps[:, :n])
                ub = UB[f % 2]
                for (lo, n, t0) in ((CO, TC, 0), (LO, TL, TC)):
                    P.tt('pool', T1[:, :n], pp[:, lo - 1:lo - 1 + n], pp[:, lo + 1:lo + 1 + n], ALU.add)
                    P.ts('dve', T2[:, :n], pp[:, lo:lo + n], OMU[:, f:f + 1], None, ALU.mult)
                    P.stt('dve', T2[:, :n], T1[:, :n], HMU[:, f:f + 1], T2[:, :n], ALU.mult, ALU.add)
                    func = AF.Tanh if f == 12 else (AF.Sigmoid if f == 14 else AF.Identity)
                    P.act(ub[:, t0:t0 + n], T2[:, :n], func)
                P.dma('sp', ua_d[f, :, :], ub[:])
            P.barrier()
        if STOP == 3:
            es_ht.close()
            nc._declared_inputs = set(k for k in dram if k in SPEC)
            return nc

        LAMBDA_INIT0 = 0.8 - 0.6 * math.exp(0.0)
        with ExitStack() as ph:
            WB = sb('WB', [128, 8, 1536], BF16, ph)
            STG = [sb('STGb%d' % i, [128, 1536], F32, ph) for i in range(2)]
            load_w(WB, ev_w_in, 8, 1920, 3456, STG)
            CB = sb('CB', [128, 16, 32], F32, ph)
            SNB = sb('SNB', [128, 16, 32], F32, ph)
            P.dma('sp', CB[:], W('cosb').rearrange("(n p) f -> p n f", p=128))
            P.dma('sp', SNB[:], W('sinb').rearrange("(n p) f -> p n f", p=128))
            VB = sb('VB', [128, NT, 512], BF16, ph)
            QT = sb('QT', [128, 4, T], BF16, ph)
            KT = sb('KT', [128, 4, T], BF16, ph)
            QR = [sb('QR%d' % i, [128, 512], BF16, ph) for i in range(5)]
            RTMS = [[sb('RTM%d_%d' % (j, i), [128, 8, 32], F32, ph) for i in range(4)] for j in range(3)]
            qi = [0]

            def proj_b(tt, groups):
                tok = slice(tt * 128, (tt + 1) * 128)
                for g in groups:
                    ps = PSF()
                    for kc in range(8):
                        P.mm(ps[:, :], HT[:, kc, tok], WB[:, kc, g * 512:(g + 1) * 512], start=(kc == 0), stop=(kc == 7))
                    if g == 2:
                        P.copy(EV(), VB[:, tt, :], ps[:, :])
                        continue
                    qr = QR[qi[0] % 5]
                    qi[0] += 1
                    if tt < 2:
                        P.copy(EV(), qr[:], ps[:, :])
                    else:
                        v4 = ps[:, :].rearrange("p (g two d) -> p g two d", g=8, two=2)
                        o4 = qr[:].rearrange("p (g two d) -> p g two d", g=8, two=2)
                        x1, x2 = v4[:, :, 0, :], v4[:, :, 1, :]
                        cs = CB[:, tt - 2, :].unsqueeze(1).broadcast_to([128, 8, 32])
                        sn = SNB[:, tt - 2, :].unsqueeze(1).broadcast_to([128, 8, 32])
                        t1, t2, t3, t4 = [r[:] for r in RTMS[qi[0] % 3]]
                        P.tt('dve', t1, x1, cs, ALU.mult)
                        P.tt('dve', t2, x2, sn, ALU.mult)
                        P.tt('pool', o4[:, :, 0, :], t1, t2, ALU.subtract)
                        P.tt('dve', t3, x1, sn, ALU.mult)
                        P.tt('dve', t4, x2, cs, ALU.mult)
                        P.tt('pool', o4[:, :, 1, :], t3, t4, ALU.add)
                    def fin(qr=qr, g=g, tok=tok):
                        pb = PSB()
                        for h in range(4):
                            P.tr(pb[:, h * 128:(h + 1) * 128], qr[:, h * 128:(h + 1) * 128], IDB[:])
                        dst = QT if g == 0 else KT
                        P.copy(EV(), dst[:, :, tok], pb[:, 0:512].rearrange("p (a b) -> p a b", a=4))
                    pend_b.append(fin)
                    while len(pend_b) > 2:
                        pend_b.pop(0)()

            def flush_b():
                while pend_b:
                    pend_b.pop(0)()

            pend_b = []
            for tt in range(NT):
                proj_b(tt, [1, 2])
            flush_b()
            if 'qt_tap' in taps:
                for nm, src in (('qt_tap', QT), ('kt_tap', KT)):
                    tp = dscr(nm, [128, 4, T], BF16)
                    for h in range(4):
                        P.dma('sp', tp[:, h, :], src[:, h, :])
            LT = sb('LT', [128, 128], F32, ph)
            LS = sb('LS', [128, 2], F32, ph)
            NL = sb('NL', [128, 1], F32, ph)
            SG = sb('SG', [128, 1], F32, ph)
            P.tt('dve', LT[:, 0:64], rvc('lam', 0, 64), rvc('lam', 64, 64), ALU.mult)
            P.tt('dve', LT[:, 64:128], rvc('lam', 128, 64), rvc('lam', 192, 64), ALU.mult)
            P.reduce('dve', LS[:, 0:2], LT[:].rearrange("p (a b) -> p a b", a=2), ALU.add)
            P.act(LS[:, 0:2], LS[:, 0:2], AF.Exp)
            P.tt('dve', NL[:], LS[:, 1:2], LS[:, 0:1], ALU.subtract)
            P.ts('dve', NL[:], NL[:], -LAMBDA_INIT0, None, ALU.add)
            P.ts('dve', SG[:], svc('subln_g'), 1.0 - LAMBDA_INIT0, None, ALU.mult)
            PT = [sb('PT%d' % i, [128, 512], BF16, ph) for i in range(3)]
            R1 = sb('R1', [128, 512], F32, ph)
            R2 = sb('R2', [128, 512], F32, ph)
            O1 = sb('O1', [128, 512], F32, ph)
            O2 = sb('O2', [128, 512], F32, ph)
            SQ = sb('SQ', [128, 512], F32, ph)
            YB = [sb('YB%d' % i, [128, 512], BF16, ph) for i in range(2)]
            ACC = [[sb('ACC%d%d' % (m_, j_), [128, 512], F32, ph) for j_ in range(2)] for m_ in range(2)]
            cnt = 0
            yi = 0
            def q_seg(si):
                a, b = SEGS[si]
                for tt in range(a // 128, b // 128):
                    proj_b(tt, [0])
                flush_b()

            q_seg(0)
            q_seg(1)
            for si, (a, b) in enumerate(SEGS):
                if si + 2 < len(SEGS):
                    q_seg(si + 2)
                for h in range(4):
                    n = b - a
                    kts = list(range(2)) if a < TC else list(range(NT))
                    psO = [psf[0], psf[1]]
                    psD = [psf[2], psf[3]]
                    for m in range(2):
                        def s_mm(i):
                            kt = kts[i]
                            P.mm(psf[4 + (cnt + i) % 2][:, :n], KT[m * 64:(m + 1) * 64, h, kt * 128:(kt + 1) * 128],
                                 QT[m * 64:(m + 1) * 64, h, a:b])
                        s_mm(0)
                        for i, kt in enumerate(kts):
                            pS = psf[4 + (cnt + i) % 2]
                            pt = PT[(cnt + i) % 3]
                            if i + 1 < len(kts):
                                s_mm(i + 1)
                            P.act(pt[:, :n], pS[:, :n], AF.Exp, scale=0.125)
                            P.mm(psO[m][:, :n], VB[:, kt, h * 128:(h + 1) * 128], pt[:, :n],
                                 start=(i == 0), stop=(i == len(kts) - 1))
                            ai = 1 if i % 3 == 2 else 0
                            acc = ACC[m][ai]
                            aeng = 'pool' if ai else 'dve'
                            if i == 0 or i == 2:
                                P.copy(aeng, acc[:, :n], pt[:, :n])
                            else:
                                P.tt(aeng, acc[:, :n], acc[:, :n], pt[:, :n], ALU.add)
                        cnt += len(kts)
                        if len(kts) > 2:
                            P.mmf(psD[m][:, :n], ONESF[:], ACC[m][0][:, :n], start=True, stop=False)
                            P.mmf(psD[m][:, :n], ONESF[:], ACC[m][1][:, :n], start=False, stop=True)
                        else:
                            P.mmf(psD[m][:, :n], ONESF[:], ACC[m][0][:, :n], start=True, stop=True)
                    P.recip(R1[:, :n], psD[0][:, :n])
                    P.recip(R2[:, :n], psD[1][:, :n])
                    P.tt('dve', O1[:, :n], psO[0][:, :n], R1[:, :n], ALU.mult)
                    P.tt('dve', O2[:, :n], psO[1][:, :n], R2[:, :n], ALU.mult)
                    P.stt('dve', O1[:, :n], O2[:, :n], NL[:, 0:1], O1[:, :n], ALU.mult, ALU.add)
                    P.act(SQ[:, :n], O1[:, :n], AF.Square)
                    pq = psf[4 + cnt % 2]
                    cnt += 1
                    P.mmf(pq[:, :n], ONESF[:], SQ[:, :n])
                    P.act(R1[:, :n], pq[:, :n], AF.Sqrt, bias=1e-5, scale=1.0 / 128)
                    P.recip(R1[:, :n], R1[:, :n])
                    P.tt('dve', O1[:, :n], O1[:, :n], R1[:, :n], ALU.mult)
                    yb = YB[yi % 2]
                    yi += 1
                    P.ts('dve', yb[:, :n], O1[:, :n], SG[:, 0:1], None, ALU.mult)
                    P.dma('sp', ym_d[4 + h, :, a:b], yb[:, :n])
            P.barrier()
        if STOP == 4:
            es_ht.close()
            nc._declared_inputs = set(k for k in dram if k in SPEC)
            return nc


        es_ht.close()
        with ExitStack() as ph:
            C = 64
            NCK = T // C
            MSK = sb('MSK', [128, 4 * 128], F32, ph)
            P.dma('sp', MSK[:], W('masks')[:, 0:512])
            MK4 = [sb('MK4_%d' % i, [128, 512], F32, ph) for i in range(2)]
            for d, (m1, m2) in enumerate(((0, 1), (2, 3))):
                for q in range(4):
                    mm_ = m1 if q % 2 == 0 else m2
                    P.copy('dve', MK4[d][:, q * 128:(q + 1) * 128], MSK[:, mm_ * 128:(mm_ + 1) * 128])
            MKA = [MSK[:, 256:384], MSK[:, 0:128]]
            LW = sb('LW', [128, T], F32, ph)
            W2B = sb('W2B', [128, 512], BF16, ph)
            A2B = sb('A2B', [128, 512], BF16, ph)
            G2B = sb('G2B', [128, 512], BF16, ph)
            for dst, src in ((W2B, W('a_w2').rearrange("d l c -> (d l) c")), (A2B, W('a_a2').rearrange("d l c -> (d l) c")),
                             (G2B, W('a_g2'))):
                P.dma('sp', LW[:, 0:512], src)
                P.copy('dve', dst[:], LW[:, 0:512])
            OMKA = sb('OMKA', [128, 4], F32, ph)
            P.ts('dve', OMKA[:], svc('ka', 0, 4), -1.0, 1.0, ALU.mult, ALU.add)
            LORA = sb('LORA', [128, 3, T], BF16, ph)
            for i in range(3):
                P.dma('sp', LORA[:, i, :], ua_d[12 + i, :, :])
            RKV = sb('RKV', [128, 3, T], BF16, ph)
            KKt = sb('KKt', [128, T], BF16, ph)
            Ad = sb('Ad', [128, T], BF16, ph)
            LA = sb('LA', [128, T], F32, ph)
            LB = sb('LB', [128, T], F32, ph)
            PRs = [sb('PR%d' % i, [128, 6, T], BF16, ph) for i in range(2)]
            VBD = sb('VBD', [128, NCK, 128], BF16, ph)
            YACC = sb('YACC', [128, T], F32, ph)
            KDS = sb('KDS', [128, T], BF16, ph)
            LCts = [sb('LCt%d' % i, [128, NCK], F32, ph) for i in range(2)]
            GCs = [sb('GC%d' % i, [128, NCK], F32, ph) for i in range(2)]
            HFs = [sb('HF%d' % i, [128, 128], F32, ph) for i in range(2)]
            HBs = [sb('HB%d' % i, [128, 128], BF16, ph) for i in range(2)]
            TS = [sb('TSg%d' % i, [128, 512], F32, ph) for i in range(4)]
            G = int(os.environ.get('KG', '3'))
            NS = 2 * G
            XBD = [sb('XBD%d' % i, [128, 6, 128], BF16, ph) for i in range(NS)]
            W4 = [sb('W4_%d' % i, [128, 512], BF16, ph) for i in range(NS)]
            NAb = [sb('NAb%d' % i, [128, 256], BF16, ph) for i in range(2 * NS)]
            PQb = [sb('PQb%d' % i, [128, 256], BF16, ph) for i in range(2 * NS)]
            TOK = [sb('TOK%d' % i, [128, 3, 128], BF16, ph) for i in range(NS)]
            ZS = [sb('ZS%d' % i, [128, 128], BF16, ph) for i in range(NS)]
            US = [sb('US%d' % i, [128, 128], BF16, ph) for i in range(NS)]
            YOUT = [sb('YOUT%d' % i, [128, 512], BF16, ph) for i in range(2)]
            bdm3 = BDMB[:].rearrange("p (a b) -> p a b", a=2)
            CEXP = -math.exp(-0.5)
            v3 = lambda t_: t_[:].rearrange("p (c j) -> p c j", j=C)

            for hp in range(4):
                for i in range(3):
                    P.dma('sp', RKV[:, i, :], ua_d[4 * i + hp, :, :])
                r_, k_, v_ = RKV[:, 0, :], RKV[:, 1, :], RKV[:, 2, :]
                hc = slice(hp * 128, (hp + 1) * 128)
                for (a, b) in SEGS:
                    n = b - a
                    kx, sq, rn = TS[0], TS[1], TS[2]
                    P.ts('dve', kx[:, :n], k_[:, a:b], svc('kk', hp), None, ALU.mult)
                    P.act(sq[:, :n], kx[:, :n], AF.Square)
                    ps = PSF()
                    P.mmf(ps[:, :n], BDM[:], sq[:, :n])
                    P.act(rn[:, :n], ps[:, :n], AF.Sqrt, bias=1e-24)
                    P.recip(rn[:, :n], rn[:, :n])
                    P.tt('dve', KKt[:, a:b], kx[:, :n], rn[:, :n], ALU.mult)
                P.tt('pool', VBD[:].rearrange("p c (a b) -> p c a b", a=2),
                     v_.rearrange("p (c j) -> p c j", j=C).unsqueeze(2).broadcast_to([128, NCK, 2, C]),
                     bdm3.unsqueeze(1).broadcast_to([128, NCK, 2, C]), ALU.mult)
                P.memset('pool', YACC[:], 0.0)
                for d in range(2):
                    PR, LCt, GC = PRs[d], LCts[d], GCs[d]
                    dsl = slice(d * 64, (d + 1) * 64)
                    for (a, b) in SEGS:
                        n = b - a
                        ps = PSF()
                        P.mm(ps[:, :n], W2B[dsl, hc], LORA[dsl, 0, a:b])
                        P.act(TS[0][:, :n], ps[:, :n], AF.Sigmoid, bias=svc('w0_%d' % d, hp))
                        P.ts('pool', LW[:, a:b], TS[0][:, :n], CEXP, None, ALU.mult)
                        ps = PSF()
                        P.mm(ps[:, :n], A2B[dsl, hc], LORA[dsl, 1, a:b])
                        P.act(Ad[:, a:b], ps[:, :n], AF.Sigmoid, bias=svc('a0_%d' % d, hp))
                    seq = [(LW, LA), (LA, LB), (LB, LA), (LA, LB), (LB, LA), (LA, LB)]
                    for si, (src, dst) in enumerate(seq):
                        sft = 1 << si
                        s3, d3 = v3(src), v3(dst)
                        if d == 0:
                            P.tt('dve', d3[:, :, sft:], s3[:, :, sft:], s3[:, :, :C - sft], ALU.add)
                            P.copy('pool', d3[:, :, :sft], s3[:, :, :sft])
                        else:
                            P.tt('dve', d3[:, :, :C - sft], s3[:, :, :C - sft], s3[:, :, sft:], ALU.add)
                            P.copy('pool', d3[:, :, C - sft:], s3[:, :, C - sft:])
                    L3 = v3(LB)
                    P.tt('dve', LA[:], LB[:], LW[:], ALU.subtract)
                    P.copy('dve', LCt[:], L3[:, :, C - 1] if d == 0 else L3[:, :, 0])
                    P.act(GC[:], LCt[:], AF.Exp)
                    for (a, b) in SEGS:
                        n = b - a
                        c0, c1 = a // C, b // C
                        e, ba, kd, tq = TS[0], TS[1], TS[2], TS[3]
                        P.act(e[:, :n], LB[:, a:b], AF.Exp)
                        P.tt('dve', PR[:, 1, a:b], r_[:, a:b], e[:, :n], ALU.mult)
                        P.act(e[:, :n], LA[:, a:b], AF.Exp)
                        P.stt('dve', PR[:, 0, a:b], KKt[:, a:b], -1.0, e[:, :n], ALU.mult, ALU.mult)
                        P.tt('pool', ba[:, :n], KKt[:, a:b], Ad[:, a:b], ALU.mult)
                        P.ts('dve', tq[:, :n], Ad[:, a:b], svc('ka', hp), OMKA[:, hp:hp + 1], ALU.mult, ALU.add)
                        P.tt('dve', kd[:, :n], tq[:, :n], k_[:, a:b], ALU.mult)
                        if d == 0:
                            P.copy('pool', KDS[:, a:b], kd[:, :n])
                        else:
                            P.tt('pool', KDS[:, a:b], KDS[:, a:b], kd[:, :n], ALU.add)
                        P.act(e[:, :n], LB[:, a:b], AF.Exp, scale=-1.0)
                        P.tt('dve', PR[:, 2, a:b], ba[:, :n], e[:, :n], ALU.mult)
                        P.tt('pool', PR[:, 3, a:b], kd[:, :n], e[:, :n], ALU.mult)
                        P.tt('dve', tq[:, :n].rearrange("p (c j) -> p c j", j=C),
                             LCt[:, c0:c1].unsqueeze(2).broadcast_to([128, c1 - c0, C]),
                             LB[:, a:b].rearrange("p (c j) -> p c j", j=C), ALU.subtract)
                        P.act(e[:, :n], tq[:, :n], AF.Exp)
                        P.tt('dve', PR[:, 4, a:b], ba[:, :n], e[:, :n], ALU.mult)
                        P.tt('pool', PR[:, 5, a:b], kd[:, :n], e[:, :n], ALU.mult)
                    P.memset('dve', HFs[d][:], 0.0)
                    P.memset('pool', HBs[d][:], 0.0)

                seq_pos = [0, 0]

                freef = list(psf)
                freeb = list(psb)

                def unit(d, pos, c):
                    bi = d * G + pos % G
                    PR, GC, HF, HB = PRs[d], GCs[d], HFs[d], HBs[d]
                    cs = slice(c * C, (c + 1) * C)
                    xbd, w4, tok, zs, us = XBD[bi], W4[bi], TOK[bi], ZS[bi], US[bi]
                    P.tt('dve' if d == 0 else 'pool', xbd[:].rearrange("p s (a b) -> p s a b", a=2),
                         PR[:, :, cs].unsqueeze(2).broadcast_to([128, 6, 2, C]),
                         bdm3.unsqueeze(1).broadcast_to([128, 6, 2, C]), ALU.mult)
                    yield
                    AtBD, RtBD, BtBD, KtBD, BhBD, KhBD = [xbd[:, i, :] for i in range(6)]
                    AR = xbd[:, 0:2, :].rearrange("p s f -> p (s f)")
                    while len(freef) < 2 or len(freeb) < 1:
                        yield
                    ps1, ps2, pb = freef.pop(0), freef.pop(0), freeb.pop(0)
                    P.mm(ps1[:, 0:256], BtBD, AR)
                    P.mm(ps1[:, 256:512], KtBD, AR)
                    P.mm(ps2[:, 0:128], AtBD, BtBD)
                    P.tr(pb[:, 0:128], VBD[:, c, :], IDB[:])
                    P.tr(pb[:, 128:256], BhBD, IDB[:])
                    P.tr(pb[:, 256:384], KhBD, IDB[:])
                    yield
                    na = NAb[2 * bi]
                    pq = PQb[2 * bi]
                    P.tt('dve', w4[:], ps1[:, :], MK4[d][:], ALU.mult)
                    P.tt('dve', na[:, 128:256], ps2[:, 0:128], MKA[d], ALU.mult)
                    P.copy('act', tok[:].rearrange("p s f -> p (s f)"), pb[:, 0:384])
                    freef.extend([ps1, ps2])
                    freeb.append(pb)
                    P.copy('act', na[:, 0:128], w4[:, 0:128])
                    P.tt('pool', pq[:].rearrange("p (a b) -> p a b", a=2), na[:].rearrange("p (a b) -> p a b", a=2),
                         IDB[:].unsqueeze(1).broadcast_to([128, 2, 128]), ALU.add)
                    for lv in range(5):
                        na2 = NAb[2 * bi + (lv + 1) % 2]
                        pq2 = PQb[2 * bi + (lv + 1) % 2]
                        while len(freef) < 1:
                            yield
                        psn = freef.pop(0)
                        P.mm(psn[:, 0:128], na[:, 128:256], na[:, 0:128])
                        P.mm(psn[:, 128:256], na[:, 0:128], na[:, 128:256])
                        yield
                        P.copy('act', na2[:], psn[:, 0:256])
                        freef.append(psn)
                        while len(freef) < 1:
                            yield
                        psp = freef.pop(0)
                        P.mm(psp[:, 0:128], pq[:, 128:256], na2[:, 0:128])
                        P.mm(psp[:, 128:256], na2[:, 0:128], pq[:, 128:256])
                        yield
                        P.tt('dve', pq2[:], psp[:, 0:256], pq[:], ALU.add)
                        freef.append(psp)
                        na, pq = na2, pq2
                    VtBD, BhT, KhT = tok[:, 0, :], tok[:, 1, :], tok[:, 2, :]
                    while seq_pos[d] != pos or len(freef) < 1:
                        yield
                    psz = freef.pop(0)
                    P.mm(psz[:, 0:128], AtBD, HB[:], start=True, stop=False)
                    P.mm(psz[:, 0:128], w4[:, 256:384], VtBD, start=False, stop=True)
                    yield
                    P.copy('act', zs[:], psz[:, 0:128])
                    freef.append(psz)
                    while len(freef) < 1:
                        yield
                    psu = freef.pop(0)
                    P.mm(psu[:, 0:128], pq[:, 0:128], zs[:])
                    yield
                    P.copy('act', us[:], psu[:, 0:128])
                    freef.append(psu)
                    while len(freef) < 2:
                        yield
                    psh, psy = freef.pop(0), freef.pop(0)
                    P.mm(psh[:, 0:128], BhT, us[:], start=True, stop=False)
                    P.mm(psh[:, 0:128], KhT, VtBD, start=False, stop=True)
                    P.mm(psy[:, 0:128], HB[:], RtBD, start=True, stop=False)
                    P.mm(psy[:, 0:128], us[:], w4[:, 128:256], start=False, stop=False)
                    P.mm(psy[:, 0:128], VtBD, w4[:, 384:512], start=False, stop=True)
                    yield
                    P.stt('dve', HF[:], HF[:], GC[:, c:c + 1], psh[:, 0:128], ALU.mult, ALU.add)
                    P.copy('act', HB[:], HF[:])
                    for hh in range(2):
                        rs = slice(hh * 64, (hh + 1) * 64)
                        P.tt('dve', YACC[rs, cs], YACC[rs, cs], psy[rs, hh * 64:(hh + 1) * 64], ALU.add)
                    freef.extend([psh, psy])
                    seq_pos[d] += 1

                orders = [list(range(NCK)), [3, 2, 1, 0] + list(range(NCK - 1, 3, -1))]
                nxt = [0, 0]
                active = []
                while active or nxt[0] < NCK or nxt[1] < NCK:
                    for d in range(2):
                        while sum(1 for (dd, _) in active if dd == d) < G and nxt[d] < NCK:
                            active.append((d, unit(d, nxt[d], orders[d][nxt[d]])))
                            nxt[d] += 1
                    for item in list(active):
                        try:
                            next(item[1])
                        except StopIteration:
                            active.remove(item)
                for (a, b) in SEGS:
                    n = b - a
                    psg = PSF()
                    P.mm(psg[:, :n], G2B[:, hc], LORA[:, 2, a:b])
                    psm = PSF()
                    P.mmf(psm[:, :n], BDM[:], YACC[:, a:b])
                    sq, mu, t3, pr = TS[0], TS[1], TS[2], TS[3]
                    P.act(sq[:, :n], YACC[:, a:b], AF.Square)
                    psq = PSF()
                    P.mmf(psq[:, :n], BDM[:], sq[:, :n])
                    P.ts('dve', mu[:, :n], psm[:, :n], 1.0 / 64, None, ALU.mult)
                    P.tt('dve', t3[:, :n], mu[:, :n], mu[:, :n], ALU.mult)
                    P.stt('dve', t3[:, :n], psq[:, :n], 1.0 / 64, t3[:, :n], ALU.mult, ALU.subtract)
                    P.act(t3[:, :n], t3[:, :n], AF.Sqrt, bias=64e-5)
                    P.recip(t3[:, :n], t3[:, :n])
                    P.tt('dve', sq[:, :n], YACC[:, a:b], mu[:, :n], ALU.subtract)
                    P.tt('dve', sq[:, :n], sq[:, :n], t3[:, :n], ALU.mult)
                    P.ts('dve', sq[:, :n], sq[:, :n], svc('lnx_g', hp), svc('lnx_b', hp), ALU.mult, ALU.add)
                    P.stt('dve', pr[:, :n], r_[:, a:b], svc('rk', hp), KDS[:, a:b], ALU.mult, ALU.mult)
                    psb_ = PSF()
                    P.mmf(psb_[:, :n], BDM[:], pr[:, :n])
                    P.tt('dve', mu[:, :n], psb_[:, :n], v_[:, a:b], ALU.mult)
                    P.tt('dve', sq[:, :n], sq[:, :n], mu[:, :n], ALU.add)
                    yo = YOUT[(a // 512) % 2]
                    P.tt('dve', yo[:, :n], sq[:, :n], psg[:, :n], ALU.mult)
                    P.dma('sp', ym_d[hp, :, a:b], yo[:, :n])
            P.barrier()
        if STOP == 5:
            nc._declared_inputs = set(k for k in dram if k in SPEC)
            return nc


        def load_w2(dst, src, rows_kc, ncols, stg):
            i = 0
            for kc in range(rows_kc):
                for c0 in range(0, ncols, 512):
                    st = stg[i % 2]
                    i += 1
                    P.dma('sp', st[:, 0:512], src[kc * 128:(kc + 1) * 128, c0:c0 + 512])
                    P.copy(CV(), dst[:, kc, c0:c0 + 512], st[:, 0:512])

        def layer_norm(XT, a, b, gname, bname, tmp):
            n = b - a
            SQa, SQb, MU, RS = tmp
            psm = PSF()
            for c in range(8):
                P.mmf(psm[:, :n], ONESF[:], XT[:, c, a:b], start=(c == 0), stop=(c == 7))
            psq = PSF()
            for c in range(8):
                sq = SQa if c % 2 == 0 else SQb
                P.act(sq[:, :n], XT[:, c, a:b], AF.Square)
                P.mmf(psq[:, :n], ONESF[:], sq[:, :n], start=(c == 0), stop=(c == 7))
            P.ts('dve', MU[:, :n], psm[:, :n], 1.0 / D, None, ALU.mult)
            P.tt('dve', RS[:, :n], MU[:, :n], MU[:, :n], ALU.mult)
            P.stt('dve', RS[:, :n], psq[:, :n], 1.0 / D, RS[:, :n], ALU.mult, ALU.subtract)
            P.act(RS[:, :n], RS[:, :n], AF.Sqrt, bias=LN_EPS / (ALPHA * ALPHA))
            P.recip(RS[:, :n], RS[:, :n])
            for c in range(8):
                eng = 'dve' if c % 2 == 0 else 'pool'
                P.tt(eng, XT[:, c, a:b], XT[:, c, a:b], MU[:, :n], ALU.subtract)
                P.tt(eng, XT[:, c, a:b], XT[:, c, a:b], RS[:, :n], ALU.mult)
                P.ts(eng, XT[:, c, a:b], XT[:, c, a:b], svc(gname, c), svc(bname, c), ALU.mult, ALU.add)

        def phase_D(layer, w_out_name, segs, tiles, last):
            L = layer
            with ExitStack() as ph:
                XT = sb('XTd%d' % L, [128, NCH, T], F32, ph)
                for c in range(8):
                    P.dma('sp', XT[:, c, :], xt_d[:, c, :])
                HT2 = sb('HT2_%d' % L, [128, NCH, T], BF16, ph)
                LNT = [sb('LNT%d_%d' % (L, i), [128, 512], F32, ph) for i in range(4)]
                with ExitStack() as p1:
                    WO = sb('WO%d' % L, [128, 8, 1024], BF16, p1)
                    STG = [sb('STGo%d_%d' % (L, i), [128, 512], F32, p1) for i in range(2)]
                    load_w2(WO, W(w_out_name), 8, 1024, STG)
                    YM = [sb('YM%d_%d' % (L, i), [128, 8, 512], BF16, p1) for i in range(2)]
                    for si, (a, b) in enumerate(segs):
                        n = b - a
                        w = 1 if a < TC else 0
                        ym = YM[si % 2]
                        for kc in range(8):
                            P.dma('sp', ym[:, kc, :n], ym_d[kc, :, a:b])
                        for c in range(8):
                            ps = PSF()
                            for kc in range(8):
                                P.mm(ps[:, :n], WO[:, kc, c * 128:(c + 1) * 128], ym[:, kc, :n], start=(kc == 0), stop=(kc == 7))
                            P.stt('dve', XT[:, c, a:b], ps[:, :n], modc(L, 2, c, w), XT[:, c, a:b], ALU.mult, ALU.add)
                        layer_norm(XT, a, b, 'ln1_g%d' % L, 'ln1_b%d' % L, LNT)
                        for c in range(8):
                            P.ts('pool' if c % 2 else 'dve', HT2[:, c, a:b], XT[:, c, a:b], modc(L, 4, c, w), modc(L, 3, c, w),
                                 ALU.mult, ALU.add)
                    P.barrier()
                if ('xln1_tap%d' % L) in taps:
                    tp = dscr('xln1_tap%d' % L, [128, NCH, T])
                    for c in range(8):
                        P.dma('sp', tp[:, c, :], XT[:, c, :])
                if STOP == 61:
                    P.barrier(); return
                GTb = sb('GTb%d' % L, [16, T], BF16, ph)
                with ExitStack() as p2:
                    RW = sb('RW%d' % L, [128, 8, 16], F32, p2)
                    P.dma('sp', RW[:], W('router_w').rearrange("(c p) e -> p c e", p=128))
                    RWS = [sb('RWS%d_%d' % (L, i), [128, 8, 16], F32, p2) for i in range(2)]
                    SHB = [sb('SHB%d_%d' % (L, i), [128, 8, 128], F32, p2) for i in range(2)]
                    for w in range(2):
                        for c in range(8):
                            P.ts('dve', RWS[w][:, c, :], RW[:, c, :], modc(L, 4, c, w), None, ALU.mult)
                            P.ts('pool', SHB[w][:, c, :], ONESF[:], modc(L, 3, c, w), None, ALU.mult)
                    RT_ = [sb('RTR%d_%d' % (L, i), [128, 16], F32, p2) for i in range(8)]
                    RS_ = [sb('RSM%d_%d' % (L, i), [128, 4], F32, p2) for i in range(6)]
                    GTf = sb('GTf%d' % L, [16, 128], F32, p2)
                    for tt in tiles:
                        w = 1 if tt < 2 else 0
                        tok = slice(tt * 128, (tt + 1) * 128)
                        ps = PSF()
                        for c in range(8):
                            P.mmf(ps[:, 0:16], XT[:, c, tok], RWS[w][:, c, :], start=(c == 0), stop=False)
                        for c in range(8):
                            P.mmf(ps[:, 0:16], SHB[w][:, c, :], RW[:, c, :], start=False, stop=(c == 7))
                        LG, E, EQ, EM, SEL, GATE = [t_[:] for t_ in RT_[:6]]
                        M1, M2, GS, ING, MX, RG = [t_[:] for t_ in RS_]
                        v3 = lambda x: x.rearrange("p (g e) -> p g e", g=4)
                        P.tt('dve', LG, ps[:, 0:16], rvc('router_b', 0, 16), ALU.add)
                        P.reduce('dve', MX[:, 0:1], LG, ALU.max)
                        P.ts('dve', MX[:, 1:2], MX[:, 0:1], -1.0, None, ALU.mult)
                        P.act(E, LG, AF.Exp, bias=MX[:, 1:2])
                        P.reduce('dve', M1, v3(E), ALU.max)
                        P.tt('dve', v3(EQ), v3(E), M1.unsqueeze(2).broadcast_to([128, 4, 4]), ALU.is_equal)
                        P.tt('dve', EQ, EQ, E, ALU.mult)
                        P.tt('dve', EM, E, EQ, ALU.subtract)
                        P.reduce('dve', M2, v3(EM), ALU.max)
                        P.tt('dve', GS, M1, M2, ALU.add)
                        P.reduce('dve', MX[:, 2:3], GS, ALU.max)
                        P.ts('dve', ING, GS, MX[:, 2:3], None, ALU.is_equal)
                        P.tt('dve', v3(SEL), v3(E), M2.unsqueeze(2).broadcast_to([128, 4, 4]), ALU.is_ge)
                        P.tt('dve', v3(SEL), v3(SEL), ING.unsqueeze(2).broadcast_to([128, 4, 4]), ALU.mult)
                        P.recip(RG[:, 0:1], MX[:, 2:3])
                        P.stt('dve', GATE, E, RG[:, 0:1], SEL, ALU.mult, ALU.mult)
                        pt_ = PSF()
                        P.tr(pt_[0:16, 0:128], GATE, IDF[:])
                        P.copy('act', GTb[:, tok], pt_[0:16, 0:128])
                    P.barrier()
                if ('gate_tap%d' % L) in taps:
                    tp = dscr('gate_tap%d' % L, [16, T], BF16)
                    P.dma('sp', tp[:, :], GTb[:])
                if STOP == 62:
                    P.barrier(); return
                with ExitStack() as p3:
                    SELM = sb('SELM%d' % L, [16, 16, 128], BF16, p3)
                    for e in range(16):
                        P.ts('dve', SELM[:, e, :], ONESF[0:16, :], IDF[0:16, e:e + 1], None, ALU.mult)
                    W1s = [sb('W1_%d_%d' % (L, i), [128, 8, 512], BF16, p3) for i in range(2)]
                    W3s = [sb('W3_%d_%d' % (L, i), [128, 8, 512], BF16, p3) for i in range(2)]
                    W2s = [sb('W2_%d_%d' % (L, i), [128, 4, 1024], BF16, p3) for i in range(2)]
                    STG = [sb('STGe%d_%d' % (L, i), [128, 512], F32, p3) for i in range(2)]
                    HID = [sb('HID%d_%d' % (L, i), [128, 4, 512], BF16, p3) for i in range(2)]
                    GB = [sb('GB%d_%d' % (L, i), [128, 512], F32, p3) for i in range(2)]
                    S1 = [sb('S1_%d_%d' % (L, i), [128, 512], F32, p3) for i in range(2)]
                    print('MoE phase sbuf remaining', nc.sbuf_bytes_remaining)
                    NE = int(os.environ.get('KEXP', '16'))
                    items = [(e, si, a, b) for e in range(NE) for si, (a, b) in enumerate(segs)]

                    def up(k):
                        e, si, a, b = items[k]
                        n = b - a
                        W1, W3, W2 = W1s[e % 2], W3s[e % 2], W2s[e % 2]
                        if si == 0:
                            load_w2(W1, W('moe_w1')[L, e], 8, 512, STG)
                            load_w2(W3, W('moe_w3')[L, e], 8, 512, STG)
                            load_w2(W2, W('moe_w2')[L, e], 4, 1024, STG)
                        gb, hid = GB[k % 2], HID[k % 2]
                        psg = PSF()
                        P.mm(psg[:, :n], SELM[:, e, :], GTb[:, a:b])
                        P.copy('act', gb[:, :n], psg[:, :n])
                        for fc in range(4):
                            ps1 = PSF()
                            for kc in range(8):
                                P.mm(ps1[:, :n], W1[:, kc, fc * 128:(fc + 1) * 128], HT2[:, kc, a:b], start=(kc == 0), stop=(kc == 7))
                            ps3 = PSF()
                            for kc in range(8):
                                P.mm(ps3[:, :n], W3[:, kc, fc * 128:(fc + 1) * 128], HT2[:, kc, a:b], start=(kc == 0), stop=(kc == 7))
                            s1 = S1[fc % 2]
                            P.act(s1[:, :n], ps1[:, :n], AF.Silu)
                            P.tt('dve', s1[:, :n], s1[:, :n], ps3[:, :n], ALU.mult)
                            P.tt('pool', hid[:, fc, :n], s1[:, :n], gb[:, :n], ALU.mult)

                    def down(k):
                        e, si, a, b = items[k]
                        n = b - a
                        w = 1 if a < TC else 0
                        W2 = W2s[e % 2]
                        hid = HID[k % 2]
                        for c in range(8):
                            ps = PSF()
                            for fc in range(4):
                                P.mm(ps[:, :n], W2[:, fc, c * 128:(c + 1) * 128], hid[:, fc, :n], start=(fc == 0), stop=(fc == 3))
                            P.stt('dve', XT[:, c, a:b], ps[:, :n], modc(L, 5, c, w), XT[:, c, a:b], ALU.mult, ALU.add)

                    up(0)
                    for k in range(len(items)):
                        if k + 1 < len(items):
                            up(k + 1)
                        down(k)
                    P.barrier()
                for (a, b) in segs:
                    layer_norm(XT, a, b, 'ln2_g%d' % L, 'ln2_b%d' % L, LNT)
                if not last:
                    for c in range(8):
                        P.dma('sp', xt_d[:, c, :], XT[:, c, :])
                else:
                    OS = [sb('OS%d' % i, [128, D], F32, ph) for i in range(2)]
                    for tt in range(2, NT):
                        os_ = OS[tt % 2]
                        tok = slice(tt * 128, (tt + 1) * 128)
                        for hh in range(2):
                            ps = PSF()
                            for j in range(4):
                                P.tr(ps[:, j * 128:(j + 1) * 128], XT[:, hh * 4 + j, tok], IDF[:])
                            P.copy(EV(), os_[:, hh * 512:(hh + 1) * 512], ps[:, :])
                        P.dma('sp', out_d[(tt - 2) * 128:(tt - 1) * 128, :], os_[:])
                P.barrier()

        phase_D(0, 'ev_w_out', SEGS, list(range(NT)), False)
        if STOP in (6, 61, 62):
            nc._declared_inputs = set(k for k in dram if k in SPEC)
            return nc


        with ExitStack() as ph:
            HT1 = sb('HT1', [128, NCH, T], BF16, ph)
            with ExitStack() as px:
                XC = [sb('XC%d' % i, [128, T], F32, px) for i in range(2)]
                for c in range(8):
                    xc = XC[c % 2]
                    P.dma('sp', xc[:], xt_d[:, c, :])
                    P.ts('dve', HT1[:, c, 0:TC], xc[:, 0:TC], modc(1, 1, c, 1), modc(1, 0, c, 1), ALU.mult, ALU.add)
                    P.ts('pool', HT1[:, c, TC:T], xc[:, TC:T], modc(1, 1, c, 0), modc(1, 0, c, 0), ALU.mult, ALU.add)
                P.barrier()
            WQ = sb('WQ', [128, 8, 1536], BF16, ph)
            STG = [sb('STGq%d' % i, [128, 512], F32, ph) for i in range(2)]
            load_w2(WQ, W('od_w_in'), 8, 1536, STG)
            CC = sb('CC', [128, 16, 64], F32, ph)
            SC = sb('SC', [128, 16, 64], F32, ph)
            P.dma('sp', CC[:], W('cosc').rearrange("(n p) f -> p n f", p=128))
            P.dma('sp', SC[:], W('sinc').rearrange("(n p) f -> p n f", p=128))
            QT1 = sb('QT1', [128, 8, TL], BF16, ph)
            KT1 = sb('KT1', [128, 2, T], BF16, ph)
            V1 = sb('V1', [128, NT, 256], BF16, ph)
            XQs = [sb('XQ%d' % j, [128, 512], F32, ph) for j in range(3)]
            TQs = [sb('TQ%d' % j, [128, 512], F32, ph) for j in range(3)]
            RTQs = [[sb('RTQ%d_%d' % (j, i), [128, 4, 64], F32, ph) for i in range(4)] for j in range(3)]
            SSQs = [sb('SSQ%d' % j, [128, 4], F32, ph) for j in range(3)]
            QN = [sb('QN%d' % i, [128, 512], BF16, ph) for i in range(5)]
            qn_i = [0]

            def normrope(psv, H, gname, rope, tile_i):
                n = H * 128
                out = QN[qn_i[0] % 5]
                XQ, TQ, RTQ, SSQ = XQs[qn_i[0] % 3], TQs[qn_i[0] % 3], RTQs[qn_i[0] % 3], SSQs[qn_i[0] % 3]
                qn_i[0] += 1
                x3 = XQ[:, :n].rearrange("p (h d) -> p h d", h=H)
                t3 = TQ[:, :n].rearrange("p (h d) -> p h d", h=H)
                o3 = out[:, :n].rearrange("p (h d) -> p h d", h=H)
                P.copy('act', XQ[:, :n], psv)
                P.tt('dve', TQ[:, :n], XQ[:, :n], XQ[:, :n], ALU.mult)
                P.reduce('dve', SSQ[:, :H], t3, ALU.add)
                P.act(SSQ[:, :H], SSQ[:, :H], AF.Sqrt, bias=1e-6, scale=1.0 / 128)
                P.recip(SSQ[:, :H], SSQ[:, :H])
                P.tt('dve', t3, x3, SSQ[:, :H].unsqueeze(2).broadcast_to([128, H, 128]), ALU.mult)
                gb = rvc(gname, 0, 128).unsqueeze(1).broadcast_to([128, H, 128])
                if not rope:
                    P.tt('dve', o3, t3, gb, ALU.mult)
                    return out
                P.tt('pool', x3, t3, gb, ALU.mult)
                x1, x2 = x3[:, :, 0:64], x3[:, :, 64:128]
                cs = CC[:, tile_i, :].unsqueeze(1).broadcast_to([128, H, 64])
                sn = SC[:, tile_i, :].unsqueeze(1).broadcast_to([128, H, 64])
                t1, t2, t3_, t4 = [r[:, :H, :] for r in RTQ]
                P.tt('dve', t1, x1, cs, ALU.mult)
                P.tt('pool', t2, x2, sn, ALU.mult)
                P.tt('dve', o3[:, :, 0:64], t1, t2, ALU.subtract)
                P.tt('pool', t3_, x1, sn, ALU.mult)
                P.tt('dve', t4, x2, cs, ALU.mult)
                P.tt('pool', o3[:, :, 64:128], t3_, t4, ALU.add)
                return out

            pend_c = []
            for tt in range(NT):
                tok = slice(tt * 128, (tt + 1) * 128)
                lat = tt >= 2
                groups = [0, 1, 2] if lat else [2]
                for g in groups:
                    ps = PSF()
                    for kc in range(8):
                        P.mm(ps[:, :], HT1[:, kc, tok], WQ[:, kc, g * 512:(g + 1) * 512], start=(kc == 0), stop=(kc == 7))
                    if g < 2:
                        qn = normrope(ps[:, 0:512], 4, 'qn_g', True, tt - 2)

                        def fin(qn=qn, g=g, tt=tt):
                            pb = PSB()
                            for h in range(4):
                                P.tr(pb[:, h * 128:(h + 1) * 128], qn[:, h * 128:(h + 1) * 128], IDB[:])
                            P.copy(EV(), QT1[:, g * 4:(g + 1) * 4, (tt - 2) * 128:(tt - 1) * 128],
                                   pb[:, 0:512].rearrange("p (a b) -> p a b", a=4))
                    else:
                        P.copy('act', V1[:, tt, :], ps[:, 256:512])
                        kn = normrope(ps[:, 0:256], 2, 'kn_g', lat, tt - 2)

                        def fin(kn=kn, tok=tok):
                            pb = PSB()
                            for h in range(2):
                                P.tr(pb[:, h * 128:(h + 1) * 128], kn[:, h * 128:(h + 1) * 128], IDB[:])
                            P.copy(EV(), KT1[:, :, tok], pb[:, 0:256].rearrange("p (a b) -> p a b", a=2))
                    pend_c.append(fin)
                    while len(pend_c) > 2:
                        pend_c.pop(0)()
            while pend_c:
                pend_c.pop(0)()
            PT1 = [sb('PT1_%d' % i, [128, 512], BF16, ph) for i in range(3)]
            RR = sb('RR', [128, 512], F32, ph)
            ACC1 = [sb('ACC1_%d' % j_, [128, 512], F32, ph) for j_ in range(2)]
            OB = [sb('OB%d' % i, [128, 512], BF16, ph) for i in range(2)]
            cnt = 0
            SCL = 128 ** -0.5
            for kvh in range(2):
                for g4 in range(4):
                    head = kvh * 4 + g4
                    for qs in range(4):
                        a, b = qs * 512, (qs + 1) * 512
                        psO, psD = psf[0], psf[1]
                        def s_mm1(kt):
                            P.mm(psf[2 + (cnt + kt) % 4][:, :], KT1[:, kvh, kt * 128:(kt + 1) * 128], QT1[:, head, a:b])
                        s_mm1(0)
                        s_mm1(1)
                        for kt in range(NT):
                            pS = psf[2 + (cnt + kt) % 4]
                            pt = PT1[(cnt + kt) % 3]
                            if kt + 2 < NT:
                                s_mm1(kt + 2)
                            P.act(pt[:], pS[:, :], AF.Exp, scale=SCL)
                            P.mm(psO[:, :], V1[:, kt, kvh * 128:(kvh + 1) * 128], pt[:], start=(kt == 0), stop=(kt == NT - 1))
                            ai = 1 if kt % 3 == 2 else 0
                            acc = ACC1[ai]
                            aeng = 'pool' if ai else 'dve'
                            if kt == 0 or kt == 2:
                                P.copy(aeng, acc[:], pt[:])
                            else:
                                P.tt(aeng, acc[:], acc[:], pt[:], ALU.add)
                        cnt += NT
                        P.mmf(psD[:, :], ONESF[:], ACC1[0][:], start=True, stop=False)
                        P.mmf(psD[:, :], ONESF[:], ACC1[1][:], start=False, stop=True)
                        P.recip(RR[:], psD[:, :])
                        ob = OB[(head * 4 + qs) % 2]
                        P.tt('dve', ob[:], psO[:, :], RR[:], ALU.mult)
                        P.dma('sp', ym_d[head, :, TC + a:TC + b], ob[:])
            P.barrier()
        if STOP == 7:
            nc._declared_inputs = set(k for k in dram if k in SPEC)
            return nc
        phase_D(1, 'od_w_out', SEGS[1:], list(range(2, NT)), True)

        P.barrier()
        print("ops", P.nops)
    nc._declared_inputs = set(k for k in dram if k in SPEC)
    return nc


_CACHE = {}


def kernel(**inp):
    inp = {k: np.asarray(v) for k, v in inp.items()}
    taps = tuple(inp.pop('_taps', ()))
    ncores = int(inp.pop('_ncores', 8))
    cosb, sinb = _rope_tables(64)
    cosc, sinc = _rope_tables(128)
    ident = np.eye(128, dtype=np.float32)
    bdmask = np.zeros((128, 128), np.float32)
    bdmask[:64, :64] = 1.0
    bdmask[64:, 64:] = 1.0
    tri = np.zeros((4, 128, 128), np.float32)
    for blk in range(2):
        o = blk * 64
        ii, jj = np.meshgrid(np.arange(64), np.arange(64), indexing='ij')
        tri[0, o:o + 64, o:o + 64] = (ii < jj)
        tri[1, o:o + 64, o:o + 64] = (ii <= jj)
        tri[2, o:o + 64, o:o + 64] = (ii > jj)
        tri[3, o:o + 64, o:o + 64] = (ii >= jj)
    masks = np.ascontiguousarray(np.concatenate([tri[0], tri[1], tri[2], tri[3], tri[0], tri[0]], axis=1))
    shared = {
        'ident': ident, 'bdmask': bdmask, 'masks': masks,
        'ada_w': np.ascontiguousarray(inp['ada_w'], np.float32),
        'ev_w_in': np.ascontiguousarray(inp['ev_w_in'][0]), 'ev_w_out': np.ascontiguousarray(inp['ev_w_out'][0]),
        'od_w_in': np.ascontiguousarray(inp['od_w_in'][0]), 'od_w_out': np.ascontiguousarray(inp['od_w_out'][0]),
        'moe_w1': inp['moe_w1'], 'moe_w3': inp['moe_w3'], 'moe_w2': inp['moe_w2'],
        'router_w': inp['router_w'],
        'a_w2': np.ascontiguousarray(inp['ev_a_w2'][0]), 'a_a2': np.ascontiguousarray(inp['ev_a_a2'][0]),
        'a_g2': np.ascontiguousarray(inp['ev_a_g2'][0]),
        'cosb': cosb, 'sinb': sinb, 'cosc': cosc, 'sinc': sinc,
    }
    in_maps = []
    for b in range(ncores):
        sv, rv = _pack_small(inp, b)
        m = dict(shared)
        m['xin'] = np.ascontiguousarray(np.concatenate([inp['ctx'][b], inp['x'][b]], axis=0), np.float32)
        m['sv'] = sv
        m['rv'] = rv
        in_maps.append(m)
    nc = build_program(SV_LAYOUT['_n'], RV_LAYOUT['_n'], taps)
    used = nc._declared_inputs
    in_maps = [{k: v for k, v in m.items() if k in used} for m in in_maps]
    res = run_bass_kernel_spmd(nc, in_maps, core_ids=list(range(ncores)))
    if taps:
        return res
    return np.stack([np.asarray(r['out'], np.float32) for r in res.results], axis=0)
```

```python
import math
from contextlib import ExitStack
import numpy as np
import concourse.bass as bass
import concourse.mybir as mybir
from concourse.bass_utils import run_bass_kernel_spmd

F32 = mybir.dt.float32
BF16 = mybir.dt.bfloat16
AF = mybir.ActivationFunctionType
ALU = mybir.AluOpType
AX = mybir.AxisListType

D = 1024
NCH = 8
TC = 256
TL = 2048
T = TC + TL
NT = T // 128
ALPHA = 4 ** 0.25
LN_EPS = 1e-5
import os
STOP = int(os.environ.get('KSTOP', '0'))
SEGS = [(0, 256)] + [(256 + 512 * i, 256 + 512 * (i + 1)) for i in range(4)]


class Prog:
    def __init__(self, nc, es):
        self.nc = nc
        self.E = {'pe': nc.tensor, 'act': nc.scalar, 'dve': nc.vector, 'pool': nc.gpsimd, 'sp': nc.sync}
        self.NR = 8
        self.sem = {}
        for e in ('pe', 'act', 'dve', 'pool'):
            self.sem['c_' + e] = es.enter_context(nc.semaphore('c_' + e))
        for q in ('sp', 'pool'):
            for i in range(self.NR):
                k = 'd_%s_%d' % (q, i)
                self.sem[k] = es.enter_context(nc.semaphore(k))
        self.val = {k: 0 for k in self.sem}
        self.dn = {'sp': 0, 'pool': 0}
        self.seen = {e: {} for e in self.E}
        self.recs = {}
        self.ro = set()
        self.nops = 0

    @staticmethod
    def box(ap):
        name = ap.tensor.name
        dims = ap.ap
        off = ap.offset
        if 'DRAM' in str(ap.space).upper():
            return name, 0, 1, off, off + sum(s * (c - 1) for s, c in dims) + 1
        if 'PSUM' in str(ap.space).upper():
            return name, 0, 128, 0, 1 << 30
        pst, pn = dims[0]
        if pst <= 0:
            p0, f0 = 0, off
        else:
            p0, f0 = off // pst, off % pst
        f1 = f0 + sum(s * (c - 1) for s, c in dims[1:]) + 1
        return name, p0, p0 + pn, f0, f1

    def _wait(self, eng, key, v):
        if self.seen[eng].get(key, 0) >= v:
            return
        self.E[eng].wait_ge(self.sem[key], v)
        self.seen[eng][key] = v

    def op(self, eng, emit, reads=(), writes=(), dma=False):
        deps = {}

        def need(r):
            (_, _, _, _, w, key, v, reng) = r
            if not dma and reng == eng:
                if eng == 'pe':
                    return
            if deps.get(key, 0) < v:
                deps[key] = v

        rb = []
        wb = []
        for ap in reads:
            b = self.box(ap)
            if b[0] in self.ro:
                continue
            rb.append(b)
            for r in self.recs.get(b[0], ()):
                if r[4] and r[0] < b[2] and b[1] < r[1] and r[2] < b[4] and b[3] < r[3]:
                    need(r)
        for ap in writes:
            b = self.box(ap)
            wb.append(b)
            for r in self.recs.get(b[0], ()):
                if r[0] < b[2] and b[1] < r[1] and r[2] < b[4] and b[3] < r[3]:
                    need(r)
        if dma:
            slot = self.dn[eng] % self.NR
            use = self.dn[eng] // self.NR
            self.dn[eng] += 1
            key = 'd_%s_%d' % (eng, slot)
            if use > 0 and deps.get(key, 0) < 16 * use:
                deps[key] = 16 * use
            val = 16 * (use + 1)
            inc = 16
            reng = 'dma'
        else:
            key = 'c_' + eng
            val = self.val[key] + 1
            inc = 1
            reng = eng
        for k, v in deps.items():
            self._wait(eng, k, v)
        ins = emit(self.E[eng])
        ins.then_inc(self.sem[key], inc)
        self.val[key] = val
        self.nops += 1
        for b in wb:
            lst = self.recs.setdefault(b[0], [])
            lst[:] = [r for r in lst if not (b[1] <= r[0] and r[1] <= b[2] and b[3] <= r[2] and r[3] <= b[4])]
            lst.append((b[1], b[2], b[3], b[4], True, key, val, reng))
        for b in rb:
            lst = self.recs.setdefault(b[0], [])
            lst[:] = [r for r in lst if not ((not r[4]) and r[7] == reng and reng != 'dma'
                                             and r[0] == b[1] and r[1] == b[2] and r[2] == b[3] and r[3] == b[4])]
            lst.append((b[1], b[2], b[3], b[4], False, key, val, reng))
        return ins

    def barrier(self):
        for e in self.E:
            for k, v in self.val.items():
                if v > 0:
                    self._wait(e, k, v)
        self.recs.clear()

    def mm(self, out, lhsT, rhs, start=True, stop=True):
        return self.op('pe', lambda e: e.matmul(out, lhsT, rhs, start=start, stop=stop),
                       reads=[lhsT, rhs], writes=[out])

    def mmf(self, out, lhsT, rhs, start=True, stop=True):
        return self.op('pe', lambda e: e.matmul(out, lhsT, rhs, start=start, stop=stop), reads=[lhsT, rhs], writes=[out])

    def tr(self, out, in_, ident):
        return self.op('pe', lambda e: e.transpose(out, in_, ident), reads=[in_, ident], writes=[out])

    def act(self, out, in_, func, bias=None, scale=1.0, accum_out=None):
        rd = [in_]
        kw = {}
        if bias is not None:
            kw['bias'] = bias
            if not isinstance(bias, (int, float)):
                rd.append(bias)
        if not isinstance(scale, (int, float)):
            rd.append(scale)
        wr = [out]
        if accum_out is not None:
            kw['accum_out'] = accum_out
            wr.append(accum_out)
        return self.op('act', lambda e: e.activation(out, in_, func, scale=scale, **kw), reads=rd, writes=wr)

    def tt(self, eng, out, in0, in1, op):
        return self.op(eng, lambda e: e.tensor_tensor(out, in0, in1, op), reads=[in0, in1], writes=[out])

    def ts(self, eng, out, in0, s1, s2=None, op0=ALU.mult, op1=None):
        rd = [in0] + [s for s in (s1, s2) if s is not None and not isinstance(s, (int, float))]
        if op1 is None:
            return self.op(eng, lambda e: e.tensor_scalar(out, in0, s1, None, op0), reads=rd, writes=[out])
        return self.op(eng, lambda e: e.tensor_scalar(out, in0, s1, s2, op0, op1), reads=rd, writes=[out])

    def stt(self, eng, out, in0, scalar, in1, op0, op1):
        rd = [in0, in1] + ([] if isinstance(scalar, (int, float)) else [scalar])
        return self.op(eng, lambda e: e.scalar_tensor_tensor(out, in0, scalar, in1, op0, op1), reads=rd, writes=[out])

    def copy(self, eng, out, in_):
        if eng == 'act':
            return self.op('act', lambda e: e.copy(out, in_), reads=[in_], writes=[out])
        return self.op(eng, lambda e: e.tensor_copy(out, in_), reads=[in_], writes=[out])

    def memset(self, eng, ap, v):
        return self.op(eng, lambda e: e.memset(ap, v), writes=[ap])

    def reduce(self, eng, out, in_, op, axis=AX.X):
        return self.op(eng, lambda e: e.tensor_reduce(out, in_, axis, op), reads=[in_], writes=[out])

    def recip(self, out, in_):
        return self.op('dve', lambda e: e.reciprocal(out, in_), reads=[in_], writes=[out])

    def dma(self, q, out, in_):
        return self.op(q, lambda e: e.dma_start(out=out, in_=in_), reads=[in_], writes=[out], dma=True)


def _featT(v):
    v = np.asarray(v, np.float32).reshape(-1)
    return np.ascontiguousarray(v.reshape(v.size // 128, 128).T)


class _Cols:
    def __init__(self):
        self.parts = []
        self.off = {}
        self.n = 0

    def add(self, name, arr):
        self.off[name] = self.n
        self.parts.append(arr)
        self.n += arr.shape[1]

    def build(self):
        return np.ascontiguousarray(np.concatenate(self.parts, axis=1))


def _rope_tables(head_dim):
    rows = TL // 64
    rr, cc = np.meshgrid(np.arange(rows), np.arange(64), indexing='ij')
    row_pos = rr.reshape(-1).astype(np.float32)
    col_pos = cc.reshape(-1).astype(np.float32)
    axis_dim = head_dim // 2
    inv = (np.float32(10000.0) ** (-np.arange(0, axis_dim, 2, dtype=np.float32) / axis_dim)).astype(np.float32)
    ang = np.concatenate([row_pos[:, None] * inv, col_pos[:, None] * inv], -1).astype(np.float32)
    return np.cos(ang).astype(np.float32), np.sin(ang).astype(np.float32)


SV_LAYOUT = {}
RV_LAYOUT = {}


def _pack_small(inp, b):
    sv = _Cols()
    sv.add('c', _featT(inp['c'][b]))
    sv.add('cctx', _featT(inp['c_ctx']))
    for i in range(2):
        sv.add('ada_b%d' % i, _featT(inp['ada_b'][i]))
        for nm in ('ln1_g', 'ln1_b', 'ln2_g', 'ln2_b'):
            sv.add('%s%d' % (nm, i), _featT(inp[nm][i]))
    sv.add('mu', _featT(inp['ev_a_mu'][0]))
    for d in range(2):
        sv.add('w0_%d' % d, _featT(inp['ev_a_w0'][0, d]))
        sv.add('a0_%d' % d, _featT(inp['ev_a_a0'][0, d]))
    for nm in ('kk', 'ka', 'lnx_g', 'lnx_b'):
        sv.add(nm, _featT(inp['ev_a_' + nm][0]))
    sv.add('rk', _featT(inp['ev_a_rk'][0]))
    sv.add('subln_g', _featT(inp['ev_b_subln_g'][0]))
    rv = _Cols()
    rv.add('lam', np.asarray(inp['ev_b_lam'][0], np.float32).reshape(1, 256))
    rv.add('router_b', np.asarray(inp['router_b'], np.float32).reshape(1, 16))
    rv.add('qn_g', np.asarray(inp['od_qn_g'][0], np.float32).reshape(1, 128))
    rv.add('kn_g', np.asarray(inp['od_kn_g'][0], np.float32).reshape(1, 128))
    SV_LAYOUT.update(sv.off)
    SV_LAYOUT['_n'] = sv.n
    RV_LAYOUT.update(rv.off)
    RV_LAYOUT['_n'] = rv.n
    return sv.build(), rv.build()


def build_program(nsv, nrv, taps=()):
    nc = bass.Bass("TRN2", target_bir_lowering=False)
    dram = {}

    def din(name, shape, dt=F32):
        dram[name] = nc.dram_tensor(name, list(shape), dt, kind="ExternalInput").ap()
        return dram[name]

    def dscr(name, shape, dt=F32):
        kind = "ExternalOutput" if name in taps else "Internal"
        dram[name] = nc.dram_tensor(name, list(shape), dt, kind=kind).ap()
        return dram[name]

    SPEC = {
        'xin': [T, D], 'sv': [128, nsv], 'rv': [1, nrv], 'ident': [128, 128],
        'ada_w': [2, D, 6 * D], 'ev_w_in': [D, 3456], 'ev_w_out': [D, D],
        'od_w_in': [D, 1536], 'od_w_out': [D, D],
        'moe_w1': [2, 16, D, 512], 'moe_w3': [2, 16, D, 512], 'moe_w2': [2, 16, 512, D],
        'router_w': [D, 16], 'a_w2': [2, 64, 512], 'a_a2': [2, 64, 512], 'a_g2': [128, 512],
        'cosb': [TL, 32], 'sinb': [TL, 32], 'cosc': [TL, 64], 'sinc': [TL, 64], 'bdmask': [128, 128],
        'masks': [128, 6 * 128],
    }

    def W(name):
        if name not in dram:
            din(name, SPEC[name])
        return dram[name]

    xin = W('xin')
    sv_d = W('sv')
    rv_d = W('rv')
    ident_d = W('ident')
    ada_w = W('ada_w')
    bdmask_d = W('bdmask')
    out_d = nc.dram_tensor('out', [TL, D], F32, kind="ExternalOutput").ap()

    xt_d = dscr('xt_d', [128, NCH, T])
    ua_d = dscr('ua_d', [15, 128, T], BF16)
    ym_d = dscr('ym_d', [8, 128, T], BF16)

    SVO = SV_LAYOUT
    RVO = RV_LAYOUT

    with ExitStack() as es:
        es.enter_context(nc.allow_low_precision("bf16 matmul operands, fp32 accumulation"))
        P = Prog(nc, es)
        P.ro.update(SPEC.keys())

        def sb(name, shape, dt=F32, stack=es):
            return stack.enter_context(nc.sbuf_tensor(name, list(shape), dt))

        psf = [es.enter_context(nc.psum_tensor('psf%d' % i, [128, 512], F32)) for i in range(6)]
        psb = [es.enter_context(nc.psum_tensor('psb%d' % i, [128, 1024], BF16)) for i in range(2)]
        rot = {'f': 0, 'b': 0, 'e': 0}

        def PSF():
            rot['f'] += 1
            return psf[rot['f'] % 6]

        def PSB():
            rot['b'] += 1
            return psb[rot['b'] % 2]

        def EV():
            rot['e'] += 1
            return 'dve' if rot['e'] % 2 else 'act'

        SV = sb('SV', [128, nsv])
        RV = sb('RV', [128, nrv])
        IDF = sb('IDF', [128, 128])
        IDB = sb('IDB', [128, 128], BF16)
        ONESB = sb('ONESB', [128, 128], BF16)
        ONESF = sb('ONESF', [128, 128])
        BDM = sb('BDM', [128, 128])
        BDMB = sb('BDMB', [128, 128], BF16)
        MOD = sb('MOD', [128, 2, 48, 2])
        es_ht = ExitStack()
        HT = sb('HT', [128, NCH, T], BF16, es_ht)
        P.dma('sp', SV[:], sv_d[:, :])
        P.dma('sp', RV[:], rv_d.partition_broadcast(128))
        P.dma('sp', IDF[:], ident_d[:, :])
        P.copy('dve', IDB[:], IDF[:])
        P.dma('sp', BDM[:], bdmask_d[:, :])
        P.copy('dve', BDMB[:], BDM[:])
        P.memset('dve', ONESB[:], 1.0)
        P.memset('dve', ONESF[:], 1.0)

        def svc(name, j=0, n=1):
            o = SVO[name] + j
            return SV[:, o:o + n]

        def modc(i, m, c, w):
            return MOD[:, i, m * 8 + c, w:w + 1]

        with ExitStack() as ph:
            XT = sb('XT', [128, NCH, T], F32, ph)
            XS = [sb('XS%d' % i, [128, D], F32, ph) for i in range(2)]
            for tt in range(NT):
                xs = XS[tt % 2]
                P.dma('sp', xs[:], xin[tt * 128:(tt + 1) * 128, :])
                for hh in range(2):
                    ps = PSF()
                    for j in range(4):
                        c = hh * 4 + j
                        P.tr(ps[:, j * 128:(j + 1) * 128], xs[:, c * 128:(c + 1) * 128], IDF[:])
                    P.copy(EV(), XT[:, hh * 4:hh * 4 + 4, tt * 128:(tt + 1) * 128],
                           ps[:, :].rearrange("p (a b) -> p a b", a=4))
            for c in range(NCH):
                P.dma('sp', xt_d[:, c, :], XT[:, c, :])
            if STOP == 1:
                P.barrier(); nc._declared_inputs = set(k for k in dram if k in SPEC); return nc
            ST = sb('ST', [128, 8, 2], BF16, ph)
            P.act(ST[:, :, 0], svc('c', 0, 8), AF.Silu)
            P.act(ST[:, :, 1], svc('cctx', 0, 8), AF.Silu)
            AWF = [sb('AWF%d' % i, [128, 8, 512], F32, ph) for i in range(2)]
            AWB = [sb('AWB%d' % i, [128, 8, 512], BF16, ph) for i in range(2)]
            for i in range(2):
                for pc in range(12):
                    awf = AWF[(i * 12 + pc) % 2]
                    aw = AWB[(i * 12 + pc) % 2]
                    for kc in range(8):
                        P.dma('sp', awf[:, kc, :], ada_w[i, kc * 128:(kc + 1) * 128, pc * 512:(pc + 1) * 512])
                    P.copy('pool', aw[:, 0:4, :], awf[:, 0:4, :])
                    P.copy('act', aw[:, 4:8, :], awf[:, 4:8, :])
                    ps = PSF()
                    for j in range(4):
                        for kc in range(8):
                            P.mm(ps[:, 16 * j:16 * j + 2], aw[:, kc, j * 128:(j + 1) * 128], ST[:, kc, :],
                                 start=(kc == 0), stop=(kc == 7))
                    for j in range(4):
                        P.ts('dve', MOD[:, i, pc * 4 + j, :], ps[:, 16 * j:16 * j + 2],
                             svc('ada_b%d' % i, pc * 4 + j), None, ALU.add)
                for m in (1, 4):
                    P.ts('dve', MOD[:, i, m * 8:(m + 1) * 8, :], MOD[:, i, m * 8:(m + 1) * 8, :], 1.0, None, ALU.add)
                for m in (2, 5):
                    P.ts('dve', MOD[:, i, m * 8:(m + 1) * 8, :], MOD[:, i, m * 8:(m + 1) * 8, :], 1.0 / ALPHA, None, ALU.mult)
            if 'mod_tap' in taps:
                mdt = dscr('mod_tap', [128, 192])
                P.dma('sp', mdt[:, :], MOD[:].rearrange('p a b c -> p (a b c)'))
            if STOP == 2:
                P.barrier(); nc._declared_inputs = set(k for k in dram if k in SPEC); return nc
            for c in range(NCH):
                P.ts('dve', HT[:, c, 0:TC], XT[:, c, 0:TC], modc(0, 1, c, 1), modc(0, 0, c, 1), ALU.mult, ALU.add)
                P.ts('pool' if c % 2 else 'dve', HT[:, c, TC:T], XT[:, c, TC:T], modc(0, 1, c, 0), modc(0, 0, c, 0),
                     ALU.mult, ALU.add)
            if 'ht_tap' in taps:
                htt = dscr('ht_tap', [128, NCH, T], BF16)
                for c in range(NCH):
                    P.dma('sp', htt[:, c, :], HT[:, c, :])
            P.barrier()


        def rvc(name, j=0, n=1):
            o = RVO[name] + j
            return RV[:, o:o + n]

        def CV():
            rot['c'] = rot.get('c', 0) + 1
            return ('pool', 'act', 'dve')[rot['c'] % 3]

        def load_w(dst, src, rows_kc, c0, c1, stg):
            for kc in range(rows_kc):
                st = stg[kc % 2]
                P.dma('sp', st[:, 0:c1 - c0], src[kc * 128:(kc + 1) * 128, c0:c1])
                P.copy(CV(), dst[:, kc, :], st[:, 0:c1 - c0])

        ev_w_in = W('ev_w_in')
        with ExitStack() as ph:
            WA = sb('WA', [128, 8, 1920], BF16, ph)
            STG = [sb('STGa%d' % i, [128, 1920], F32, ph) for i in range(2)]
            load_w(WA, ev_w_in, 8, 0, 1920, STG)
            OMU = sb('OMU', [128, 15], F32, ph)
            HMU = sb('HMU', [128, 15], F32, ph)
            P.ts('dve', OMU[:], svc('mu', 0, 15), -1.0, 1.0, ALU.mult, ALU.add)
            P.ts('dve', HMU[:], svc('mu', 0, 15), 0.5, None, ALU.mult)
            PP = [sb('PP%d' % i, [128, 2312], F32, ph) for i in range(2)]
            P.memset('dve', PP[0][:], 0.0)
            P.memset('pool', PP[1][:], 0.0)
            T1 = sb('T1', [128, TL], F32, ph)
            T2 = sb('T2', [128, TL], F32, ph)
            UB = [sb('UB%d' % i, [128, T], BF16, ph) for i in range(2)]
            CO, LO = 1, 260
            for f in range(15):
                pp = PP[f % 2]
                for (a, b) in SEGS:
                    n = b - a
                    ps = PSF()
                    for kc in range(8):
                        P.mm(ps[:, :n], WA[:, kc, f * 128:(f + 1) * 128], HT[:, kc, a:b], start=(kc == 0), stop=(kc == 7))
                    off = CO + a if a < TC else LO + (a - TC)
                    P.copy(EV(), pp[:, off:off + n], ps[:, :n])
                ub = UB[f % 2]
                for (lo, n, t0) in ((CO, TC, 0), (LO, TL, TC)):
                    P.tt('pool', T1[:, :n], pp[:, lo - 1:lo - 1 + n], pp[:, lo + 1:lo + 1 + n], ALU.add)
                    P.ts('dve', T2[:, :n], pp[:, lo:lo + n], OMU[:, f:f + 1], None, ALU.mult)
                    P.stt('dve', T2[:, :n], T1[:, :n], HMU[:, f:f + 1], T2[:, :n], ALU.mult, ALU.add)
                    func = AF.Tanh if f == 12 else (AF.Sigmoid if f == 14 else AF.Identity)
                    P.act(ub[:, t0:t0 + n], T2[:, :n], func)
                P.dma('sp', ua_d[f, :, :], ub[:])
            P.barrier()
        if STOP == 3:
            es_ht.close()
            nc._declared_inputs = set(k for k in dram if k in SPEC)
            return nc

        LAMBDA_INIT0 = 0.8 - 0.6 * math.exp(0.0)
        with ExitStack() as ph:
            WB = sb('WB', [128, 8, 1536], BF16, ph)
            STG = [sb('STGb%d' % i, [128, 1536], F32, ph) for i in range(2)]
            load_w(WB, ev_w_in, 8, 1920, 3456, STG)
            CB = sb('CB', [128, 16, 32], F32, ph)
            SNB = sb('SNB', [128, 16, 32], F32, ph)
            P.dma('sp', CB[:], W('cosb').rearrange("(n p) f -> p n f", p=128))
            P.dma('sp', SNB[:], W('sinb').rearrange("(n p) f -> p n f", p=128))
            VB = sb('VB', [128, NT, 512], BF16, ph)
            QT = sb('QT', [128, 4, T], BF16, ph)
            KT = sb('KT', [128, 4, T], BF16, ph)
            QR = [sb('QR%d' % i, [128, 512], BF16, ph) for i in range(5)]
            RTMS = [[sb('RTM%d_%d' % (j, i), [128, 8, 32], F32, ph) for i in range(4)] for j in range(3)]
            qi = [0]

            def proj_b(tt, groups):
                tok = slice(tt * 128, (tt + 1) * 128)
                for g in groups:
                    ps = PSF()
                    for kc in range(8):
                        P.mm(ps[:, :], HT[:, kc, tok], WB[:, kc, g * 512:(g + 1) * 512], start=(kc == 0), stop=(kc == 7))
                    if g == 2:
                        P.copy(EV(), VB[:, tt, :], ps[:, :])
                        continue
                    qr = QR[qi[0] % 5]
                    qi[0] += 1
                    if tt < 2:
                        P.copy(EV(), qr[:], ps[:, :])
                    else:
                        v4 = ps[:, :].rearrange("p (g two d) -> p g two d", g=8, two=2)
                        o4 = qr[:].rearrange("p (g two d) -> p g two d", g=8, two=2)
                        x1, x2 = v4[:, :, 0, :], v4[:, :, 1, :]
                        cs = CB[:, tt - 2, :].unsqueeze(1).broadcast_to([128, 8, 32])
                        sn = SNB[:, tt - 2, :].unsqueeze(1).broadcast_to([128, 8, 32])
                        t1, t2, t3, t4 = [r[:] for r in RTMS[qi[0] % 3]]
                        P.tt('dve', t1, x1, cs, ALU.mult)
                        P.tt('dve', t2, x2, sn, ALU.mult)
                        P.tt('pool', o4[:, :, 0, :], t1, t2, ALU.subtract)
                        P.tt('dve', t3, x1, sn, ALU.mult)
                        P.tt('dve', t4, x2, cs, ALU.mult)
                        P.tt('pool', o4[:, :, 1, :], t3, t4, ALU.add)
                    def fin(qr=qr, g=g, tok=tok):
                        pb = PSB()
                        for h in range(4):
                            P.tr(pb[:, h * 128:(h + 1) * 128], qr[:, h * 128:(h + 1) * 128], IDB[:])
                        dst = QT if g == 0 else KT
                        P.copy(EV(), dst[:, :, tok], pb[:, 0:512].rearrange("p (a b) -> p a b", a=4))
                    pend_b.append(fin)
                    while len(pend_b) > 2:
                        pend_b.pop(0)()

            def flush_b():
                while pend_b:
                    pend_b.pop(0)()

            pend_b = []
            for tt in range(NT):
                proj_b(tt, [1, 2])
            flush_b()
            if 'qt_tap' in taps:
                for nm, src in (('qt_tap', QT), ('kt_tap', KT)):
                    tp = dscr(nm, [128, 4, T], BF16)
                    for h in range(4):
                        P.dma('sp', tp[:, h, :], src[:, h, :])
            LT = sb('LT', [128, 128], F32, ph)
            LS = sb('LS', [128, 2], F32, ph)
            NL = sb('NL', [128, 1], F32, ph)
            SG = sb('SG', [128, 1], F32, ph)
            P.tt('dve', LT[:, 0:64], rvc('lam', 0, 64), rvc('lam', 64, 64), ALU.mult)
            P.tt('dve', LT[:, 64:128], rvc('lam', 128, 64), rvc('lam', 192, 64), ALU.mult)
            P.reduce('dve', LS[:, 0:2], LT[:].rearrange("p (a b) -> p a b", a=2), ALU.add)
            P.act(LS[:, 0:2], LS[:, 0:2], AF.Exp)
            P.tt('dve', NL[:], LS[:, 1:2], LS[:, 0:1], ALU.subtract)
            P.ts('dve', NL[:], NL[:], -LAMBDA_INIT0, None, ALU.add)
            P.ts('dve', SG[:], svc('subln_g'), 1.0 - LAMBDA_INIT0, None, ALU.mult)
            PT = [sb('PT%d' % i, [128, 512], BF16, ph) for i in range(3)]
            R1 = sb('R1', [128, 512], F32, ph)
            R2 = sb('R2', [128, 512], F32, ph)
            O1 = sb('O1', [128, 512], F32, ph)
            O2 = sb('O2', [128, 512], F32, ph)
            SQ = sb('SQ', [128, 512], F32, ph)
            YB = [sb('YB%d' % i, [128, 512], BF16, ph) for i in range(2)]
            ACC = [[sb('ACC%d%d' % (m_, j_), [128, 512], F32, ph) for j_ in range(2)] for m_ in range(2)]
            cnt = 0
            yi = 0
            def q_seg(si):
                a, b = SEGS[si]
                for tt in range(a // 128, b // 128):
                    proj_b(tt, [0])
                flush_b()

            q_seg(0)
            q_seg(1)
            for si, (a, b) in enumerate(SEGS):
                if si + 2 < len(SEGS):
                    q_seg(si + 2)
                for h in range(4):
                    n = b - a
                    kts = list(range(2)) if a < TC else list(range(NT))
                    psO = [psf[0], psf[1]]
                    psD = [psf[2], psf[3]]
                    for m in range(2):
                        def s_mm(i):
                            kt = kts[i]
                            P.mm(psf[4 + (cnt + i) % 2][:, :n], KT[m * 64:(m + 1) * 64, h, kt * 128:(kt + 1) * 128],
                                 QT[m * 64:(m + 1) * 64, h, a:b])
                        s_mm(0)
                        for i, kt in enumerate(kts):
                            pS = psf[4 + (cnt + i) % 2]
                            pt = PT[(cnt + i) % 3]
                            if i + 1 < len(kts):
                                s_mm(i + 1)
                            P.act(pt[:, :n], pS[:, :n], AF.Exp, scale=0.125)
                            P.mm(psO[m][:, :n], VB[:, kt, h * 128:(h + 1) * 128], pt[:, :n],
                                 start=(i == 0), stop=(i == len(kts) - 1))
                            ai = 1 if i % 3 == 2 else 0
                            acc = ACC[m][ai]
                            aeng = 'pool' if ai else 'dve'
                            if i == 0 or i == 2:
                                P.copy(aeng, acc[:, :n], pt[:, :n])
                            else:
                                P.tt(aeng, acc[:, :n], acc[:, :n], pt[:, :n], ALU.add)
                        cnt += len(kts)
                        if len(kts) > 2:
                            P.mmf(psD[m][:, :n], ONESF[:], ACC[m][0][:, :n], start=True, stop=False)
                            P.mmf(psD[m][:, :n], ONESF[:], ACC[m][1][:, :n], start=False, stop=True)
                        else:
                            P.mmf(psD[m][:, :n], ONESF[:], ACC[m][0][:, :n], start=True, stop=True)
                    P.recip(R1[:, :n], psD[0][:, :n])
                    P.recip(R2[:, :n], psD[1][:, :n])
                    P.tt('dve', O1[:, :n], psO[0][:, :n], R1[:, :n], ALU.mult)
                    P.tt('dve', O2[:, :n], psO[1][:, :n], R2[:, :n], ALU.mult)
                    P.stt('dve', O1[:, :n], O2[:, :n], NL[:, 0:1], O1[:, :n], ALU.mult, ALU.add)
                    P.act(SQ[:, :n], O1[:, :n], AF.Square)
                    pq = psf[4 + cnt % 2]
                    cnt += 1
                    P.mmf(pq[:, :n], ONESF[:], SQ[:, :n])
                    P.act(R1[:, :n], pq[:, :n], AF.Sqrt, bias=1e-5, scale=1.0 / 128)
                    P.recip(R1[:, :n], R1[:, :n])
                    P.tt('dve', O1[:, :n], O1[:, :n], R1[:, :n], ALU.mult)
                    yb = YB[yi % 2]
                    yi += 1
                    P.ts('dve', yb[:, :n], O1[:, :n], SG[:, 0:1], None, ALU.mult)
                    P.dma('sp', ym_d[4 + h, :, a:b], yb[:, :n])
            P.barrier()
        if STOP == 4:
            es_ht.close()
            nc._declared_inputs = set(k for k in dram if k in SPEC)
            return nc


        es_ht.close()
        with ExitStack() as ph:
            C = 64
            NCK = T // C
            MSK = sb('MSK', [128, 4 * 128], F32, ph)
            P.dma('sp', MSK[:], W('masks')[:, 0:512])
            MK4 = [sb('MK4_%d' % i, [128, 512], F32, ph) for i in range(2)]
            for d, (m1, m2) in enumerate(((0, 1), (2, 3))):
                for q in range(4):
                    mm_ = m1 if q % 2 == 0 else m2
                    P.copy('dve', MK4[d][:, q * 128:(q + 1) * 128], MSK[:, mm_ * 128:(mm_ + 1) * 128])
            MKA = [MSK[:, 256:384], MSK[:, 0:128]]
            LW = sb('LW', [128, T], F32, ph)
            W2B = sb('W2B', [128, 512], BF16, ph)
            A2B = sb('A2B', [128, 512], BF16, ph)
            G2B = sb('G2B', [128, 512], BF16, ph)
            for dst, src in ((W2B, W('a_w2').rearrange("d l c -> (d l) c")), (A2B, W('a_a2').rearrange("d l c -> (d l) c")),
                             (G2B, W('a_g2'))):
                P.dma('sp', LW[:, 0:512], src)
                P.copy('dve', dst[:], LW[:, 0:512])
            OMKA = sb('OMKA', [128, 4], F32, ph)
            P.ts('dve', OMKA[:], svc('ka', 0, 4), -1.0, 1.0, ALU.mult, ALU.add)
            LORA = sb('LORA', [128, 3, T], BF16, ph)
            for i in range(3):
                P.dma('sp', LORA[:, i, :], ua_d[12 + i, :, :])
            RKV = sb('RKV', [128, 3, T], BF16, ph)
            KKt = sb('KKt', [128, T], BF16, ph)
            Ad = sb('Ad', [128, T], BF16, ph)
            LA = sb('LA', [128, T], F32, ph)
            LB = sb('LB', [128, T], F32, ph)
            PRs = [sb('PR%d' % i, [128, 6, T], BF16, ph) for i in range(2)]
            VBD = sb('VBD', [128, NCK, 128], BF16, ph)
            YACC = sb('YACC', [128, T], F32, ph)
            KDS = sb('KDS', [128, T], BF16, ph)
            LCts = [sb('LCt%d' % i, [128, NCK], F32, ph) for i in range(2)]
            GCs = [sb('GC%d' % i, [128, NCK], F32, ph) for i in range(2)]
            HFs = [sb('HF%d' % i, [128, 128], F32, ph) for i in range(2)]
            HBs = [sb('HB%d' % i, [128, 128], BF16, ph) for i in range(2)]
            TS = [sb('TSg%d' % i, [128, 512], F32, ph) for i in range(4)]
            G = int(os.environ.get('KG', '3'))
            NS = 2 * G
            XBD = [sb('XBD%d' % i, [128, 6, 128], BF16, ph) for i in range(NS)]
            W4 = [sb('W4_%d' % i, [128, 512], BF16, ph) for i in range(NS)]
            NAb = [sb('NAb%d' % i, [128, 256], BF16, ph) for i in range(2 * NS)]
            PQb = [sb('PQb%d' % i, [128, 256], BF16, ph) for i in range(2 * NS)]
            TOK = [sb('TOK%d' % i, [128, 3, 128], BF16, ph) for i in range(NS)]
            ZS = [sb('ZS%d' % i, [128, 128], BF16, ph) for i in range(NS)]
            US = [sb('US%d' % i, [128, 128], BF16, ph) for i in range(NS)]
            YOUT = [sb('YOUT%d' % i, [128, 512], BF16, ph) for i in range(2)]
            bdm3 = BDMB[:].rearrange("p (a b) -> p a b", a=2)
            CEXP = -math.exp(-0.5)
            v3 = lambda t_: t_[:].rearrange("p (c j) -> p c j", j=C)

            for hp in range(4):
                for i in range(3):
                    P.dma('sp', RKV[:, i, :], ua_d[4 * i + hp, :, :])
                r_, k_, v_ = RKV[:, 0, :], RKV[:, 1, :], RKV[:, 2, :]
                hc = slice(hp * 128, (hp + 1) * 128)
                for (a, b) in SEGS:
                    n = b - a
                    kx, sq, rn = TS[0], TS[1], TS[2]
                    P.ts('dve', kx[:, :n], k_[:, a:b], svc('kk', hp), None, ALU.mult)
                    P.act(sq[:, :n], kx[:, :n], AF.Square)
                    ps = PSF()
                    P.mmf(ps[:, :n], BDM[:], sq[:, :n])
                    P.act(rn[:, :n], ps[:, :n], AF.Sqrt, bias=1e-24)
                    P.recip(rn[:, :n], rn[:, :n])
                    P.tt('dve', KKt[:, a:b], kx[:, :n], rn[:, :n], ALU.mult)
                P.tt('pool', VBD[:].rearrange("p c (a b) -> p c a b", a=2),
                     v_.rearrange("p (c j) -> p c j", j=C).unsqueeze(2).broadcast_to([128, NCK, 2, C]),
                     bdm3.unsqueeze(1).broadcast_to([128, NCK, 2, C]), ALU.mult)
                P.memset('pool', YACC[:], 0.0)
                for d in range(2):
                    PR, LCt, GC = PRs[d], LCts[d], GCs[d]
                    dsl = slice(d * 64, (d + 1) * 64)
                    for (a, b) in SEGS:
                        n = b - a
                        ps = PSF()
                        P.mm(ps[:, :n], W2B[dsl, hc], LORA[dsl, 0, a:b])
                        P.act(TS[0][:, :n], ps[:, :n], AF.Sigmoid, bias=svc('w0_%d' % d, hp))
                        P.ts('pool', LW[:, a:b], TS[0][:, :n], CEXP, None, ALU.mult)
                        ps = PSF()
                        P.mm(ps[:, :n], A2B[dsl, hc], LORA[dsl, 1, a:b])
                        P.act(Ad[:, a:b], ps[:, :n], AF.Sigmoid, bias=svc('a0_%d' % d, hp))
                    seq = [(LW, LA), (LA, LB), (LB, LA), (LA, LB), (LB, LA), (LA, LB)]
                    for si, (src, dst) in enumerate(seq):
                        sft = 1 << si
                        s3, d3 = v3(src), v3(dst)
                        if d == 0:
                            P.tt('dve', d3[:, :, sft:], s3[:, :, sft:], s3[:, :, :C - sft], ALU.add)
                            P.copy('pool', d3[:, :, :sft], s3[:, :, :sft])
                        else:
                            P.tt('dve', d3[:, :, :C - sft], s3[:, :, :C - sft], s3[:, :, sft:], ALU.add)
                            P.copy('pool', d3[:, :, C - sft:], s3[:, :, C - sft:])
                    L3 = v3(LB)
                    P.tt('dve', LA[:], LB[:], LW[:], ALU.subtract)
                    P.copy('dve', LCt[:], L3[:, :, C - 1] if d == 0 else L3[:, :, 0])
                    P.act(GC[:], LCt[:], AF.Exp)
                    for (a, b) in SEGS:
                        n = b - a
                        c0, c1 = a // C, b // C
                        e, ba, kd, tq = TS[0], TS[1], TS[2], TS[3]
                        P.act(e[:, :n], LB[:, a:b], AF.Exp)
                        P.tt('dve', PR[:, 1, a:b], r_[:, a:b], e[:, :n], ALU.mult)
                        P.act(e[:, :n], LA[:, a:b], AF.Exp)
                        P.stt('dve', PR[:, 0, a:b], KKt[:, a:b], -1.0, e[:, :n], ALU.mult, ALU.mult)
                        P.tt('pool', ba[:, :n], KKt[:, a:b], Ad[:, a:b], ALU.mult)
                        P.ts('dve', tq[:, :n], Ad[:, a:b], svc('ka', hp), OMKA[:, hp:hp + 1], ALU.mult, ALU.add)
                        P.tt('dve', kd[:, :n], tq[:, :n], k_[:, a:b], ALU.mult)
                        if d == 0:
                            P.copy('pool', KDS[:, a:b], kd[:, :n])
                        else:
                            P.tt('pool', KDS[:, a:b], KDS[:, a:b], kd[:, :n], ALU.add)
                        P.act(e[:, :n], LB[:, a:b], AF.Exp, scale=-1.0)
                        P.tt('dve', PR[:, 2, a:b], ba[:, :n], e[:, :n], ALU.mult)
                        P.tt('pool', PR[:, 3, a:b], kd[:, :n], e[:, :n], ALU.mult)
                        P.tt('dve', tq[:, :n].rearrange("p (c j) -> p c j", j=C),
                             LCt[:, c0:c1].unsqueeze(2).broadcast_to([128, c1 - c0, C]),
                             LB[:, a:b].rearrange("p (c j) -> p c j", j=C), ALU.subtract)
                        P.act(e[:, :n], tq[:, :n], AF.Exp)
                        P.tt('dve', PR[:, 4, a:b], ba[:, :n], e[:, :n], ALU.mult)
                        P.tt('pool', PR[:, 5, a:b], kd[:, :n], e[:, :n], ALU.mult)
                    P.memset('dve', HFs[d][:], 0.0)
                    P.memset('pool', HBs[d][:], 0.0)

                seq_pos = [0, 0]

                freef = list(psf)
                freeb = list(psb)

                def unit(d, pos, c):
                    bi = d * G + pos % G
                    PR, GC, HF, HB = PRs[d], GCs[d], HFs[d], HBs[d]
                    cs = slice(c * C, (c + 1) * C)
                    xbd, w4, tok, zs, us = XBD[bi], W4[bi], TOK[bi], ZS[bi], US[bi]
                    P.tt('dve' if d == 0 else 'pool', xbd[:].rearrange("p s (a b) -> p s a b", a=2),
                         PR[:, :, cs].unsqueeze(2).broadcast_to([128, 6, 2, C]),
                         bdm3.unsqueeze(1).broadcast_to([128, 6, 2, C]), ALU.mult)
                    yield
                    AtBD, RtBD, BtBD, KtBD, BhBD, KhBD = [xbd[:, i, :] for i in range(6)]
                    AR = xbd[:, 0:2, :].rearrange("p s f -> p (s f)")
                    while len(freef) < 2 or len(freeb) < 1:
                        yield
                    ps1, ps2, pb = freef.pop(0), freef.pop(0), freeb.pop(0)
                    P.mm(ps1[:, 0:256], BtBD, AR)
                    P.mm(ps1[:, 256:512], KtBD, AR)
                    P.mm(ps2[:, 0:128], AtBD, BtBD)
                    P.tr(pb[:, 0:128], VBD[:, c, :], IDB[:])
                    P.tr(pb[:, 128:256], BhBD, IDB[:])
                    P.tr(pb[:, 256:384], KhBD, IDB[:])
                    yield
                    na = NAb[2 * bi]
                    pq = PQb[2 * bi]
                    P.tt('dve', w4[:], ps1[:, :], MK4[d][:], ALU.mult)
                    P.tt('dve', na[:, 128:256], ps2[:, 0:128], MKA[d], ALU.mult)
                    P.copy('act', tok[:].rearrange("p s f -> p (s f)"), pb[:, 0:384])
                    freef.extend([ps1, ps2])
                    freeb.append(pb)
                    P.copy('act', na[:, 0:128], w4[:, 0:128])
                    P.tt('pool', pq[:].rearrange("p (a b) -> p a b", a=2), na[:].rearrange("p (a b) -> p a b", a=2),
                         IDB[:].unsqueeze(1).broadcast_to([128, 2, 128]), ALU.add)
                    for lv in range(5):
                        na2 = NAb[2 * bi + (lv + 1) % 2]
                        pq2 = PQb[2 * bi + (lv + 1) % 2]
                        while len(freef) < 1:
                            yield
                        psn = freef.pop(0)
                        P.mm(psn[:, 0:128], na[:, 128:256], na[:, 0:128])
                        P.mm(psn[:, 128:256], na[:, 0:128], na[:, 128:256])
                        yield
                        P.copy('act', na2[:], psn[:, 0:256])
                        freef.append(psn)
                        while len(freef) < 1:
                            yield
                        psp = freef.pop(0)
                        P.mm(psp[:, 0:128], pq[:, 128:256], na2[:, 0:128])
                        P.mm(psp[:, 128:256], na2[:, 0:128], pq[:, 128:256])
                        yield
                        P.tt('dve', pq2[:], psp[:, 0:256], pq[:], ALU.add)
                        freef.append(psp)
                        na, pq = na2, pq2
                    VtBD, BhT, KhT = tok[:, 0, :], tok[:, 1, :], tok[:, 2, :]
                    while seq_pos[d] != pos or len(freef) < 1:
                        yield
                    psz = freef.pop(0)
                    P.mm(psz[:, 0:128], AtBD, HB[:], start=True, stop=False)
                    P.mm(psz[:, 0:128], w4[:, 256:384], VtBD, start=False, stop=True)
                    yield
                    P.copy('act', zs[:], psz[:, 0:128])
                    freef.append(psz)
                    while len(freef) < 1:
                        yield
                    psu = freef.pop(0)
                    P.mm(psu[:, 0:128], pq[:, 0:128], zs[:])
                    yield
                    P.copy('act', us[:], psu[:, 0:128])
                    freef.append(psu)
                    while len(freef) < 2:
                        yield
                    psh, psy = freef.pop(0), freef.pop(0)
                    P.mm(psh[:, 0:128], BhT, us[:], start=True, stop=False)
                    P.mm(psh[:, 0:128], KhT, VtBD, start=False, stop=True)
                    P.mm(psy[:, 0:128], HB[:], RtBD, start=True, stop=False)
                    P.mm(psy[:, 0:128], us[:], w4[:, 128:256], start=False, stop=False)
                    P.mm(psy[:, 0:128], VtBD, w4[:, 384:512], start=False, stop=True)
                    yield
                    P.stt('dve', HF[:], HF[:], GC[:, c:c + 1], psh[:, 0:128], ALU.mult, ALU.add)
                    P.copy('act', HB[:], HF[:])
                    for hh in range(2):
                        rs = slice(hh * 64, (hh + 1) * 64)
                        P.tt('dve', YACC[rs, cs], YACC[rs, cs], psy[rs, hh * 64:(hh + 1) * 64], ALU.add)
                    freef.extend([psh, psy])
                    seq_pos[d] += 1

                orders = [list(range(NCK)), [3, 2, 1, 0] + list(range(NCK - 1, 3, -1))]
                nxt = [0, 0]
                active = []
                while active or nxt[0] < NCK or nxt[1] < NCK:
                    for d in range(2):
                        while sum(1 for (dd, _) in active if dd == d) < G and nxt[d] < NCK:
                            active.append((d, unit(d, nxt[d], orders[d][nxt[d]])))
                            nxt[d] += 1
                    for item in list(active):
                        try:
                            next(item[1])
                        except StopIteration:
                            active.remove(item)
                for (a, b) in SEGS:
                    n = b - a
                    psg = PSF()
                    P.mm(psg[:, :n], G2B[:, hc], LORA[:, 2, a:b])
                    psm = PSF()
                    P.mmf(psm[:, :n], BDM[:], YACC[:, a:b])
                    sq, mu, t3, pr = TS[0], TS[1], TS[2], TS[3]
                    P.act(sq[:, :n], YACC[:, a:b], AF.Square)
                    psq = PSF()
                    P.mmf(psq[:, :n], BDM[:], sq[:, :n])
                    P.ts('dve', mu[:, :n], psm[:, :n], 1.0 / 64, None, ALU.mult)
                    P.tt('dve', t3[:, :n], mu[:, :n], mu[:, :n], ALU.mult)
                    P.stt('dve', t3[:, :n], psq[:, :n], 1.0 / 64, t3[:, :n], ALU.mult, ALU.subtract)
                    P.act(t3[:, :n], t3[:, :n], AF.Sqrt, bias=64e-5)
                    P.recip(t3[:, :n], t3[:, :n])
                    P.tt('dve', sq[:, :n], YACC[:, a:b], mu[:, :n], ALU.subtract)
                    P.tt('dve', sq[:, :n], sq[:, :n], t3[:, :n], ALU.mult)
                    P.ts('dve', sq[:, :n], sq[:, :n], svc('lnx_g', hp), svc('lnx_b', hp), ALU.mult, ALU.add)
                    P.stt('dve', pr[:, :n], r_[:, a:b], svc('rk', hp), KDS[:, a:b], ALU.mult, ALU.mult)
                    psb_ = PSF()
                    P.mmf(psb_[:, :n], BDM[:], pr[:, :n])
                    P.tt('dve', mu[:, :n], psb_[:, :n], v_[:, a:b], ALU.mult)
                    P.tt('dve', sq[:, :n], sq[:, :n], mu[:, :n], ALU.add)
                    yo = YOUT[(a // 512) % 2]
                    P.tt('dve', yo[:, :n], sq[:, :n], psg[:, :n], ALU.mult)
                    P.dma('sp', ym_d[hp, :, a:b], yo[:, :n])
            P.barrier()
        if STOP == 5:
            nc._declared_inputs = set(k for k in dram if k in SPEC)
            return nc


        def load_w2(dst, src, rows_kc, ncols, stg):
            i = 0
            for kc in range(rows_kc):
                for c0 in range(0, ncols, 512):
                    st = stg[i % 2]
                    i += 1
                    P.dma('sp', st[:, 0:512], src[kc * 128:(kc + 1) * 128, c0:c0 + 512])
                    P.copy(CV(), dst[:, kc, c0:c0 + 512], st[:, 0:512])

        def layer_norm(XT, a, b, gname, bname, tmp):
            n = b - a
            SQa, SQb, MU, RS = tmp
            psm = PSF()
            for c in range(8):
                P.mmf(psm[:, :n], ONESF[:], XT[:, c, a:b], start=(c == 0), stop=(c == 7))
            psq = PSF()
            for c in range(8):
                sq = SQa if c % 2 == 0 else SQb
                P.act(sq[:, :n], XT[:, c, a:b], AF.Square)
                P.mmf(psq[:, :n], ONESF[:], sq[:, :n], start=(c == 0), stop=(c == 7))
            P.ts('dve', MU[:, :n], psm[:, :n], 1.0 / D, None, ALU.mult)
            P.tt('dve', RS[:, :n], MU[:, :n], MU[:, :n], ALU.mult)
            P.stt('dve', RS[:, :n], psq[:, :n], 1.0 / D, RS[:, :n], ALU.mult, ALU.subtract)
            P.act(RS[:, :n], RS[:, :n], AF.Sqrt, bias=LN_EPS / (ALPHA * ALPHA))
            P.recip(RS[:, :n], RS[:, :n])
            for c in range(8):
                eng = 'dve' if c % 2 == 0 else 'pool'
                P.tt(eng, XT[:, c, a:b], XT[:, c, a:b], MU[:, :n], ALU.subtract)
                P.tt(eng, XT[:, c, a:b], XT[:, c, a:b], RS[:, :n], ALU.mult)
                P.ts(eng, XT[:, c, a:b], XT[:, c, a:b], svc(gname, c), svc(bname, c), ALU.mult, ALU.add)

        def phase_D(layer, w_out_name, segs, tiles, last):
            L = layer
            with ExitStack() as ph:
                XT = sb('XTd%d' % L, [128, NCH, T], F32, ph)
                for c in range(8):
                    P.dma('sp', XT[:, c, :], xt_d[:, c, :])
                HT2 = sb('HT2_%d' % L, [128, NCH, T], BF16, ph)
                LNT = [sb('LNT%d_%d' % (L, i), [128, 512], F32, ph) for i in range(4)]
                with ExitStack() as p1:
                    WO = sb('WO%d' % L, [128, 8, 1024], BF16, p1)
                    STG = [sb('STGo%d_%d' % (L, i), [128, 512], F32, p1) for i in range(2)]
                    load_w2(WO, W(w_out_name), 8, 1024, STG)
                    YM = [sb('YM%d_%d' % (L, i), [128, 8, 512], BF16, p1) for i in range(2)]
                    for si, (a, b) in enumerate(segs):
                        n = b - a
                        w = 1 if a < TC else 0
                        ym = YM[si % 2]
                        for kc in range(8):
                            P.dma('sp', ym[:, kc, :n], ym_d[kc, :, a:b])
                        for c in range(8):
                            ps = PSF()
                            for kc in range(8):
                                P.mm(ps[:, :n], WO[:, kc, c * 128:(c + 1) * 128], ym[:, kc, :n], start=(kc == 0), stop=(kc == 7))
                            P.stt('dve', XT[:, c, a:b], ps[:, :n], modc(L, 2, c, w), XT[:, c, a:b], ALU.mult, ALU.add)
                        layer_norm(XT, a, b, 'ln1_g%d' % L, 'ln1_b%d' % L, LNT)
                        for c in range(8):
                            P.ts('pool' if c % 2 else 'dve', HT2[:, c, a:b], XT[:, c, a:b], modc(L, 4, c, w), modc(L, 3, c, w),
                                 ALU.mult, ALU.add)
                    P.barrier()
                if ('xln1_tap%d' % L) in taps:
                    tp = dscr('xln1_tap%d' % L, [128, NCH, T])
                    for c in range(8):
                        P.dma('sp', tp[:, c, :], XT[:, c, :])
                if STOP == 61:
                    P.barrier(); return
                GTb = sb('GTb%d' % L, [16, T], BF16, ph)
                with ExitStack() as p2:
                    RW = sb('RW%d' % L, [128, 8, 16], F32, p2)
                    P.dma('sp', RW[:], W('router_w').rearrange("(c p) e -> p c e", p=128))
                    RWS = [sb('RWS%d_%d' % (L, i), [128, 8, 16], F32, p2) for i in range(2)]
                    SHB = [sb('SHB%d_%d' % (L, i), [128, 8, 128], F32, p2) for i in range(2)]
                    for w in range(2):
                        for c in range(8):
                            P.ts('dve', RWS[w][:, c, :], RW[:, c, :], modc(L, 4, c, w), None, ALU.mult)
                            P.ts('pool', SHB[w][:, c, :], ONESF[:], modc(L, 3, c, w), None, ALU.mult)
                    RT_sets = [[sb('RTR%d_%d_%d' % (L, j, i), [128, 16], F32, p2) for i in range(6)] for j in range(4)]
                    RS_sets = [[sb('RSM%d_%d_%d' % (L, j, i), [128, 4], F32, p2) for i in range(6)] for j in range(4)]
                    pend_r = []
                    GTf = sb('GTf%d' % L, [16, 128], F32, p2)
                    for tt in tiles:
                        w = 1 if tt < 2 else 0
                        tok = slice(tt * 128, (tt + 1) * 128)
                        ps = PSF()
                        for c in range(8):
                            P.mmf(ps[:, 0:16], XT[:, c, tok], RWS[w][:, c, :], start=(c == 0), stop=False)
                        for c in range(8):
                            P.mmf(ps[:, 0:16], SHB[w][:, c, :], RW[:, c, :], start=False, stop=(c == 7))
                        LG, E, EQ, EM, SEL, GATE = [t_[:] for t_ in RT_sets[tt % 4]]
                        M1, M2, GS, ING, MX, RG = [t_[:] for t_ in RS_sets[tt % 4]]
                        v3 = lambda x: x.rearrange("p (g e) -> p g e", g=4)
                        P.tt('dve', LG, ps[:, 0:16], rvc('router_b', 0, 16), ALU.add)
                        P.reduce('dve', MX[:, 0:1], LG, ALU.max)
                        P.ts('dve', MX[:, 1:2], MX[:, 0:1], -1.0, None, ALU.mult)
                        P.act(E, LG, AF.Exp, bias=MX[:, 1:2])
                        P.reduce('dve', M1, v3(E), ALU.max)
                        P.tt('dve', v3(EQ), v3(E), M1.unsqueeze(2).broadcast_to([128, 4, 4]), ALU.is_equal)
                        P.tt('dve', EQ, EQ, E, ALU.mult)
                        P.tt('dve', EM, E, EQ, ALU.subtract)
                        P.reduce('dve', M2, v3(EM), ALU.max)
                        P.tt('dve', GS, M1, M2, ALU.add)
                        P.reduce('dve', MX[:, 2:3], GS, ALU.max)
                        P.ts('dve', ING, GS, MX[:, 2:3], None, ALU.is_equal)
                        P.tt('dve', v3(SEL), v3(E), M2.unsqueeze(2).broadcast_to([128, 4, 4]), ALU.is_ge)
                        P.tt('dve', v3(SEL), v3(SEL), ING.unsqueeze(2).broadcast_to([128, 4, 4]), ALU.mult)
                        P.recip(RG[:, 0:1], MX[:, 2:3])
                        P.stt('dve', GATE, E, RG[:, 0:1], SEL, ALU.mult, ALU.mult)
                        def fin(GATE=GATE, tok=tok):
                            pt_ = PSF()
                            P.tr(pt_[0:16, 0:128], GATE, IDF[:])
                            P.copy('act', GTb[:, tok], pt_[0:16, 0:128])
                        pend_r.append(fin)
                        while len(pend_r) > 2:
                            pend_r.pop(0)()
                    while pend_r:
                        pend_r.pop(0)()
                    P.barrier()
                if ('gate_tap%d' % L) in taps:
                    tp = dscr('gate_tap%d' % L, [16, T], BF16)
                    P.dma('sp', tp[:, :], GTb[:])
                if STOP == 62:
                    P.barrier(); return
                with ExitStack() as p3:
                    SELM = sb('SELM%d' % L, [16, 16, 128], BF16, p3)
                    for e in range(16):
                        P.ts('dve', SELM[:, e, :], ONESF[0:16, :], IDF[0:16, e:e + 1], None, ALU.mult)
                    W1s = [sb('W1_%d_%d' % (L, i), [128, 8, 512], BF16, p3) for i in range(2)]
                    W3s = [sb('W3_%d_%d' % (L, i), [128, 8, 512], BF16, p3) for i in range(2)]
                    W2s = [sb('W2_%d_%d' % (L, i), [128, 4, 1024], BF16, p3) for i in range(2)]
                    STG = [sb('STGe%d_%d' % (L, i), [128, 512], F32, p3) for i in range(2)]
                    HID = [sb('HID%d_%d' % (L, i), [128, 4, 512], BF16, p3) for i in range(2)]
                    GB = [sb('GB%d_%d' % (L, i), [128, 512], F32, p3) for i in range(2)]
                    S1 = [sb('S1_%d_%d' % (L, i), [128, 512], F32, p3) for i in range(2)]
                    print('MoE phase sbuf remaining', nc.sbuf_bytes_remaining)
                    NE = int(os.environ.get('KEXP', '16'))
                    items = [(e, si, a, b) for e in range(NE) for si, (a, b) in enumerate(segs)]

                    def up(k):
                        e, si, a, b = items[k]
                        n = b - a
                        W1, W3, W2 = W1s[e % 2], W3s[e % 2], W2s[e % 2]
                        if si == 0:
                            load_w2(W1, W('moe_w1')[L, e], 8, 512, STG)
                            load_w2(W3, W('moe_w3')[L, e], 8, 512, STG)
                            load_w2(W2, W('moe_w2')[L, e], 4, 1024, STG)
                        gb, hid = GB[k % 2], HID[k % 2]
                        psg = PSF()
                        P.mm(psg[:, :n], SELM[:, e, :], GTb[:, a:b])
                        P.copy('act', gb[:, :n], psg[:, :n])
                        for fc in range(4):
                            ps1 = PSF()
                            for kc in range(8):
                                P.mm(ps1[:, :n], W1[:, kc, fc * 128:(fc + 1) * 128], HT2[:, kc, a:b], start=(kc == 0), stop=(kc == 7))
                            ps3 = PSF()
                            for kc in range(8):
                                P.mm(ps3[:, :n], W3[:, kc, fc * 128:(fc + 1) * 128], HT2[:, kc, a:b], start=(kc == 0), stop=(kc == 7))
                            s1 = S1[fc % 2]
                            P.act(s1[:, :n], ps1[:, :n], AF.Silu)
                            P.tt('dve', s1[:, :n], s1[:, :n], ps3[:, :n], ALU.mult)
                            P.tt('pool', hid[:, fc, :n], s1[:, :n], gb[:, :n], ALU.mult)

                    def down(k):
                        e, si, a, b = items[k]
                        n = b - a
                        w = 1 if a < TC else 0
                        W2 = W2s[e % 2]
                        hid = HID[k % 2]
                        for c in range(8):
                            ps = PSF()
                            for fc in range(4):
                                P.mm(ps[:, :n], W2[:, fc, c * 128:(c + 1) * 128], hid[:, fc, :n], start=(fc == 0), stop=(fc == 3))
                            P.stt('dve', XT[:, c, a:b], ps[:, :n], modc(L, 5, c, w), XT[:, c, a:b], ALU.mult, ALU.add)

                    up(0)
                    for k in range(len(items)):
                        if k + 1 < len(items):
                            up(k + 1)
                        down(k)
                    P.barrier()
                for (a, b) in segs:
                    layer_norm(XT, a, b, 'ln2_g%d' % L, 'ln2_b%d' % L, LNT)
                if not last:
                    for c in range(8):
                        P.dma('sp', xt_d[:, c, :], XT[:, c, :])
                else:
                    OS = [sb('OS%d' % i, [128, D], F32, ph) for i in range(2)]
                    for tt in range(2, NT):
                        os_ = OS[tt % 2]
                        tok = slice(tt * 128, (tt + 1) * 128)
                        for hh in range(2):
                            ps = PSF()
                            for j in range(4):
                                P.tr(ps[:, j * 128:(j + 1) * 128], XT[:, hh * 4 + j, tok], IDF[:])
                            P.copy(EV(), os_[:, hh * 512:(hh + 1) * 512], ps[:, :])
                        P.dma('sp', out_d[(tt - 2) * 128:(tt - 1) * 128, :], os_[:])
                P.barrier()

        phase_D(0, 'ev_w_out', SEGS, list(range(NT)), False)
        if STOP in (6, 61, 62):
            nc._declared_inputs = set(k for k in dram if k in SPEC)
            return nc


        with ExitStack() as ph:
            HT1 = sb('HT1', [128, NCH, T], BF16, ph)
            with ExitStack() as px:
                XC = [sb('XC%d' % i, [128, T], F32, px) for i in range(2)]
                for c in range(8):
                    xc = XC[c % 2]
                    P.dma('sp', xc[:], xt_d[:, c, :])
                    P.ts('dve', HT1[:, c, 0:TC], xc[:, 0:TC], modc(1, 1, c, 1), modc(1, 0, c, 1), ALU.mult, ALU.add)
                    P.ts('pool', HT1[:, c, TC:T], xc[:, TC:T], modc(1, 1, c, 0), modc(1, 0, c, 0), ALU.mult, ALU.add)
                P.barrier()
            WQ = sb('WQ', [128, 8, 1536], BF16, ph)
            STG = [sb('STGq%d' % i, [128, 512], F32, ph) for i in range(2)]
            load_w2(WQ, W('od_w_in'), 8, 1536, STG)
            CC = sb('CC', [128, 16, 64], F32, ph)
            SC = sb('SC', [128, 16, 64], F32, ph)
            P.dma('sp', CC[:], W('cosc').rearrange("(n p) f -> p n f", p=128))
            P.dma('sp', SC[:], W('sinc').rearrange("(n p) f -> p n f", p=128))
            QT1 = sb('QT1', [128, 8, TL], BF16, ph)
            KT1 = sb('KT1', [128, 2, T], BF16, ph)
            V1 = sb('V1', [128, NT, 256], BF16, ph)
            XQs = [sb('XQ%d' % j, [128, 512], F32, ph) for j in range(3)]
            TQs = [sb('TQ%d' % j, [128, 512], F32, ph) for j in range(3)]
            RTQs = [[sb('RTQ%d_%d' % (j, i), [128, 4, 64], F32, ph) for i in range(4)] for j in range(3)]
            SSQs = [sb('SSQ%d' % j, [128, 4], F32, ph) for j in range(3)]
            QN = [sb('QN%d' % i, [128, 512], BF16, ph) for i in range(5)]
            qn_i = [0]

            def normrope(psv, H, gname, rope, tile_i):
                n = H * 128
                out = QN[qn_i[0] % 5]
                XQ, TQ, RTQ, SSQ = XQs[qn_i[0] % 3], TQs[qn_i[0] % 3], RTQs[qn_i[0] % 3], SSQs[qn_i[0] % 3]
                qn_i[0] += 1
                x3 = XQ[:, :n].rearrange("p (h d) -> p h d", h=H)
                t3 = TQ[:, :n].rearrange("p (h d) -> p h d", h=H)
                o3 = out[:, :n].rearrange("p (h d) -> p h d", h=H)
                P.copy('act', XQ[:, :n], psv)
                P.tt('dve', TQ[:, :n], XQ[:, :n], XQ[:, :n], ALU.mult)
                P.reduce('dve', SSQ[:, :H], t3, ALU.add)
                P.act(SSQ[:, :H], SSQ[:, :H], AF.Sqrt, bias=1e-6, scale=1.0 / 128)
                P.recip(SSQ[:, :H], SSQ[:, :H])
                P.tt('dve', t3, x3, SSQ[:, :H].unsqueeze(2).broadcast_to([128, H, 128]), ALU.mult)
                gb = rvc(gname, 0, 128).unsqueeze(1).broadcast_to([128, H, 128])
                if not rope:
                    P.tt('dve', o3, t3, gb, ALU.mult)
                    return out
                P.tt('pool', x3, t3, gb, ALU.mult)
                x1, x2 = x3[:, :, 0:64], x3[:, :, 64:128]
                cs = CC[:, tile_i, :].unsqueeze(1).broadcast_to([128, H, 64])
                sn = SC[:, tile_i, :].unsqueeze(1).broadcast_to([128, H, 64])
                t1, t2, t3_, t4 = [r[:, :H, :] for r in RTQ]
                P.tt('dve', t1, x1, cs, ALU.mult)
                P.tt('pool', t2, x2, sn, ALU.mult)
                P.tt('dve', o3[:, :, 0:64], t1, t2, ALU.subtract)
                P.tt('pool', t3_, x1, sn, ALU.mult)
                P.tt('dve', t4, x2, cs, ALU.mult)
                P.tt('pool', o3[:, :, 64:128], t3_, t4, ALU.add)
                return out

            pend_c = []
            for tt in range(NT):
                tok = slice(tt * 128, (tt + 1) * 128)
                lat = tt >= 2
                groups = [0, 1, 2] if lat else [2]
                for g in groups:
                    ps = PSF()
                    for kc in range(8):
                        P.mm(ps[:, :], HT1[:, kc, tok], WQ[:, kc, g * 512:(g + 1) * 512], start=(kc == 0), stop=(kc == 7))
                    if g < 2:
                        qn = normrope(ps[:, 0:512], 4, 'qn_g', True, tt - 2)

                        def fin(qn=qn, g=g, tt=tt):
                            pb = PSB()
                            for h in range(4):
                                P.tr(pb[:, h * 128:(h + 1) * 128], qn[:, h * 128:(h + 1) * 128], IDB[:])
                            P.copy(EV(), QT1[:, g * 4:(g + 1) * 4, (tt - 2) * 128:(tt - 1) * 128],
                                   pb[:, 0:512].rearrange("p (a b) -> p a b", a=4))
                    else:
                        P.copy('act', V1[:, tt, :], ps[:, 256:512])
                        kn = normrope(ps[:, 0:256], 2, 'kn_g', lat, tt - 2)

                        def fin(kn=kn, tok=tok):
                            pb = PSB()
                            for h in range(2):
                                P.tr(pb[:, h * 128:(h + 1) * 128], kn[:, h * 128:(h + 1) * 128], IDB[:])
                            P.copy(EV(), KT1[:, :, tok], pb[:, 0:256].rearrange("p (a b) -> p a b", a=2))
                    pend_c.append(fin)
                    while len(pend_c) > 2:
                        pend_c.pop(0)()
            while pend_c:
                pend_c.pop(0)()
            PT1 = [sb('PT1_%d' % i, [128, 512], BF16, ph) for i in range(3)]
            RR = sb('RR', [128, 512], F32, ph)
            ACC1 = [sb('ACC1_%d' % j_, [128, 512], F32, ph) for j_ in range(2)]
            OB = [sb('OB%d' % i, [128, 512], BF16, ph) for i in range(2)]
            cnt = 0
            SCL = 128 ** -0.5
            for kvh in range(2):
                for g4 in range(4):
                    head = kvh * 4 + g4
                    for qs in range(4):
                        a, b = qs * 512, (qs + 1) * 512
                        psO, psD = psf[0], psf[1]
                        def s_mm1(kt):
                            P.mm(psf[2 + (cnt + kt) % 4][:, :], KT1[:, kvh, kt * 128:(kt + 1) * 128], QT1[:, head, a:b])
                        s_mm1(0)
                        s_mm1(1)
                        for kt in range(NT):
                            pS = psf[2 + (cnt + kt) % 4]
                            pt = PT1[(cnt + kt) % 3]
                            if kt + 2 < NT:
                                s_mm1(kt + 2)
                            P.act(pt[:], pS[:, :], AF.Exp, scale=SCL)
                            P.mm(psO[:, :], V1[:, kt, kvh * 128:(kvh + 1) * 128], pt[:], start=(kt == 0), stop=(kt == NT - 1))
                            ai = 1 if kt % 3 == 2 else 0
                            acc = ACC1[ai]
                            aeng = 'pool' if ai else 'dve'
                            if kt == 0 or kt == 2:
                                P.copy(aeng, acc[:], pt[:])
                            else:
                                P.tt(aeng, acc[:], acc[:], pt[:], ALU.add)
                        cnt += NT
                        P.mmf(psD[:, :], ONESF[:], ACC1[0][:], start=True, stop=False)
                        P.mmf(psD[:, :], ONESF[:], ACC1[1][:], start=False, stop=True)
                        P.recip(RR[:], psD[:, :])
                        ob = OB[(head * 4 + qs) % 2]
                        P.tt('dve', ob[:], psO[:, :], RR[:], ALU.mult)
                        P.dma('sp', ym_d[head, :, TC + a:TC + b], ob[:])
            P.barrier()
        if STOP == 7:
            nc._declared_inputs = set(k for k in dram if k in SPEC)
            return nc
        phase_D(1, 'od_w_out', SEGS[1:], list(range(2, NT)), True)

        P.barrier()
        print("ops", P.nops)
    nc._declared_inputs = set(k for k in dram if k in SPEC)
    return nc


_CACHE = {}


def kernel(**inp):
    inp = {k: np.asarray(v) for k, v in inp.items()}
    taps = tuple(inp.pop('_taps', ()))
    ncores = int(inp.pop('_ncores', 8))
    cosb, sinb = _rope_tables(64)
    cosc, sinc = _rope_tables(128)
    ident = np.eye(128, dtype=np.float32)
    bdmask = np.zeros((128, 128), np.float32)
    bdmask[:64, :64] = 1.0
    bdmask[64:, 64:] = 1.0
    tri = np.zeros((4, 128, 128), np.float32)
    for blk in range(2):
        o = blk * 64
        ii, jj = np.meshgrid(np.arange(64), np.arange(64), indexing='ij')
        tri[0, o:o + 64, o:o + 64] = (ii < jj)
        tri[1, o:o + 64, o:o + 64] = (ii <= jj)
        tri[2, o:o + 64, o:o + 64] = (ii > jj)
        tri[3, o:o + 64, o:o + 64] = (ii >= jj)
    masks = np.ascontiguousarray(np.concatenate([tri[0], tri[1], tri[2], tri[3], tri[0], tri[0]], axis=1))
    shared = {
        'ident': ident, 'bdmask': bdmask, 'masks': masks,
        'ada_w': np.ascontiguousarray(inp['ada_w'], np.float32),
        'ev_w_in': np.ascontiguousarray(inp['ev_w_in'][0]), 'ev_w_out': np.ascontiguousarray(inp['ev_w_out'][0]),
        'od_w_in': np.ascontiguousarray(inp['od_w_in'][0]), 'od_w_out': np.ascontiguousarray(inp['od_w_out'][0]),
        'moe_w1': inp['moe_w1'], 'moe_w3': inp['moe_w3'], 'moe_w2': inp['moe_w2'],
        'router_w': inp['router_w'],
        'a_w2': np.ascontiguousarray(inp['ev_a_w2'][0]), 'a_a2': np.ascontiguousarray(inp['ev_a_a2'][0]),
        'a_g2': np.ascontiguousarray(inp['ev_a_g2'][0]),
        'cosb': cosb, 'sinb': sinb, 'cosc': cosc, 'sinc': sinc,
    }
    in_maps = []
    for b in range(ncores):
        sv, rv = _pack_small(inp, b)
        m = dict(shared)
        m['xin'] = np.ascontiguousarray(np.concatenate([inp['ctx'][b], inp['x'][b]], axis=0), np.float32)
        m['sv'] = sv
        m['rv'] = rv
        in_maps.append(m)
    nc = build_program(SV_LAYOUT['_n'], RV_LAYOUT['_n'], taps)
    used = nc._declared_inputs
    in_maps = [{k: v for k, v in m.items() if k in used} for m in in_maps]
    res = run_bass_kernel_spmd(nc, in_maps, core_ids=list(range(ncores)))
    if taps:
        return res
    return np.stack([np.asarray(r['out'], np.float32) for r in res.results], axis=0)
```
